# Optimizing a Trainium2 kernel written in Bass

```python
import math
import jax, jax.numpy as jnp
from jax import lax
import numpy as np

D_MODEL = 2048
BATCH = 4
SEQ = 8192
DEPTH = 1

D_MIX = D_MODEL
N_ATTN_HEADS = 8
DIFF_HEAD_DIM = 64
ATTN_WIDTH = N_ATTN_HEADS * 2 * DIFF_HEAD_DIM
CONV_WIDTH = D_MIX - ATTN_WIDTH
CONV_TAPS = 3
IN_COLS = 3 * ATTN_WIDTH + 3 * CONV_WIDTH
Q_BLOCK = 128
N_MEM = 256
MEM_HEADS = 4
MEM_HEAD_DIM = D_MODEL // MEM_HEADS
N_EXPERTS = 32
TOP_K = 4
D_FF = D_MODEL
SWIGLU_LIMIT = 7.0
SWIGLU_ALPHA = 1.702
EXPERT_BLOCK = 512
DEEPNORM_ALPHA = (2.0 * DEPTH) ** 0.25
DEEPNORM_BETA = (8.0 * DEPTH) ** -0.25
LN_EPS = 1e-5

kernel_name = "hybrid_diffattn_shortconv_memxattn_moe_deepnorm"


def _layer_norm(x, g, b):
    xf = x.astype(jnp.float32)
    mu = jnp.mean(xf, axis=-1, keepdims=True)
    var = jnp.mean(jnp.square(xf - mu), axis=-1, keepdims=True)
    y = (xf - mu) * lax.rsqrt(var + LN_EPS)
    return (y * g.astype(jnp.float32) + b.astype(jnp.float32)).astype(x.dtype)


def _rms_norm(x, g):
    xf = x.astype(jnp.float32)
    y = xf * lax.rsqrt(jnp.mean(jnp.square(xf), axis=-1, keepdims=True) + LN_EPS)
    return (y * g.astype(jnp.float32)).astype(x.dtype)


def _alibi_slopes(n_heads):
    return jnp.exp2(-8.0 / n_heads * jnp.arange(1, n_heads + 1, dtype=jnp.float32))


def _diff_attention(q, k, v, lam):
    bsz, seq = q.shape[0], q.shape[1]
    nqb = seq // Q_BLOCK
    qb = q.reshape(bsz, nqb, Q_BLOCK, N_ATTN_HEADS, 2, DIFF_HEAD_DIM).transpose(1, 0, 4, 3, 2, 5)
    kt = k.transpose(0, 3, 2, 1, 4)
    vt = v.transpose(0, 2, 1, 3)
    slopes = _alibi_slopes(N_ATTN_HEADS)
    kpos = jnp.arange(seq, dtype=jnp.int32)
    scale = DIFF_HEAD_DIM ** -0.5

    def block(args):
        q_blk, i = args
        qpos = i * Q_BLOCK + jnp.arange(Q_BLOCK, dtype=jnp.int32)
        dist = jnp.abs(qpos[:, None] - kpos[None, :]).astype(jnp.float32)
        bias = -slopes[:, None, None] * dist
        s = jnp.einsum('bmhqd,bmhkd->bmhqk', q_blk, kt).astype(jnp.float32) * scale + bias
        p = jax.nn.softmax(s, axis=-1)
        a = (p[:, 0] - lam * p[:, 1]).astype(vt.dtype)
        return jnp.einsum('bhqk,bhkd->bhqd', a, vt)

    o = lax.map(block, (qb, jnp.arange(nqb, dtype=jnp.int32)))
    return o.transpose(1, 0, 3, 2, 4).reshape(bsz, seq, N_ATTN_HEADS, 2 * DIFF_HEAD_DIM)


def _centred_depthwise_conv3(u, w):
    up = jnp.pad(u, ((0, 0), (1, 1), (0, 0)))
    return up[:, :-2] * w[0] + up[:, 1:-1] * w[1] + up[:, 2:] * w[2]


def _hybrid_mixer(h, w_in, conv_w, subln_w, lq1, lk1, lq2, lk2, w_out, lambda_init):
    bsz, seq, _ = h.shape
    proj = h @ w_in
    a, c = ATTN_WIDTH, CONV_WIDTH
    q, k, v, gate_b, gate_c, u = jnp.split(
        proj, [a, 2 * a, 3 * a, 3 * a + c, 3 * a + 2 * c], axis=-1)
    q = q.reshape(bsz, seq, N_ATTN_HEADS, 2, DIFF_HEAD_DIM)
    k = k.reshape(bsz, seq, N_ATTN_HEADS, 2, DIFF_HEAD_DIM)
    v = v.reshape(bsz, seq, N_ATTN_HEADS, 2 * DIFF_HEAD_DIM)
    lam = (jnp.exp(jnp.sum(lq1.astype(jnp.float32) * lk1.astype(jnp.float32)))
           - jnp.exp(jnp.sum(lq2.astype(jnp.float32) * lk2.astype(jnp.float32)))
           + lambda_init)
    o_attn = _diff_attention(q, k, v, lam)
    o_attn = (_rms_norm(o_attn, subln_w) * (1.0 - lambda_init)).reshape(bsz, seq, ATTN_WIDTH)
    o_conv = gate_b * _centred_depthwise_conv3(gate_c * u, conv_w)
    return jnp.concatenate([o_attn, o_conv], axis=-1) @ w_out


def _memory_attention(h, mem, wq, wkv, wo):
    bsz, seq, _ = h.shape
    n_mem = mem.shape[1]
    q = (h @ wq).reshape(bsz, seq, MEM_HEADS, MEM_HEAD_DIM)
    k, v = jnp.split(mem @ wkv, 2, axis=-1)
    k = k.reshape(bsz, n_mem, MEM_HEADS, MEM_HEAD_DIM)
    v = v.reshape(bsz, n_mem, MEM_HEADS, MEM_HEAD_DIM)
    s = jnp.einsum('bshd,bmhd->bhsm', q, k).astype(jnp.float32) * (MEM_HEAD_DIM ** -0.5)
    p = jax.nn.softmax(s, axis=-1).astype(v.dtype)
    o = jnp.einsum('bhsm,bmhd->bshd', p, v).reshape(bsz, seq, D_MODEL)
    return o @ wo


def _moe(h, router_w, router_b, w_gate, b_gate, w_up, b_up, w_down, b_down):
    bsz, seq, d = h.shape
    n_tok = bsz * seq
    xf = h.reshape(n_tok, d)
    logits = (xf @ router_w + router_b).astype(jnp.float32)
    top_v, top_e = lax.top_k(logits, TOP_K)
    gates = jax.nn.softmax(top_v, axis=-1).astype(h.dtype)
    n_assign = n_tok * TOP_K
    flat_e = top_e.reshape(n_assign).astype(jnp.int32)
    flat_tok = jnp.repeat(jnp.arange(n_tok, dtype=jnp.int32), TOP_K)
    flat_g = gates.reshape(n_assign)
    order = jnp.argsort(flat_e)
    se, stok, sg = flat_e[order], flat_tok[order], flat_g[order]
    counts = jnp.bincount(flat_e, length=N_EXPERTS).astype(jnp.int32)
    starts = jnp.cumsum(counts) - counts
    padded = (counts + EXPERT_BLOCK - 1) // EXPERT_BLOCK * EXPERT_BLOCK
    pad_ends = jnp.cumsum(padded)
    pad_starts = pad_ends - padded
    dest = pad_starts[se] + (jnp.arange(n_assign, dtype=jnp.int32) - starts[se])
    n_blocks = -(-n_assign // EXPERT_BLOCK) + N_EXPERTS
    n_rows = n_blocks * EXPERT_BLOCK
    row_tok = jnp.zeros((n_rows,), jnp.int32).at[dest].set(stok)
    row_g = jnp.zeros((n_rows,), h.dtype).at[dest].set(sg)
    block_start = jnp.arange(n_blocks, dtype=jnp.int32) * EXPERT_BLOCK
    block_e = jnp.minimum(jnp.searchsorted(pad_ends, block_start, side='right'),
                          N_EXPERTS - 1).astype(jnp.int32)

    def expert_block(args):
        e, tok, g = args
        xb = xf[tok]
        gate = xb @ w_gate[e] + b_gate[e]
        up = xb @ w_up[e] + b_up[e]
        gate = jnp.minimum(gate, SWIGLU_LIMIT)
        up = jnp.clip(up, -SWIGLU_LIMIT, SWIGLU_LIMIT)
        act = gate * jax.nn.sigmoid(SWIGLU_ALPHA * gate) * (up + 1.0)
        return (act @ w_down[e] + b_down[e]) * g[:, None]

    outs = lax.map(expert_block, (block_e,
                                  row_tok.reshape(n_blocks, EXPERT_BLOCK),
                                  row_g.reshape(n_blocks, EXPERT_BLOCK)))
    y = jnp.zeros((n_tok, d), h.dtype).at[row_tok].add(outs.reshape(n_rows, d))
    return y.reshape(bsz, seq, d)


def _nrm(k, shape, scale):
    return scale * jax.random.normal(k, shape, jnp.float32)


def setup_inputs(seed: int = 0) -> dict:
    key = jax.random.key(seed)
    ks = jax.random.split(key, 32)
    L, D, E, F = DEPTH, D_MODEL, N_EXPERTS, D_FF
    beta = DEEPNORM_BETA
    col_scale = jnp.concatenate([
        jnp.ones((2 * ATTN_WIDTH,), jnp.float32),
        jnp.full((ATTN_WIDTH,), beta, jnp.float32),
        jnp.ones((3 * CONV_WIDTH,), jnp.float32)])
    kv_scale = jnp.concatenate([jnp.ones((D,), jnp.float32), jnp.full((D,), beta, jnp.float32)])
    return {
        "x": _nrm(ks[0], (BATCH, SEQ, D), 1.0),
        "mem": _nrm(ks[1], (BATCH, N_MEM, D), 1.0),
        "w_in": _nrm(ks[2], (L, D, IN_COLS), D ** -0.5) * col_scale,
        "conv_w": _nrm(ks[3], (L, CONV_TAPS, CONV_WIDTH), 0.5),
        "attn_subln_w": 1.0 + _nrm(ks[4], (L, 2 * DIFF_HEAD_DIM), 0.02),
        "lambda_q1": _nrm(ks[5], (L, DIFF_HEAD_DIM), 0.1),
        "lambda_k1": _nrm(ks[6], (L, DIFF_HEAD_DIM), 0.1),
        "lambda_q2": _nrm(ks[7], (L, DIFF_HEAD_DIM), 0.1),
        "lambda_k2": _nrm(ks[8], (L, DIFF_HEAD_DIM), 0.1),
        "w_out": _nrm(ks[9], (L, D_MIX, D), D_MIX ** -0.5 * beta),
        "ln1_g": 1.0 + _nrm(ks[10], (L, D), 0.02),
        "ln1_b": _nrm(ks[11], (L, D), 0.02),
        "mem_wq": _nrm(ks[12], (L, D, D), D ** -0.5),
        "mem_wkv": _nrm(ks[13], (L, D, 2 * D), D ** -0.5) * kv_scale,
        "mem_wo": _nrm(ks[14], (L, D, D), D ** -0.5 * beta),
        "ln2_g": 1.0 + _nrm(ks[15], (L, D), 0.02),
        "ln2_b": _nrm(ks[16], (L, D), 0.02),
        "router_w": _nrm(ks[17], (L, D, E), D ** -0.5),
        "router_b": _nrm(ks[18], (L, E), 0.01),
        "w_gate": _nrm(ks[19], (L, E, D, F), D ** -0.5),
        "b_gate": _nrm(ks[20], (L, E, F), 0.01),
        "w_up": _nrm(ks[21], (L, E, D, F), D ** -0.5),
        "b_up": _nrm(ks[22], (L, E, F), 0.01),
        "w_down": _nrm(ks[23], (L, E, F, D), F ** -0.5 * beta),
        "b_down": _nrm(ks[24], (L, E, D), 0.01),
        "ln3_g": 1.0 + _nrm(ks[25], (L, D), 0.02),
        "ln3_b": _nrm(ks[26], (L, D), 0.02),
    }


def reference(x, mem, w_in, conv_w, attn_subln_w, lambda_q1, lambda_k1, lambda_q2, lambda_k2,
              w_out, ln1_g, ln1_b, mem_wq, mem_wkv, mem_wo, ln2_g, ln2_b,
              router_w, router_b, w_gate, b_gate, w_up, b_up, w_down, b_down,
              ln3_g, ln3_b):
    for l in range(DEPTH):
        lambda_init = 0.8 - 0.6 * math.exp(-0.3 * l)
        mix = _hybrid_mixer(x, w_in[l], conv_w[l], attn_subln_w[l], lambda_q1[l], lambda_k1[l],
                            lambda_q2[l], lambda_k2[l], w_out[l], lambda_init)
        x = _layer_norm(DEEPNORM_ALPHA * x + mix, ln1_g[l], ln1_b[l])
        xa = _memory_attention(x, mem, mem_wq[l], mem_wkv[l], mem_wo[l])
        x = _layer_norm(DEEPNORM_ALPHA * x + xa, ln2_g[l], ln2_b[l])
        ff = _moe(x, router_w[l], router_b[l], w_gate[l], b_gate[l], w_up[l], b_up[l],
                  w_down[l], b_down[l])
        x = _layer_norm(DEEPNORM_ALPHA * x + ff, ln3_g[l], ln3_b[l])
    return x
```

```python
import math
import os
from contextlib import ExitStack

import numpy as np

import concourse.bass as bass
import concourse.mybir as mybir
from concourse.bass_utils import run_bass_kernel_spmd

F32 = mybir.dt.float32
BF16 = mybir.dt.bfloat16
I32 = mybir.dt.int32
AF = mybir.ActivationFunctionType
ALU = mybir.AluOpType
AX = mybir.AxisListType

D = 2048
SEQ = 8192
NB = 4
TOK = 4096
NH = 8
NE = 32
CAP = 768
NROWS = NE * CAP
ALPHA = 2.0 ** 0.25
LAMBDA_INIT = 0.8 - 0.6 * math.exp(0.0)
EPS = 1e-5
BIGROW = 1.0e6


class _Op:
    __slots__ = ("eng", "fn", "deps", "marked", "sem", "inc", "sigval", "is_dma", "gidx")

    def __init__(self, eng, fn):
        self.eng = eng
        self.fn = fn
        self.deps = []
        self.marked = False
        self.sem = None
        self.inc = 1
        self.sigval = None
        self.is_dma = False
        self.gidx = 0


class Sched:
    ENGS = ("pe", "act", "dve", "pool", "sp")

    def __init__(self, nc, stack):
        self.nc = nc
        self.stack = stack
        self.ops = {e: [] for e in self.ENGS}
        self.buf = {}
        self.sems = {}
        self.dma_count = {}
        self.dma_hist = {}
        self.gcount = 0
        self.last_dma = {}
        self.regs = {}
        self.pool_prev = {}

    def sem(self, name):
        if name not in self.sems:
            self.sems[name] = self.stack.enter_context(self.nc.semaphore(name))
        return self.sems[name]

    def _track(self, op, reads, writes):
        deps = []
        for k in reads:
            st = self.buf.get(k)
            if st is None:
                st = self.buf[k] = [None, []]
            if st[0] is not None:
                deps.append(st[0])
            st[1].append(op)
        for k in writes:
            st = self.buf.get(k)
            if st is None:
                st = self.buf[k] = [None, []]
            if st[0] is not None:
                deps.append(st[0])
            deps.extend(st[1])
            self.buf[k] = [op, []]
        seen = set()
        for d in deps:
            if d is op or id(d) in seen:
                continue
            seen.add(id(d))
            if d.eng == "pe" and op.eng == "pe" and not d.is_dma and not op.is_dma:
                continue
            d.marked = True
            op.deps.append(d)

    def op(self, eng, fn, reads=(), writes=()):
        o = _Op(eng, fn)
        self.gcount += 1
        o.gidx = self.gcount
        o.sem = "c_" + eng
        self._track(o, reads, writes)
        self.ops[eng].append(o)
        return o

    def dma(self, eng, fn, sem, reads=(), writes=()):
        o = _Op(eng, fn)
        self.gcount += 1
        o.gidx = self.gcount
        o.is_dma = True
        if eng == "pool":
            self.prot = (getattr(self, "prot", -1) + 1) % 16
            o.sem = "d_pq%d" % self.prot
            prev = self.last_dma.get(o.sem) or self.pool_prev.get(o.sem)
            if prev is not None:
                o.deps.append(prev)
            self.pool_prev[o.sem] = o
        else:
            o.sem = "d_" + sem
        o.inc = 16
        o.marked = True
        c = self.dma_count.get(o.sem, 0) + 16
        self.dma_count[o.sem] = c
        o.sigval = c
        self.dma_hist.setdefault(o.sem, []).append((o.gidx, c))
        self.last_dma[o.sem] = o
        self._track(o, reads, writes)
        self.ops[eng].append(o)
        return o

    def barrier(self):
        lasts = []
        for e in self.ENGS:
            for o in reversed(self.ops[e]):
                if not o.is_dma and o.fn is not None:
                    lasts.append(o)
                    break
        lasts.extend(self.last_dma.values())
        self.last_dma = {}
        for o in lasts:
            o.marked = True
        for e in self.ENGS:
            b = _Op(e, None)
            self.gcount += 1
            b.gidx = self.gcount
            b.sem = "c_" + e
            b.deps = [o for o in lasts]
            self.ops[e].append(b)
        self.buf = {}

    def emit(self, final_waits=()):
        nc = self.nc
        cnt = {e: 0 for e in self.ENGS}
        for e in self.ENGS:
            for o in self.ops[e]:
                if o.is_dma or o.fn is None:
                    continue
                if o.marked:
                    cnt[e] += 1
                    o.sigval = cnt[e]
        for name in sorted({o.sem for e in self.ENGS for o in self.ops[e] if o.marked}):
            self.sem(name)
        fw = {}
        for (ename, o) in final_waits:
            fw.setdefault(ename, []).append((o.sem, o.sigval))

        def run(ename, eng):
            waited = {}
            for o in self.ops[ename]:
                need = {}
                for d in o.deps:
                    v = d.sigval
                    if d.is_dma:
                        for (g, c) in self.dma_hist[d.sem]:
                            if g < o.gidx and c > v:
                                v = c
                    if need.get(d.sem, 0) < v:
                        need[d.sem] = v
                for sname, v in need.items():
                    if waited.get(sname, 0) >= v:
                        continue
                    if sname == "c_" + ename and ename == "pe":
                        continue
                    waited[sname] = v
                    eng.wait_ge(self.sems[sname], v)
                if o.fn is None:
                    continue
                ins = o.fn(eng)
                if o.marked:
                    ins.then_inc(self.sems[o.sem], o.inc)
            for (sname, v) in fw.get(ename, ()):
                eng.wait_ge(self.sems[sname], v)

        with nc.Block() as block:
            @block.tensor
            def _(eng):
                run("pe", eng)

            @block.scalar
            def _(eng):
                run("act", eng)

            @block.vector
            def _(eng):
                run("dve", eng)

            @block.gpsimd
            def _(eng):
                run("pool", eng)

            @block.sync
            def _(eng):
                run("sp", eng)


def MM(out, lhsT, rhs, start, stop):
    return lambda e: e.matmul(out, lhsT, rhs, start=start, stop=stop)


def TR(out, in_, ident):
    return lambda e: e.transpose(out, in_, ident)


def DMA(out, in_):
    return lambda e: e.dma_start(out=out, in_=in_)


def ACTF(out, in_, func, bias=None, scale=1.0, accum=None):
    def f(e):
        kw = {}
        if bias is not None:
            kw["bias"] = bias
        if accum is not None:
            kw["accum_out"] = accum
        return e.activation(out, in_, func, scale=scale, **kw)
    return f


def TC(out, in_):
    return lambda e: e.tensor_copy(out, in_)


def TS(out, in0, s1, s2, op0, op1=None):
    if op1 is None:
        return lambda e: e.tensor_scalar(out, in0, s1, None, op0=op0)
    return lambda e: e.tensor_scalar(out, in0, s1, s2, op0=op0, op1=op1)


def TT(out, in0, in1, op):
    return lambda e: e.tensor_tensor(out, in0, in1, op=op)


def STT(out, in0, scalar, in1, op0, op1):
    return lambda e: e.scalar_tensor_tensor(out, in0, scalar, in1, op0=op0, op1=op1)


def MEMSET(ap, v):
    return lambda e: e.memset(ap, v)

def mm_group(S, out_ap, pskey, pairs, reads):
    n = len(pairs)
    for i, (l, r) in enumerate(pairs):
        edge = (i == 0 or i == n - 1)
        S.op("pe", MM(out_ap, l, r, i == 0, i == n - 1),
             reads=reads if edge else (), writes=[pskey] if edge else ())


class SB:
    def __init__(self, big, nwords):
        self.big = big
        self.n = nwords
        self.off = 0
        self.base = 0

    def alloc(self, cols, dtype):
        size = 4 if dtype in (F32, I32) else 2
        words = (cols * size + 3) // 4
        assert self.off + words <= self.n, ("SBUF overflow", self.off, words, self.n)
        ap = self.big[:, self.off:self.off + words]
        self.off += words
        if dtype != F32:
            ap = ap.bitcast(dtype)
        return ap

    def persist(self):
        self.base = self.off

    def reset(self):
        self.off = self.base


def v3(ap, a):
    return ap.rearrange("p (a b) -> p a b", a=a)


def build_program(stop_after=99, dbg=False):
    nc = bass.Bass("TRN2", target_bir_lowering=False)

    def din(name, shape, dt=F32):
        return nc.dram_tensor(name, list(shape), dt, kind="ExternalInput").ap()

    def dscr(name, shape, dt, out=False):
        return nc.dram_tensor(name, list(shape), dt, kind="ExternalOutput" if out else "Internal").ap()

    xT_kv = din("xT_kv", [D, SEQ])
    xT_own = din("xT_own", [D, TOK + 2])
    x_tok = din("x_tok", [TOK, D])
    memT = din("memT", [D, 256])
    w_in = din("w_in", [D, 6144])
    convw = din("convw", [128, 24])
    subln = din("subln", [128, 1])
    lamv = din("lamv", [4, 64])
    w_out = din("w_out", [D, D])
    lnp = din("lnp", [6, D])
    mem_wq = din("mem_wq", [D, D])
    mem_wkv = din("mem_wkv", [D, 2 * D])
    mem_wo = din("mem_wo", [D, D])
    router_w = din("router_w", [D, NE])
    router_b = din("router_b", [NE])
    if stop_after >= 6:
        w_gate = din("w_gate", [NE, D, D])
        w_up = din("w_up", [NE, D, D])
        w_down = din("w_down", [NE, D, D])
    bgu = din("bgu", [128, 2 * NE * 16])
    b_down = din("b_down", [NE, D])
    kaug = din("kaug", [NH, 4, SEQ])
    qaug = din("qaug", [NH, 2, 4, TOK])
    tdiag = din("tdiag", [128, 4 * 512])
    ident_in = din("ident", [128, 128])
    ecap = din("ecap", [128, NE])
    utri = din("utri", [128, 128])

    out = dscr("out", [TOK, D], F32, out=True)
    KT = dscr("KT", [16, 64, SEQ], BF16, out=(dbg and stop_after == 1))
    VV = dscr("VV", [SEQ, 1024], BF16, out=(dbg and stop_after == 1))
    QT = dscr("QT", [16, 64, TOK], BF16, out=(dbg and stop_after == 2))
    OACT = dscr("OACT", [D, TOK], BF16, out=(dbg and stop_after in (2, 3)))
    X1 = dscr("X1", [TOK, D], F32, out=(dbg and stop_after == 4))
    X1T = dscr("X1T", [D, TOK], BF16)
    OM = dscr("OM", [D, TOK], BF16)
    X2 = dscr("X2", [TOK, D], F32, out=(dbg and stop_after == 5))
    XG = dscr("XG", [NROWS, D], BF16)
    YG = dscr("YG", [NROWS, D], F32)
    IDX4 = dscr("IDX4", [TOK, 4], I32, out=(dbg and stop_after == 5))
    G4 = dscr("G4", [TOK, 4], F32, out=(dbg and stop_after == 5))

    with ExitStack() as st:
        S = Sched(nc, st)
        NW = 53200
        big = st.enter_context(nc.sbuf_tensor("big", [128, NW], F32))
        sb = SB(big, NW)
        ps = st.enter_context(nc.psum_tensor("ps", [128, 4096], F32))

        def bank(i, n=512, off=0):
            return ps[:, i * 512 + off:i * 512 + off + n]

        ident = sb.alloc(128, F32)
        identb = sb.alloc(128, BF16)
        ones_b = sb.alloc(128, BF16)
        lam = sb.alloc(1, F32)
        mk_sb = sb.alloc(16 * 256, BF16)
        mv_sb = sb.alloc(2 * 2048, BF16)
        sb.persist()

        S.dma("sp", DMA(ident, ident_in), "c0", writes=["ident"])
        S.op("dve", TC(identb, ident), reads=["ident"], writes=["identb"])
        S.op("dve", MEMSET(ones_b, 1.0), writes=["ones_b"])

        def bcreg(e):
            if "bc" not in S.regs:
                S.regs["bc"] = e.to_reg(NROWS - 1)
            return S.regs["bc"]


        def phase0():
            lv = sb.alloc(4 * 64, F32)
            pr = sb.alloc(2 * 64, F32)
            sm = sb.alloc(2, F32)
            S.dma("sp", DMA(lv, lamv.rearrange("a b -> (a b)").partition_broadcast(128)), "c1", writes=["lv"])
            S.op("dve", TT(pr[:, 0:64], lv[:, 0:64], lv[:, 64:128], ALU.mult), reads=["lv"], writes=["pr0"])
            S.op("dve", TT(pr[:, 64:128], lv[:, 128:192], lv[:, 192:256], ALU.mult), reads=["lv"], writes=["pr1"])
            S.op("dve", lambda e: e.reduce_sum(sm[:, 0:1], pr[:, 0:64], axis=AX.X), reads=["pr0"], writes=["sm0"])
            S.op("dve", lambda e: e.reduce_sum(sm[:, 1:2], pr[:, 64:128], axis=AX.X), reads=["pr1"], writes=["sm1"])
            S.op("act", ACTF(sm, sm, AF.Exp), reads=["sm0", "sm1"], writes=["sme"])
            S.op("dve", TT(lam, sm[:, 0:1], sm[:, 1:2], ALU.subtract), reads=["sme"], writes=["lam0"])
            S.op("dve", TS(lam, lam, LAMBDA_INIT, None, ALU.add), reads=["lam0"], writes=["lam"])
            mT = sb.alloc(16 * 256, BF16)
            mT3 = v3(mT, 16)
            S.dma("pool", DMA(mT3, memT.rearrange("(kc p) m -> p kc m", p=128)), "mT", writes=["mT"])
            wg = [sb.alloc(16 * 512, BF16) for _ in range(2)]
            mk3 = v3(mk_sb, 16)
            mv3 = v3(mv_sb, 2)
            for g in range(8):
                w = wg[g % 2]
                w3 = v3(w, 16)
                wk = "wg%d" % (g % 2)
                S.dma("pool", DMA(w3, mem_wkv[:, g * 512:(g + 1) * 512].rearrange("(kc p) c -> p kc c", p=128)),
                      wk, writes=[wk])
                if g < 4:
                    for s_ in range(4):
                        b = (g * 4 + s_) % 4
                        mm_group(S, bank(b, 256), "ps%d" % b,
                                 [(w3[:, kc, s_ * 128:(s_ + 1) * 128], mT3[:, kc, :]) for kc in range(16)], [wk, "mT"])
                        last = S.op("act", lambda e, b=b, c=g * 4 + s_: e.copy(mk3[:, c, :], bank(b, 256)),
                                    reads=["ps%d" % b], writes=["mk%d" % (g * 4 + s_)])
                else:
                    for m in range(2):
                        b = 4 + (g * 2 + m) % 4
                        mm_group(S, bank(b), "ps%d" % b,
                                 [(mT3[:, kc, m * 128:(m + 1) * 128], w3[:, kc, :]) for kc in range(16)], [wk, "mT"])
                        S.op("dve", TC(mv3[:, m, (g - 4) * 512:(g - 3) * 512], bank(b)),
                             reads=["ps%d" % b], writes=["mv%d_%d" % (m, g)])

        phase0()
        S.barrier()
        sb.reset()

        def phase1a():
            wk_sb = sb.alloc(16 * 1024, BF16)
            wv_sb = sb.alloc(16 * 1024, BF16)
            wk3 = v3(wk_sb, 16)
            wv3 = v3(wv_sb, 16)
            for h in range(2):
                S.dma("pool", DMA(wk3[:, :, h * 512:(h + 1) * 512],
                                  w_in[:, 1024 + h * 512:1024 + (h + 1) * 512].rearrange("(kc p) c -> p kc c", p=128)),
                      "wk", writes=["wk%d" % h])
                S.dma("pool", DMA(wv3[:, :, h * 512:(h + 1) * 512],
                                  w_in[:, 2048 + h * 512:2048 + (h + 1) * 512].rearrange("(kc p) c -> p kc c", p=128)),
                      "wv", writes=["wv%d" % h])
            xs = [sb.alloc(16 * 512, BF16) for _ in range(2)]
            kst = [sb.alloc(512, BF16) for _ in range(4)]
            vst = [sb.alloc(1024, BF16) for _ in range(2)]
            nt = SEQ // 512
            ki = 0
            vi = 0
            for t in range(nt):
                x = xs[t % 2]
                x3 = v3(x, 16)
                xk = "x%d" % (t % 2)
                S.dma("pool", DMA(x3, xT_kv[:, t * 512:(t + 1) * 512].rearrange("(kc p) c -> p kc c", p=128)),
                      xk, writes=[xk])
                for c in range(8):
                    b = c % 4
                    mm_group(S, bank(b), "ps%d" % b,
                             [(wk3[:, kc, c * 128:(c + 1) * 128], x3[:, kc, :]) for kc in range(16)], [xk, "wk0", "wk1"])
                    ks = kst[ki % 4]
                    kk = "kst%d" % (ki % 4)
                    ki += 1
                    S.op("act", lambda e, ks=ks, b=b: e.copy(ks, bank(b)), reads=["ps%d" % b], writes=[kk])
                    for hh in range(2):
                        S.dma("sp", DMA(KT[2 * c + hh, :, t * 512:(t + 1) * 512], ks[hh * 64:(hh + 1) * 64, :]),
                              kk, reads=[kk])
                for s_ in range(4):
                    vs = vst[vi % 2]
                    vk = "vst%d" % (vi % 2)
                    vi += 1
                    for g in range(2):
                        b = 4 + (s_ * 2 + g) % 4
                        mm_group(S, bank(b), "ps%d" % b,
                                 [(x3[:, kc, s_ * 128:(s_ + 1) * 128], wv3[:, kc, g * 512:(g + 1) * 512]) for kc in range(16)],
                                 [xk, "wv0", "wv1"])
                        S.op("dve", TC(vs[:, g * 512:(g + 1) * 512], bank(b)), reads=["ps%d" % b], writes=[vk + "_%d" % g])
                    S.dma("sp", DMA(VV[t * 512 + s_ * 128:t * 512 + (s_ + 1) * 128, :], vs),
                          vk, reads=[vk + "_0", vk + "_1"])

        phase1a()
        S.barrier()
        sb.reset()
        if stop_after <= 1:
            fin = S.dma("sp", DMA(out[0:128, 0:128], ident), "fin", reads=["ident"])
            S.emit(final_waits=[("sp", fin)])
            return nc

        def phase1b():
            wq_sb = sb.alloc(16 * 4096, BF16)
            w3 = v3(wq_sb, 16)
            for g in range(8):
                src = g * 512 if g < 2 else 3072 + (g - 2) * 512
                S.dma("pool", DMA(w3[:, :, g * 512:(g + 1) * 512],
                                  w_in[:, src:src + 512].rearrange("(kc p) c -> p kc c", p=128)),
                      "w1b", writes=["w1b_%d" % g])
            wkeys = ["w1b_%d" % g for g in range(8)]
            cw = sb.alloc(24, F32)
            S.dma("sp", DMA(cw, convw), "c1", writes=["cw"])
            xs = [sb.alloc(16 * 512, BF16) for _ in range(2)]
            qst = [sb.alloc(512, BF16) for _ in range(2)]
            csb = [sb.alloc(512, F32) for _ in range(2)]
            cu = [sb.alloc(512, F32) for _ in range(2)]
            acc = [sb.alloc(512, F32) for _ in range(2)]
            ocv = [sb.alloc(512, BF16) for _ in range(2)]
            qi_ = 0
            ci_ = 0
            for j in range(9):
                h0 = 510 * j
                w = min(512, TOK + 2 - h0)
                x = xs[j % 2]
                x3 = v3(x, 16)
                xk = "x%d" % (j % 2)
                S.dma("pool", DMA(x3[:, :, 0:w], xT_own[:, h0:h0 + w].rearrange("(kc p) c -> p kc c", p=128)),
                      xk, writes=[xk])
                for c in range(8):
                    b = c % 2
                    mm_group(S, bank(b, w), "ps%d" % b,
                             [(w3[:, kc, c * 128:(c + 1) * 128], x3[:, kc, 0:w]) for kc in range(16)], [xk] + wkeys)
                    qs = qst[qi_ % 2]
                    qk = "qst%d" % (qi_ % 2)
                    qi_ += 1
                    S.op("act", ACTF(qs[:, 0:w], bank(b, w), AF.Copy, scale=0.125), reads=["ps%d" % b], writes=[qk])
                    for hh in range(2):
                        S.dma("sp", DMA(QT[2 * c + hh, :, h0:h0 + w - 2], qs[hh * 64:(hh + 1) * 64, 1:w - 1]),
                              qk, reads=[qk])
                for cc in range(8):
                    par = ci_ % 2
                    ci_ += 1
                    bB, bC, bU = 2 + 3 * par, 3 + 3 * par, 4 + 3 * par
                    for (bb, off) in ((bB, 1024), (bC, 2048), (bU, 3072)):
                        mm_group(S, bank(bb, w), "ps%d" % bb,
                                 [(w3[:, kc, off + cc * 128:off + (cc + 1) * 128], x3[:, kc, 0:w]) for kc in range(16)],
                                 [xk] + wkeys)
                    cs, cu_, ac, oc = csb[par], cu[par], acc[par], ocv[par]
                    S.op("act", lambda e, cs=cs, bC=bC, w=w: e.copy(cs[:, 0:w], bank(bC, w)),
                         reads=["ps%d" % bC], writes=["cs%d" % par])
                    S.op("dve", TT(cu_[:, 0:w], cs[:, 0:w], bank(bU, w), ALU.mult),
                         reads=["cs%d" % par, "ps%d" % bU], writes=["cu%d" % par])
                    S.op("dve", TS(ac[:, 0:w - 2], cu_[:, 1:w - 1], cw[:, cc * 3 + 1:cc * 3 + 2], None, ALU.mult),
                         reads=["cu%d" % par, "cw"], writes=["ac%d" % par])
                    S.op("dve", STT(ac[:, 0:w - 2], cu_[:, 0:w - 2], cw[:, cc * 3:cc * 3 + 1], ac[:, 0:w - 2], ALU.mult, ALU.add),
                         reads=["cu%d" % par, "cw", "ac%d" % par], writes=["ac%d" % par])
                    S.op("dve", STT(ac[:, 0:w - 2], cu_[:, 2:w], cw[:, cc * 3 + 2:cc * 3 + 3], ac[:, 0:w - 2], ALU.mult, ALU.add),
                         reads=["cu%d" % par, "cw", "ac%d" % par], writes=["ac%d" % par])
                    S.op("dve", TT(oc[:, 0:w - 2], ac[:, 0:w - 2], bank(bB, w)[:, 1:w - 1], ALU.mult),
                         reads=["ac%d" % par, "ps%d" % bB], writes=["oc%d" % par])
                    S.dma("sp", DMA(OACT[1024 + cc * 128:1024 + (cc + 1) * 128, h0:h0 + w - 2], oc[:, 0:w - 2]),
                          "oc%d" % par, reads=["oc%d" % par])

        phase1b()
        S.barrier()
        sb.reset()
        if stop_after <= 2:
            fin = S.dma("sp", DMA(out[0:128, 0:128], ident), "fin", reads=["ident"])
            S.emit(final_waits=[("sp", fin)])
            return nc

        def phase2():
            T0 = sb.alloc(2048, F32)
            S.dma("sp", DMA(T0, tdiag), "c1", writes=["T0"])
            sl_t = sb.alloc(1, F32)
            S.dma("sp", DMA(sl_t, subln), "c0", writes=["sl_raw"])
            subs = sb.alloc(1, F32)
            S.op("dve", TS(subs, sl_t, 1.0 - LAMBDA_INIT, None, ALU.mult), reads=["sl_raw"], writes=["subs"])
            neglam = sb.alloc(1, F32)
            S.op("dve", TS(neglam, lam, -1.0, None, ALU.mult), writes=["neglam"])
            eps_t = sb.alloc(1, F32)
            S.op("dve", MEMSET(eps_t, EPS), writes=["eps_t"])
            sets = []
            for _ in range(2):
                kx = [sb.alloc(SEQ, BF16) for _ in range(2)]
                vh = sb.alloc(64 * 128, BF16)
                qx = [[sb.alloc(TOK, BF16) for _ in range(2)] for _ in range(2)]
                sets.append((kx, vh, qx))
            Eb = [sb.alloc(512, BF16) for _ in range(4)]
            tmpb = [sb.alloc(512, F32) for _ in range(2)]
            rzt = sb.alloc(512, F32)
            ob = [sb.alloc(512, F32) for _ in range(2)]
            df = sb.alloc(512, F32)
            sq = sb.alloc(512, BF16)
            sd = sb.alloc(512, F32)
            o16 = [sb.alloc(512, BF16) for _ in range(2)]
            cnt = 0
            dcnt = 0
            ocnt = 0
            for h in range(NH):
                slope = 2.0 ** (-(h + 1))
                st_ = h % 2
                kx, vh, qx = sets[st_]
                vh3 = v3(vh, 64)
                semn = "set%d" % st_
                for b in range(2):
                    S.dma("sp", DMA(kx[b][0:64, :], KT[2 * h + b]), semn, writes=["Kr%d%d" % (st_, b)])
                    S.dma("pool", DMA(kx[b][64:68, :], kaug[h]), semn, writes=["Ka%d%d" % (st_, b)])
                    for sg in range(2):
                        S.dma("sp", DMA(qx[b][sg][0:64, :], QT[2 * h + b]), semn, writes=["Qr%d%d%d" % (st_, b, sg)])
                        S.dma("pool", DMA(qx[b][sg][64:68, :], qaug[h, sg]), semn, writes=["Qa%d%d%d" % (st_, b, sg)])
                for part in range(4):
                    S.dma("sp", DMA(vh3[:, part * 16:(part + 1) * 16, :],
                                    VV[part * 2048:(part + 1) * 2048, h * 128:(h + 1) * 128].rearrange("(t p) d -> p t d", p=128)),
                          semn, writes=["V%d_%d" % (st_, part)])
                vkeys = ["V%d_%d" % (st_, part) for part in range(4)]
                for qi in range(8):
                    for b in range(2):
                        par = (qi * 2 + b) % 2
                        Ob, Zb = 2 * par, 2 * par + 1
                        kkeys = ["Kr%d%d" % (st_, b), "Ka%d%d" % (st_, b)]
                        for kj in range(64):
                            sbk = 4 + cnt % 4
                            E = Eb[cnt % 4]
                            ek = "E%d" % (cnt % 4)
                            cnt += 1
                            diag = (kj < 32 and 4 * qi <= kj < 4 * qi + 4)
                            if diag:
                                sg, rows = 0, 64
                            else:
                                sg = 1 if (kj < 32 and kj < 4 * qi) else 0
                                rows = 68
                            qkeys = ["Qr%d%d%d" % (st_, b, sg), "Qa%d%d%d" % (st_, b, sg)]
                            S.op("pe", MM(bank(sbk), kx[b][0:rows, kj * 128:(kj + 1) * 128],
                                          qx[b][sg][0:rows, qi * 512:(qi + 1) * 512], True, True),
                                 reads=kkeys + qkeys, writes=["ps%d" % sbk])
                            if diag:
                                v = kj - 4 * qi
                                tm = tmpb[dcnt % 2]
                                tk = "tmp%d" % (dcnt % 2)
                                dcnt += 1
                                S.op("dve", STT(tm, T0[:, v * 512:(v + 1) * 512], slope, bank(sbk), ALU.mult, ALU.add),
                                     reads=["T0", "ps%d" % sbk], writes=[tk])
                                S.op("act", ACTF(E, tm, AF.Exp), reads=[tk], writes=[ek])
                            else:
                                S.op("act", ACTF(E, bank(sbk), AF.Exp), reads=["ps%d" % sbk], writes=[ek])
                            edge = (kj == 0 or kj == 63)
                            S.op("pe", MM(bank(Ob), vh3[:, kj, :], E, kj == 0, kj == 63),
                                 reads=[ek] + vkeys, writes=["ps%d" % Ob] if edge else ())
                            S.op("pe", MM(bank(Zb), ones_b, E, kj == 0, kj == 63),
                                 reads=[ek, "ones_b"], writes=["ps%d" % Zb] if edge else ())
                        S.op("dve", lambda e, Zb=Zb: e.reciprocal(rzt, bank(Zb)), reads=["ps%d" % Zb], writes=["rzt"])
                        S.op("dve", TT(ob[b], bank(Ob), rzt, ALU.mult), reads=["ps%d" % Ob, "rzt"], writes=["ob%d" % b])
                    S.op("dve", STT(df, ob[1], neglam[:, 0:1], ob[0], ALU.mult, ALU.add),
                         reads=["ob0", "ob1", "neglam"], writes=["df"])
                    S.op("act", ACTF(sq, df, AF.Square), reads=["df"], writes=["sq"])
                    sbk = 4 + cnt % 4
                    cnt += 1
                    S.op("pe", MM(bank(sbk), ones_b, sq, True, True), reads=["sq", "ones_b"], writes=["ps%d" % sbk])
                    S.op("act", ACTF(sd, bank(sbk), AF.Sqrt, bias=eps_t[:, 0:1], scale=1.0 / 128.0),
                         reads=["ps%d" % sbk, "eps_t"], writes=["sd"])
                    S.op("dve", lambda e: e.reciprocal(sd, sd), reads=["sd"], writes=["sd"])
                    S.op("dve", TT(df, df, sd, ALU.mult), reads=["df", "sd"], writes=["df"])
                    o_ = o16[ocnt % 2]
                    ok_ = "o16_%d" % (ocnt % 2)
                    ocnt += 1
                    S.op("dve", TS(o_, df, subs[:, 0:1], None, ALU.mult), reads=["df", "subs"], writes=[ok_])
                    S.dma("sp", DMA(OACT[h * 128:(h + 1) * 128, qi * 512:(qi + 1) * 512], o_), ok_, reads=[ok_])

        phase2()
        S.barrier()
        sb.reset()
        if stop_after <= 3:
            fin = S.dma("sp", DMA(out[0:128, 0:128], ident), "fin", reads=["ident"])
            S.emit(final_waits=[("sp", fin)])
            return nc

        def ln_rows(y, gb, bb, st4, junk, tag):
            S.op("dve", lambda e: e.reduce_sum(st4[:, 0:1], y, axis=AX.X), reads=[tag], writes=[tag + "s0"])
            S.op("dve", TS(st4[:, 1:2], st4[:, 0:1], -1.0 / D, None, ALU.mult), reads=[tag + "s0"], writes=[tag + "s1"])
            S.op("act", ACTF(junk, y, AF.Square, bias=st4[:, 1:2], accum=st4[:, 2:3]),
                 reads=[tag, tag + "s1"], writes=[tag + "s2", "junk"])
            S.op("dve", TS(st4[:, 3:4], st4[:, 2:3], 1.0 / D, EPS, ALU.mult, ALU.add), reads=[tag + "s2"], writes=[tag + "s3"])
            S.op("act", lambda e: e.sqrt(st4[:, 3:4], st4[:, 3:4]), reads=[tag + "s3"], writes=[tag + "s3"])
            S.op("dve", lambda e: e.reciprocal(st4[:, 3:4], st4[:, 3:4]), reads=[tag + "s3"], writes=[tag + "s3"])
            S.op("dve", TS(y, y, st4[:, 1:2], st4[:, 3:4], ALU.add, ALU.mult), reads=[tag, tag + "s1", tag + "s3"], writes=[tag])
            S.op("pool", TT(y, y, gb, ALU.mult), reads=[tag, "gb"], writes=[tag])
            S.op("pool", TT(y, y, bb, ALU.add), reads=[tag, "bb"], writes=[tag])

        def proj_ln(inT, Wd, resid, gi, post, extra_alloc=None):
            W = sb.alloc(16 * D, BF16)
            W3 = v3(W, 16)
            for g in range(4):
                S.dma("pool", DMA(W3[:, :, g * 512:(g + 1) * 512],
                                  Wd[:, g * 512:(g + 1) * 512].rearrange("(kc p) c -> p kc c", p=128)),
                      "wp", writes=["W%d" % g])
            wkeys = ["W%d" % g for g in range(4)]
            gb = sb.alloc(D, F32)
            bb = sb.alloc(D, F32)
            S.dma("sp", DMA(gb, lnp[gi].partition_broadcast(128)), "c0", writes=["gb"])
            S.dma("sp", DMA(bb, lnp[gi + 1].partition_broadcast(128)), "c1", writes=["bb"])
            ins = [sb.alloc(16 * 512, BF16) for _ in range(2)]
            xt = [sb.alloc(D, F32) for _ in range(2)]
            ys = [sb.alloc(D, F32) for _ in range(2)]
            st4 = [sb.alloc(4, F32) for _ in range(2)]
            junk = sb.alloc(D, BF16)
            ctx = extra_alloc() if extra_alloc else None
            for grp in range(8):
                i3 = v3(ins[grp % 2], 16)
                ik = "in%d" % (grp % 2)
                S.dma("sp", DMA(i3, inT[:, grp * 512:(grp + 1) * 512].rearrange("(kc p) t -> p kc t", p=128)), ik, writes=[ik])
                for s_ in range(4):
                    t = grp * 4 + s_
                    par = t % 2
                    x_, y_ = xt[par], ys[par]
                    S.dma("sp", DMA(x_, resid[t * 128:(t + 1) * 128, :]), "xt%d" % par, writes=["xt%d" % par])
                    for cg in range(4):
                        mm_group(S, bank(cg), "ps%d" % cg,
                                 [(i3[:, kc, s_ * 128:(s_ + 1) * 128], W3[:, kc, cg * 512:(cg + 1) * 512]) for kc in range(16)],
                                 [ik] + wkeys)
                    yk = "y%d" % par
                    for cg in range(4):
                        S.op("dve", STT(y_[:, cg * 512:(cg + 1) * 512], x_[:, cg * 512:(cg + 1) * 512], ALPHA, bank(cg),
                                        ALU.mult, ALU.add),
                             reads=["xt%d" % par, "ps%d" % cg], writes=[yk])
                    ln_rows(y_, gb, bb, st4[par], junk, yk)
                    post(ctx, grp, s_, t, y_, yk)

        def phase3():
            def extra():
                return {"x1b": [sb.alloc(D, BF16) for _ in range(2)],
                        "x1T": [sb.alloc(16 * 512, BF16) for _ in range(2)]}

            def post(ctx, grp, s_, t, y_, yk):
                par = t % 2
                S.dma("sp", DMA(X1[t * 128:(t + 1) * 128, :], y_), "x1o%d" % par, reads=[yk])
                xb_ = ctx["x1b"][par]
                S.op("act", lambda e: e.copy(xb_, y_), reads=[yk], writes=["x1b%d" % par])
                psb = ps[:, 2048:3072].bitcast(BF16)
                for kc in range(16):
                    S.op("pe", TR(psb[:, kc * 128:(kc + 1) * 128], xb_[:, kc * 128:(kc + 1) * 128], identb),
                         reads=["x1b%d" % par, "identb"], writes=["pst"] if kc in (0, 15) else ())
                xT3 = v3(ctx["x1T"][grp % 2], 16)
                tk = "x1T%d" % (grp % 2)
                S.op("act", lambda e: e.copy(xT3[:, :, s_ * 128:(s_ + 1) * 128], v3(psb, 16)),
                     reads=["pst"] + ([tk] if s_ > 0 else []), writes=[tk])
                if s_ == 3:
                    S.dma("sp", DMA(X1T[:, grp * 512:(grp + 1) * 512].rearrange("(kc p) t -> p kc t", p=128), xT3),
                          tk, reads=[tk])

            proj_ln(OACT, w_out, x_tok, 0, post, extra)

        phase3()
        S.barrier()
        sb.reset()
        if stop_after <= 4:
            fin = S.dma("sp", DMA(out[0:128, 0:128], ident), "fin", reads=["ident"])
            S.emit(final_waits=[("sp", fin)])
            return nc

        def phase4a():
            W = sb.alloc(16 * D, BF16)
            W3 = v3(W, 16)
            for g in range(4):
                S.dma("pool", DMA(W3[:, :, g * 512:(g + 1) * 512],
                                  mem_wq[:, g * 512:(g + 1) * 512].rearrange("(kc p) c -> p kc c", p=128)),
                      "wp", writes=["W%d" % g])
            wkeys = ["W%d" % g for g in range(4)]
            mk3 = v3(mk_sb, 16)
            mv3 = v3(mv_sb, 2)
            ins = [sb.alloc(16 * 512, BF16) for _ in range(2)]
            qT = sb.alloc(16 * 512, BF16)
            qT3 = v3(qT, 16)
            omT = [sb.alloc(16 * 512, BF16) for _ in range(2)]
            st4 = [sb.alloc(4, F32) for _ in range(2)]
            pe_ = [sb.alloc(256, F32) for _ in range(2)]
            pn = [sb.alloc(256, BF16) for _ in range(2)]
            pT = [sb.alloc(256, BF16) for _ in range(2)]
            scale = 512.0 ** -0.5
            psb = ps[:, 3072:3584].bitcast(BF16)
            it = 0
            for grp in range(8):
                i3 = v3(ins[grp % 2], 16)
                ik = "in%d" % (grp % 2)
                S.dma("sp", DMA(i3, X1T[:, grp * 512:(grp + 1) * 512].rearrange("(kc p) t -> p kc t", p=128)), ik, writes=[ik])
                for c in range(16):
                    b = c % 2
                    mm_group(S, bank(b), "ps%d" % b,
                             [(W3[:, kc, c * 128:(c + 1) * 128], i3[:, kc, :]) for kc in range(16)], [ik] + wkeys)
                    if c % 2 == 0:
                        S.op("act", lambda e, c=c, b=b: e.copy(qT3[:, c, :], bank(b)), reads=["ps%d" % b], writes=["qT%d" % c])
                    else:
                        S.op("dve", TC(qT3[:, c, :], bank(b)), reads=["ps%d" % b], writes=["qT%d" % c])
                o3 = v3(omT[grp % 2], 16)
                ok_ = "omT%d" % (grp % 2)
                for s_ in range(4):
                    for hd in range(4):
                        par = it % 2
                        it += 1
                        sbk = 2 + par
                        mm_group(S, bank(sbk, 256), "ps%d" % sbk,
                                 [(qT3[:, hd * 4 + c, s_ * 128:(s_ + 1) * 128], mk3[:, hd * 4 + c, :]) for c in range(4)],
                                 ["qT%d" % (hd * 4 + c) for c in range(4)])
                        s4 = st4[par]
                        sk = "s4_%d" % par
                        S.op("dve", lambda e, s4=s4, sbk=sbk: e.reduce_max(s4[:, 0:1], bank(sbk, 256), axis=AX.X),
                             reads=["ps%d" % sbk], writes=[sk + "a"])
                        S.op("dve", TS(s4[:, 1:2], s4[:, 0:1], -scale, None, ALU.mult), reads=[sk + "a"], writes=[sk + "b"])
                        S.op("act", ACTF(pe_[par], bank(sbk, 256), AF.Exp, bias=s4[:, 1:2], scale=scale, accum=s4[:, 2:3]),
                             reads=["ps%d" % sbk, sk + "b"], writes=["pe%d" % par, sk + "c"])
                        S.op("dve", lambda e, s4=s4: e.reciprocal(s4[:, 3:4], s4[:, 2:3]), reads=[sk + "c"], writes=[sk + "d"])
                        S.op("dve", TS(pn[par], pe_[par], s4[:, 3:4], None, ALU.mult), reads=["pe%d" % par, sk + "d"], writes=["pn%d" % par])
                        for mt in range(2):
                            S.op("pe", TR(psb[:, (par * 2 + mt) * 128:(par * 2 + mt + 1) * 128], pn[par][:, mt * 128:(mt + 1) * 128], identb),
                                 reads=["pn%d" % par, "identb"], writes=["pstb%d" % par] if mt in (0, 1) else ())
                        S.op("act", lambda e, par=par: e.copy(pT[par], psb[:, par * 256:(par + 1) * 256]),
                             reads=["pstb%d" % par], writes=["pT%d" % par])
                        obk = 4 + par
                        for dc in range(4):
                            for mt in range(2):
                                S.op("pe", MM(bank(obk, 128, dc * 128), mv3[:, mt, hd * 512 + dc * 128:hd * 512 + (dc + 1) * 128],
                                              pT[par][:, mt * 128:(mt + 1) * 128], mt == 0, mt == 1),
                                     reads=["pT%d" % par], writes=["ps%d" % obk] if (dc, mt) in ((0, 0), (3, 1)) else ())
                        S.op("dve", lambda e, o3=o3, hd=hd, s_=s_, obk=obk: e.tensor_copy(
                            o3[:, hd * 4:(hd + 1) * 4, s_ * 128:(s_ + 1) * 128], v3(bank(obk), 4)),
                             reads=["ps%d" % obk] + ([ok_] if (s_, hd) != (0, 0) else []), writes=[ok_])
                S.dma("sp", DMA(OM[:, grp * 512:(grp + 1) * 512].rearrange("(kc p) t -> p kc t", p=128), o3), ok_, reads=[ok_])

        phase4a()
        S.barrier()
        sb.reset()
        if stop_after <= 4.5:
            fin = S.dma("sp", DMA(out[0:128, 0:128], ident), "fin", reads=["ident"])
            S.emit(final_waits=[("sp", fin)])
            return nc

        def phase4b():
            def extra():
                c = {}
                c["x2b"] = [sb.alloc(D, BF16) for _ in range(2)]
                c["x2T"] = sb.alloc(16 * 128, F32)
                c["rw"] = sb.alloc(16 * NE, F32)
                c["rb"] = sb.alloc(NE, F32)
                c["ut"] = sb.alloc(128, F32)
                c["onesf"] = sb.alloc(128, F32)
                c["ec"] = sb.alloc(NE, F32)
                c["base"] = sb.alloc(NE, F32)
                for nm in ("lg", "mask", "ex", "G", "rowid", "keyv", "oh"):
                    c[nm] = sb.alloc(NE, F32)
                c["mx"] = sb.alloc(8, F32)
                c["kmx"] = sb.alloc(8, F32)
                c["sm"] = sb.alloc(4, F32)
                c["rows4"] = sb.alloc(4, F32)
                c["idx4"] = [sb.alloc(4, I32) for _ in range(2)]
                c["g4"] = [sb.alloc(4, F32) for _ in range(2)]
                S.dma("sp", DMA(v3(c["rw"], 16), router_w.rearrange("(kc p) e -> p kc e", p=128)), "c0", writes=["rw"])
                S.dma("sp", DMA(c["rb"], router_b.partition_broadcast(128)), "c1", writes=["rb"])
                S.dma("sp", DMA(c["ut"], utri), "c0", writes=["ut"])
                S.dma("sp", DMA(c["ec"], ecap), "c1", writes=["ec"])
                S.op("dve", MEMSET(c["onesf"], 1.0), writes=["onesf"])
                S.op("dve", MEMSET(c["base"], 0.0), writes=["base"])
                return c

            def post(c, grp, s_, t, y_, yk):
                par = t % 2
                S.dma("sp", DMA(X2[t * 128:(t + 1) * 128, :], y_), "x2o%d" % par, reads=[yk])
                xb_ = c["x2b"][par]
                xbk = "x2b%d" % par
                S.op("act", lambda e: e.copy(xb_, y_), reads=[yk], writes=[xbk])
                for kc in range(16):
                    S.op("pe", TR(ps[:, 2048 + kc * 128:2048 + (kc + 1) * 128], y_[:, kc * 128:(kc + 1) * 128], ident),
                         reads=[yk, "ident"], writes=["pst", "psp", "psc"] if kc in (0, 15) else ())
                x2T = c["x2T"]
                S.op("act", lambda e: e.copy(x2T[:, 0:1024], ps[:, 2048:3072]), reads=["pst"], writes=["x2Ta"])
                S.op("dve", TC(x2T[:, 1024:2048], ps[:, 3072:4096]), reads=["pst"], writes=["x2Tb"])
                rw3 = v3(c["rw"], 16)
                x2T3 = v3(x2T, 16)
                mm_group(S, ps[:, 2048:2048 + NE], "pst",
                         [(x2T3[:, kc, :], rw3[:, kc, :]) for kc in range(16)], ["x2Ta", "x2Tb", "rw"])
                lg, mask, ex, G, rowid, keyv, oh = (c[n] for n in ("lg", "mask", "ex", "G", "rowid", "keyv", "oh"))
                mx, kmx, sm, rows4 = c["mx"], c["kmx"], c["sm"], c["rows4"]
                S.op("dve", TT(lg, ps[:, 2048:2048 + NE], c["rb"], ALU.add), reads=["pst", "rb"], writes=["lg"])
                S.op("dve", lambda e: e.max(out=mx, in_=lg), reads=["lg"], writes=["mx"])
                S.op("dve", TS(mask, lg, mx[:, 3:4], None, ALU.is_ge), reads=["lg", "mx"], writes=["mask"])
                S.op("dve", TS(sm[:, 0:1], mx[:, 0:1], -1.0, None, ALU.mult), reads=["mx"], writes=["sm0"])
                S.op("act", ACTF(ex, lg, AF.Exp, bias=sm[:, 0:1]), reads=["lg", "sm0"], writes=["ex"])
                S.op("dve", TT(ex, ex, mask, ALU.mult), reads=["ex", "mask"], writes=["ex"])
                S.op("dve", lambda e: e.reduce_sum(sm[:, 1:2], ex, axis=AX.X), reads=["ex"], writes=["sm1"])
                S.op("dve", lambda e: e.reciprocal(sm[:, 1:2], sm[:, 1:2]), reads=["sm1"], writes=["sm1"])
                S.op("dve", TS(G, ex, sm[:, 1:2], None, ALU.mult), reads=["ex", "sm1"], writes=["G"])
                S.op("pe", MM(ps[:, 2560:2560 + NE], c["ut"], mask, True, True), reads=["ut", "mask"], writes=["psp"])
                S.op("pe", MM(ps[:, 2560 + NE:2560 + 2 * NE], c["onesf"], mask, True, True), reads=["onesf", "mask"], writes=["psc"])
                S.op("dve", TT(rowid, ps[:, 2560:2560 + NE], c["base"], ALU.add), reads=["psp", "base"], writes=["rowid"])
                S.op("dve", TT(c["base"], c["base"], ps[:, 2560 + NE:2560 + 2 * NE], ALU.add), reads=["psc", "base", "rowid"], writes=["base"])
                S.op("dve", TS(oh, rowid, float(CAP), None, ALU.is_lt), reads=["rowid"], writes=["oh"])
                S.op("dve", TT(oh, oh, mask, ALU.mult), reads=["oh", "mask"], writes=["oh"])
                S.op("dve", TT(rowid, rowid, c["ec"], ALU.add), reads=["rowid", "ec"], writes=["rowid"])
                S.op("dve", TS(keyv, rowid, -1.0, BIGROW, ALU.mult, ALU.add), reads=["rowid"], writes=["keyv"])
                S.op("dve", TT(keyv, keyv, oh, ALU.mult), reads=["keyv", "oh"], writes=["keyv"])
                S.op("dve", lambda e: e.max(out=kmx, in_=keyv), reads=["keyv"], writes=["kmx"])
                S.op("dve", TS(rows4, kmx[:, 0:4], -1.0, BIGROW, ALU.mult, ALU.add), reads=["kmx"], writes=["rows4"])
                i4 = c["idx4"][par]
                g4 = c["g4"][par]
                ik4 = "idx4_%d" % par
                gk4 = "g4_%d" % par
                S.op("dve", TC(i4, rows4), reads=["rows4"], writes=[ik4])
                for j in range(4):
                    S.op("dve", TS(oh, keyv, kmx[:, j:j + 1], None, ALU.is_equal), reads=["keyv", "kmx"], writes=["oh"])
                    S.op("dve", TT(oh, oh, G, ALU.mult), reads=["oh", "G"], writes=["oh"])
                    S.op("dve", lambda e, j=j: e.reduce_sum(g4[:, j:j + 1], oh, axis=AX.X), reads=["oh"], writes=[gk4 + "_%d" % j])
                for j in range(4):
                    S.dma("pool", lambda e, j=j: e.indirect_dma_start(
                        out=XG, out_offset=bass.IndirectOffsetOnAxis(ap=i4[:, j:j + 1], axis=0),
                        in_=xb_, in_offset=None, bounds_check=bcreg(e), oob_is_err=False),
                          "scat%d_%d" % (par, j), reads=[xbk, ik4])
                S.dma("sp", DMA(IDX4[t * 128:(t + 1) * 128, :], i4), "i4o%d" % par, reads=[ik4])
                S.dma("sp", DMA(G4[t * 128:(t + 1) * 128, :], g4), "g4o%d" % par, reads=[gk4 + "_%d" % j for j in range(4)])

            proj_ln(OM, mem_wo, X1, 2, post, extra)

        phase4b()
        S.barrier()
        sb.reset()
        if stop_after <= 5:
            fin = S.dma("sp", DMA(out[0:128, 0:128], ident), "fin", reads=["ident"])
            S.emit(final_waits=[("sp", fin)])
            return nc

        NST = CAP // 128
        R2 = CAP - 512

        def phase5():
            bg_sb = sb.alloc(2 * NE * 16, F32)
            S.dma("sp", DMA(bg_sb, bgu), "c0", writes=["bgu"])
            xgT = [sb.alloc(16 * CAP, BF16) for _ in range(2)]
            wt = [sb.alloc(16 * 512, BF16) for _ in range(4)]
            actT = sb.alloc(16 * CAP, BF16)
            a3 = v3(actT, 16)
            bd = [sb.alloc(D, F32) for _ in range(2)]
            xgt = [sb.alloc(D, BF16) for _ in range(2)]
            gs = [sb.alloc(CAP, F32) for _ in range(2)]
            sg = [sb.alloc(CAP, F32) for _ in range(2)]
            us = [sb.alloc(CAP, F32) for _ in range(2)]
            ysb = [sb.alloc(512, F32) for _ in range(4)]
            psb = ps[:, 3072:4096].bitcast(BF16)
            wi = 0
            xi = 0
            yi = 0
            fci = 0
            for e in range(NE):
                ep = e % 2
                S.dma("sp", DMA(bd[ep], b_down[e].partition_broadcast(128)), "bd%d" % ep, writes=["bd%d" % ep])
                x3 = v3(xgT[ep], 16)
                xk = "xgT%d" % ep
                for st_ in range(NST):
                    xt_ = xgt[xi % 2]
                    xtk = "xgt%d" % (xi % 2)
                    xi += 1
                    r0 = e * CAP + st_ * 128
                    S.dma("sp", DMA(xt_, XG[r0:r0 + 128, :]), xtk, writes=[xtk])
                    for kc in range(16):
                        S.op("pe", TR(psb[:, kc * 128:(kc + 1) * 128], xt_[:, kc * 128:(kc + 1) * 128], identb),
                             reads=[xtk, "identb"], writes=["ps6", "ps7"] if kc in (0, 15) else ())
                    eng = "act" if st_ % 2 == 0 else "dve"
                    S.op(eng, TC(x3[:, :, st_ * 128:(st_ + 1) * 128], v3(psb, 16)) if eng == "dve" else
                         (lambda e_, x3=x3, st_=st_: e_.copy(x3[:, :, st_ * 128:(st_ + 1) * 128], v3(psb, 16))),
                         reads=["ps6", "ps7"] + ([xk] if st_ > 0 else []), writes=[xk])
                for fg in range(4):
                    wg = wt[wi % 4]
                    wgk = "wt%d" % (wi % 4)
                    wi += 1
                    wu = wt[wi % 4]
                    wuk = "wt%d" % (wi % 4)
                    wi += 1
                    S.dma("pool", DMA(v3(wg, 16), w_gate[e][:, fg * 512:(fg + 1) * 512].rearrange("(kc p) c -> p kc c", p=128)),
                          wgk, writes=[wgk])
                    S.dma("pool", DMA(v3(wu, 16), w_up[e][:, fg * 512:(fg + 1) * 512].rearrange("(kc p) c -> p kc c", p=128)),
                          wuk, writes=[wuk])
                    wg3, wu3 = v3(wg, 16), v3(wu, 16)
                    for fs in range(4):
                        fc = fg * 4 + fs
                        set_ = fci % 2
                        fci += 1
                        g0, g1, u0, u1 = 4 * set_, 4 * set_ + 1, 4 * set_ + 2, 4 * set_ + 3
                        if set_ == 1:
                            g0, g1, u0, u1 = 4, 5, 6, 7
                        mm_group(S, bank(g0), "ps%d" % g0,
                                 [(wg3[:, kc, fs * 128:(fs + 1) * 128], x3[:, kc, 0:512]) for kc in range(16)], [wgk, xk])
                        mm_group(S, bank(g1, R2), "ps%d" % g1,
                                 [(wg3[:, kc, fs * 128:(fs + 1) * 128], x3[:, kc, 512:CAP]) for kc in range(16)], [wgk, xk])
                        mm_group(S, bank(u0), "ps%d" % u0,
                                 [(wu3[:, kc, fs * 128:(fs + 1) * 128], x3[:, kc, 0:512]) for kc in range(16)], [wuk, xk])
                        mm_group(S, bank(u1, R2), "ps%d" % u1,
                                 [(wu3[:, kc, fs * 128:(fs + 1) * 128], x3[:, kc, 512:CAP]) for kc in range(16)], [wuk, xk])
                        gs_, sg_, us_ = gs[set_], sg[set_], us[set_]
                        bgc = bg_sb[:, e * 16 + fc:e * 16 + fc + 1]
                        buc = bg_sb[:, NE * 16 + e * 16 + fc:NE * 16 + e * 16 + fc + 1]
                        S.op("dve", TS(gs_, ps[:, g0 * 512:g0 * 512 + CAP], bgc, 7.0, ALU.add, ALU.min),
                             reads=["ps%d" % g0, "ps%d" % g1, "bgu"], writes=["gs%d" % set_])
                        S.op("act", ACTF(sg_, gs_, AF.Sigmoid, scale=1.702), reads=["gs%d" % set_], writes=["sg%d" % set_])
                        S.op("dve", TS(us_, ps[:, u0 * 512:u0 * 512 + CAP], buc, 7.0, ALU.add, ALU.min),
                             reads=["ps%d" % u0, "ps%d" % u1, "bgu"], writes=["us%d" % set_])
                        S.op("pool", TS(us_, us_, -7.0, 1.0, ALU.max, ALU.add), reads=["us%d" % set_], writes=["us%d" % set_])
                        S.op("pool", TT(gs_, gs_, sg_, ALU.mult), reads=["gs%d" % set_, "sg%d" % set_], writes=["gs%d" % set_])
                        S.op("pool", TT(a3[:, fc, :], gs_, us_, ALU.mult), reads=["gs%d" % set_, "us%d" % set_], writes=["actT%d" % fc])
                akeys = ["actT%d" % fc for fc in range(16)]
                for dg in range(4):
                    wd = wt[wi % 4]
                    wdk = "wt%d" % (wi % 4)
                    wi += 1
                    S.dma("pool", DMA(v3(wd, 16), w_down[e][:, dg * 512:(dg + 1) * 512].rearrange("(fc p) c -> p fc c", p=128)),
                          wdk, writes=[wdk])
                    wd3 = v3(wd, 16)
                    for st_ in range(NST):
                        b = yi % 8
                        mm_group(S, bank(b), "ps%d" % b,
                                 [(a3[:, fc, st_ * 128:(st_ + 1) * 128], wd3[:, fc, :]) for fc in range(16)], [wdk] + akeys)
                        y_ = ysb[yi % 4]
                        yk = "ysb%d" % (yi % 4)
                        yi += 1
                        S.op("dve", TT(y_, bank(b), bd[ep][:, dg * 512:(dg + 1) * 512], ALU.add),
                             reads=["ps%d" % b, "bd%d" % ep], writes=[yk])
                        r0 = e * CAP + st_ * 128
                        S.dma("sp", DMA(YG[r0:r0 + 128, dg * 512:(dg + 1) * 512], y_), yk, reads=[yk])

        phase5()
        S.barrier()
        sb.reset()

        def phase6():
            gb = sb.alloc(D, F32)
            bb = sb.alloc(D, F32)
            S.dma("sp", DMA(gb, lnp[4].partition_broadcast(128)), "c0", writes=["gb"])
            S.dma("sp", DMA(bb, lnp[5].partition_broadcast(128)), "c1", writes=["bb"])
            yj = [sb.alloc(4 * D, F32) for _ in range(2)]
            x2t = [sb.alloc(D, F32) for _ in range(2)]
            ys = [sb.alloc(D, F32) for _ in range(2)]
            i4s = [sb.alloc(4, I32) for _ in range(2)]
            g4s = [sb.alloc(4, F32) for _ in range(2)]
            st4 = [sb.alloc(4, F32) for _ in range(2)]
            junk = sb.alloc(D, BF16)
            last = {}
            for t in range(TOK // 128):
                par = t % 2
                i4, g4, x_, y_, yy = i4s[par], g4s[par], x2t[par], ys[par], yj[par]
                S.dma("sp", DMA(i4, IDX4[t * 128:(t + 1) * 128, :]), "i4l%d" % par, writes=["i4_%d" % par])
                S.dma("sp", DMA(g4, G4[t * 128:(t + 1) * 128, :]), "g4l%d" % par, writes=["g4_%d" % par])
                S.dma("sp", DMA(x_, X2[t * 128:(t + 1) * 128, :]), "x2l%d" % par, writes=["x2_%d" % par])
                S.op("pool", MEMSET(yy, 0.0), writes=["yj%d_%d" % (par, j) for j in range(4)])
                for j in range(4):
                    S.dma("pool", lambda e, j=j, yy=yy, i4=i4: e.indirect_dma_start(
                        out=yy[:, j * D:(j + 1) * D], out_offset=None, in_=YG,
                        in_offset=bass.IndirectOffsetOnAxis(ap=i4[:, j:j + 1], axis=0),
                        bounds_check=bcreg(e), oob_is_err=False),
                          "gat%d_%d" % (par, j), reads=["i4_%d" % par], writes=["yj%d_%d" % (par, j)])
                yk = "y%d" % par
                S.op("act", ACTF(y_, x_, AF.Copy, scale=ALPHA), reads=["x2_%d" % par], writes=[yk])
                for j in range(4):
                    S.op("dve", STT(y_, yy[:, j * D:(j + 1) * D], g4[:, j:j + 1], y_, ALU.mult, ALU.add),
                         reads=["yj%d_%d" % (par, j), "g4_%d" % par, yk], writes=[yk])
                ln_rows(y_, gb, bb, st4[par], junk, yk)
                last[par] = S.dma("sp", DMA(out[t * 128:(t + 1) * 128, :], y_), "outo%d" % par, reads=[yk])
            return list(last.values())

        finals = phase6()
        S.emit(final_waits=[("sp", d) for d in finals])
    return nc


def _host_inputs(inp, ncores=8):
    x = np.asarray(inp["x"], np.float32)
    mem = np.asarray(inp["mem"], np.float32)
    common = {}
    common["w_in"] = np.ascontiguousarray(inp["w_in"][0])
    cw = np.asarray(inp["conv_w"][0], np.float32)
    common["convw"] = np.ascontiguousarray(cw.reshape(3, 8, 128).transpose(2, 1, 0).reshape(128, 24))
    common["subln"] = np.ascontiguousarray(np.asarray(inp["attn_subln_w"][0], np.float32).reshape(128, 1))
    common["lamv"] = np.ascontiguousarray(np.stack([inp["lambda_q1"][0], inp["lambda_k1"][0],
                                                    inp["lambda_q2"][0], inp["lambda_k2"][0]]).astype(np.float32))
    common["w_out"] = np.ascontiguousarray(inp["w_out"][0])
    common["lnp"] = np.ascontiguousarray(np.stack([inp["ln1_g"][0], inp["ln1_b"][0], inp["ln2_g"][0],
                                                   inp["ln2_b"][0], inp["ln3_g"][0], inp["ln3_b"][0]]).astype(np.float32))
    common["mem_wq"] = np.ascontiguousarray(inp["mem_wq"][0])
    common["mem_wkv"] = np.ascontiguousarray(inp["mem_wkv"][0])
    common["mem_wo"] = np.ascontiguousarray(inp["mem_wo"][0])
    common["router_w"] = np.ascontiguousarray(inp["router_w"][0])
    common["router_b"] = np.ascontiguousarray(inp["router_b"][0])
    common["w_gate"] = np.ascontiguousarray(inp["w_gate"][0])
    common["w_up"] = np.ascontiguousarray(inp["w_up"][0])
    common["w_down"] = np.ascontiguousarray(inp["w_down"][0])
    bg = np.asarray(inp["b_gate"][0], np.float32).reshape(NE, 16, 128).transpose(2, 0, 1).reshape(128, NE * 16)
    bu = np.asarray(inp["b_up"][0], np.float32).reshape(NE, 16, 128).transpose(2, 0, 1).reshape(128, NE * 16)
    common["bgu"] = np.ascontiguousarray(np.concatenate([bg, bu], axis=1))
    common["b_down"] = np.ascontiguousarray(inp["b_down"][0])
    kr = np.arange(128)[:, None, None]
    vv = np.arange(4)[None, :, None]
    qr = np.arange(512)[None, None, :]
    common["tdiag"] = np.ascontiguousarray((-np.abs(vv * 128 + kr - qr)).astype(np.float32).reshape(128, 2048))
    common["ident"] = np.eye(128, dtype=np.float32)
    common["ecap"] = np.ascontiguousarray(np.broadcast_to((np.arange(NE) * CAP).astype(np.float32)[None, :], (128, NE)))
    common["utri"] = np.triu(np.ones((128, 128), np.float32), k=1)
    slopes = 2.0 ** (-np.arange(1, NH + 1, dtype=np.float64))
    maps = []
    for c in range(ncores):
        b, half = c // 2, c % 2
        own = slice(half * TOK, (half + 1) * TOK)
        oth = slice((1 - half) * TOK, (2 - half) * TOK)
        xb = x[b]
        m = dict(common)
        m["xT_kv"] = np.ascontiguousarray(np.concatenate([xb[own], xb[oth]], axis=0).T)
        xo = np.zeros((TOK + 2, D), np.float32)
        xo[1:TOK + 1] = xb[own]
        if half == 1:
            xo[0] = xb[TOK - 1]
        else:
            xo[TOK + 1] = xb[TOK]
        m["xT_own"] = np.ascontiguousarray(xo.T)
        m["x_tok"] = np.ascontiguousarray(xb[own])
        m["memT"] = np.ascontiguousarray(mem[b].T)
        qpos = np.arange(half * TOK, (half + 1) * TOK)
        kpos = np.concatenate([np.arange(half * TOK, (half + 1) * TOK), np.arange((1 - half) * TOK, (2 - half) * TOK)])
        sig_other = 1.0 if half == 0 else -1.0
        ka = np.zeros((NH, 4, SEQ), np.float32)
        qa = np.zeros((NH, 2, 4, TOK), np.float32)
        for h in range(NH):
            s = slopes[h]
            ksig = np.ones(SEQ)
            ksig[TOK:] = sig_other
            ka[h, 0] = ksig
            ka[h, 1] = ksig
            ka[h, 2] = -64.0 * s * (kpos // 64) * ksig
            ka[h, 3] = -s * (kpos % 64) * ksig
            plus = np.stack([64.0 * s * (qpos // 64), s * (qpos % 64), np.ones(TOK), np.ones(TOK)])
            qa[h, 0] = plus
            qa[h, 1] = -plus
        m["kaug"] = ka
        m["qaug"] = qa
        maps.append(m)
    return maps


_CACHE = {}


def kernel(**inputs):
    stop_after = float(os.environ.get("MK_STOP", "99"))
    dbg = os.environ.get("MK_DBG", "0") == "1"
    key = (stop_after, dbg)
    if key not in _CACHE:
        _CACHE[key] = build_program(stop_after, dbg)
    nc = _CACHE[key]
    ncores = int(os.environ.get("MK_CORES", "8"))
    maps = _host_inputs(inputs, ncores)
    res = run_bass_kernel_spmd(nc, maps, core_ids=list(range(ncores)))
    if dbg:
        return res
    outp = np.empty((NB, SEQ, D), np.float32)
    for c in range(ncores):
        b, half = c // 2, c % 2
        outp[b, half * TOK:(half + 1) * TOK] = res.results[c]["out"]
    return outp
```

```python
import math
import os
from contextlib import ExitStack

import numpy as np

import concourse.bass as bass
import concourse.mybir as mybir
from concourse.bass_utils import run_bass_kernel_spmd

F32 = mybir.dt.float32
BF16 = mybir.dt.bfloat16
I32 = mybir.dt.int32
AF = mybir.ActivationFunctionType
ALU = mybir.AluOpType
AX = mybir.AxisListType

D = 2048
SEQ = 8192
NB = 4
TOK = 4096
NH = 8
NE = 32
CAP = 768
NROWS = NE * CAP
ALPHA = 2.0 ** 0.25
LAMBDA_INIT = 0.8 - 0.6 * math.exp(0.0)
EPS = 1e-5
BIGROW = 1.0e6


class _Op:
    __slots__ = ("eng", "fn", "deps", "marked", "sem", "inc", "sigval", "is_dma", "gidx")

    def __init__(self, eng, fn):
        self.eng = eng
        self.fn = fn
        self.deps = []
        self.marked = False
        self.sem = None
        self.inc = 1
        self.sigval = None
        self.is_dma = False
        self.gidx = 0


class Sched:
    ENGS = ("pe", "act", "dve", "pool", "sp")

    def __init__(self, nc, stack):
        self.nc = nc
        self.stack = stack
        self.ops = {e: [] for e in self.ENGS}
        self.buf = {}
        self.sems = {}
        self.dma_count = {}
        self.dma_hist = {}
        self.gcount = 0
        self.last_dma = {}
        self.regs = {}
        self.pool_prev = {}

    def sem(self, name):
        if name not in self.sems:
            self.sems[name] = self.stack.enter_context(self.nc.semaphore(name))
        return self.sems[name]

    def _track(self, op, reads, writes):
        deps = []
        for k in reads:
            st = self.buf.get(k)
            if st is None:
                st = self.buf[k] = [None, []]
            if st[0] is not None:
                deps.append(st[0])
            st[1].append(op)
        for k in writes:
            st = self.buf.get(k)
            if st is None:
                st = self.buf[k] = [None, []]
            if st[0] is not None:
                deps.append(st[0])
            deps.extend(st[1])
            self.buf[k] = [op, []]
        seen = set()
        for d in deps:
            if d is op or id(d) in seen:
                continue
            seen.add(id(d))
            if d.eng == "pe" and op.eng == "pe" and not d.is_dma and not op.is_dma:
                continue
            d.marked = True
            op.deps.append(d)

    def op(self, eng, fn, reads=(), writes=()):
        o = _Op(eng, fn)
        self.gcount += 1
        o.gidx = self.gcount
        o.sem = "c_" + eng
        self._track(o, reads, writes)
        self.ops[eng].append(o)
        return o

    def dma(self, eng, fn, sem, reads=(), writes=()):
        o = _Op(eng, fn)
        self.gcount += 1
        o.gidx = self.gcount
        o.is_dma = True
        if eng == "pool":
            self.prot = (getattr(self, "prot", -1) + 1) % 16
            o.sem = "d_pq%d" % self.prot
            prev = self.last_dma.get(o.sem) or self.pool_prev.get(o.sem)
            if prev is not None:
                o.deps.append(prev)
            self.pool_prev[o.sem] = o
        else:
            o.sem = "d_" + sem
        o.inc = 16
        o.marked = True
        c = self.dma_count.get(o.sem, 0) + 16
        self.dma_count[o.sem] = c
        o.sigval = c
        self.dma_hist.setdefault(o.sem, []).append((o.gidx, c))
        self.last_dma[o.sem] = o
        self._track(o, reads, writes)
        self.ops[eng].append(o)
        return o

    def barrier(self):
        lasts = []
        for e in self.ENGS:
            for o in reversed(self.ops[e]):
                if not o.is_dma and o.fn is not None:
                    lasts.append(o)
                    break
        lasts.extend(self.last_dma.values())
        self.last_dma = {}
        for o in lasts:
            o.marked = True
        for e in self.ENGS:
            b = _Op(e, None)
            self.gcount += 1
            b.gidx = self.gcount
            b.sem = "c_" + e
            b.deps = [o for o in lasts]
            self.ops[e].append(b)
        self.buf = {}

    def emit(self, final_waits=()):
        nc = self.nc
        cnt = {e: 0 for e in self.ENGS}
        for e in self.ENGS:
            for o in self.ops[e]:
                if o.is_dma or o.fn is None:
                    continue
                if o.marked:
                    cnt[e] += 1
                    o.sigval = cnt[e]
        for name in sorted({o.sem for e in self.ENGS for o in self.ops[e] if o.marked}):
            self.sem(name)
        fw = {}
        for (ename, o) in final_waits:
            fw.setdefault(ename, []).append((o.sem, o.sigval))

        def run(ename, eng):
            waited = {}
            for o in self.ops[ename]:
                need = {}
                for d in o.deps:
                    v = d.sigval
                    if d.is_dma:
                        for (g, c) in self.dma_hist[d.sem]:
                            if g < o.gidx and c > v:
                                v = c
                    if need.get(d.sem, 0) < v:
                        need[d.sem] = v
                for sname, v in need.items():
                    if waited.get(sname, 0) >= v:
                        continue
                    if sname == "c_" + ename and ename == "pe":
                        continue
                    waited[sname] = v
                    eng.wait_ge(self.sems[sname], v)
                if o.fn is None:
                    continue
                ins = o.fn(eng)
                if o.marked:
                    ins.then_inc(self.sems[o.sem], o.inc)
            for (sname, v) in fw.get(ename, ()):
                eng.wait_ge(self.sems[sname], v)

        with nc.Block() as block:
            @block.tensor
            def _(eng):
                run("pe", eng)

            @block.scalar
            def _(eng):
                run("act", eng)

            @block.vector
            def _(eng):
                run("dve", eng)

            @block.gpsimd
            def _(eng):
                run("pool", eng)

            @block.sync
            def _(eng):
                run("sp", eng)


def MM(out, lhsT, rhs, start, stop):
    return lambda e: e.matmul(out, lhsT, rhs, start=start, stop=stop)


def TR(out, in_, ident):
    return lambda e: e.transpose(out, in_, ident)


def DMA(out, in_):
    return lambda e: e.dma_start(out=out, in_=in_)


def ACTF(out, in_, func, bias=None, scale=1.0, accum=None):
    def f(e):
        kw = {}
        if bias is not None:
            kw["bias"] = bias
        if accum is not None:
            kw["accum_out"] = accum
        return e.activation(out, in_, func, scale=scale, **kw)
    return f


def TC(out, in_):
    return lambda e: e.tensor_copy(out, in_)


def TS(out, in0, s1, s2, op0, op1=None):
    if op1 is None:
        return lambda e: e.tensor_scalar(out, in0, s1, None, op0=op0)
    return lambda e: e.tensor_scalar(out, in0, s1, s2, op0=op0, op1=op1)


def TT(out, in0, in1, op):
    return lambda e: e.tensor_tensor(out, in0, in1, op=op)


def STT(out, in0, scalar, in1, op0, op1):
    return lambda e: e.scalar_tensor_tensor(out, in0, scalar, in1, op0=op0, op1=op1)


def MEMSET(ap, v):
    return lambda e: e.memset(ap, v)

def mm_group(S, out_ap, pskey, pairs, reads):
    n = len(pairs)
    for i, (l, r) in enumerate(pairs):
        edge = (i == 0 or i == n - 1)
        S.op("pe", MM(out_ap, l, r, i == 0, i == n - 1),
             reads=reads if edge else (), writes=[pskey] if edge else ())


class SB:
    def __init__(self, big, nwords):
        self.big = big
        self.n = nwords
        self.off = 0
        self.base = 0

    def alloc(self, cols, dtype):
        size = 4 if dtype in (F32, I32) else 2
        words = (cols * size + 3) // 4
        assert self.off + words <= self.n, ("SBUF overflow", self.off, words, self.n)
        ap = self.big[:, self.off:self.off + words]
        self.off += words
        if dtype != F32:
            ap = ap.bitcast(dtype)
        return ap

    def persist(self):
        self.base = self.off

    def reset(self):
        self.off = self.base


def v3(ap, a):
    return ap.rearrange("p (a b) -> p a b", a=a)


def build_program(stop_after=99, dbg=False):
    nc = bass.Bass("TRN2", target_bir_lowering=False)

    def din(name, shape, dt=F32):
        return nc.dram_tensor(name, list(shape), dt, kind="ExternalInput").ap()

    def dscr(name, shape, dt, out=False):
        return nc.dram_tensor(name, list(shape), dt, kind="ExternalOutput" if out else "Internal").ap()

    xT_kv = din("xT_kv", [D, SEQ])
    xT_own = din("xT_own", [D, TOK + 2])
    x_tok = din("x_tok", [TOK, D])
    memT = din("memT", [D, 256])
    w_in = din("w_in", [D, 6144])
    convw = din("convw", [128, 24])
    subln = din("subln", [128, 1])
    lamv = din("lamv", [4, 64])
    w_out = din("w_out", [D, D])
    lnp = din("lnp", [6, D])
    mem_wq = din("mem_wq", [D, D])
    mem_wkv = din("mem_wkv", [D, 2 * D])
    mem_wo = din("mem_wo", [D, D])
    router_w = din("router_w", [D, NE])
    router_b = din("router_b", [NE])
    if stop_after >= 6:
        w_gate = din("w_gate", [NE, D, D])
        w_up = din("w_up", [NE, D, D])
        w_down = din("w_down", [NE, D, D])
    bgu = din("bgu", [128, 2 * NE * 16])
    b_down = din("b_down", [NE, D])
    kaug = din("kaug", [NH, 4, SEQ])
    qaug = din("qaug", [NH, 2, 4, TOK])
    tdiag = din("tdiag", [128, 4 * 512])
    ident_in = din("ident", [128, 128])
    ecap = din("ecap", [128, NE])
    utri = din("utri", [128, 128])

    out = dscr("out", [TOK, D], F32, out=True)
    KT = dscr("KT", [16, 64, SEQ], BF16, out=(dbg and stop_after == 1))
    VV = dscr("VV", [SEQ, 1024], BF16, out=(dbg and stop_after == 1))
    QT = dscr("QT", [16, 64, TOK], BF16, out=(dbg and stop_after == 2))
    OACT = dscr("OACT", [D, TOK], BF16, out=(dbg and stop_after in (2, 3)))
    X1 = dscr("X1", [TOK, D], F32, out=(dbg and stop_after == 4))
    X1T = dscr("X1T", [D, TOK], BF16)
    OM = dscr("OM", [D, TOK], BF16)
    X2 = dscr("X2", [TOK, D], F32, out=(dbg and stop_after == 5))
    XG = dscr("XG", [NROWS, D], BF16)
    YG = dscr("YG", [NROWS, D], F32)
    IDX4 = dscr("IDX4", [TOK, 4], I32, out=(dbg and stop_after == 5))
    G4 = dscr("G4", [TOK, 4], F32, out=(dbg and stop_after == 5))

    with ExitStack() as st:
        S = Sched(nc, st)
        NW = 53200
        big = st.enter_context(nc.sbuf_tensor("big", [128, NW], F32))
        sb = SB(big, NW)
        ps = st.enter_context(nc.psum_tensor("ps", [128, 4096], F32))

        def bank(i, n=512, off=0):
            return ps[:, i * 512 + off:i * 512 + off + n]

        ident = sb.alloc(128, F32)
        identb = sb.alloc(128, BF16)
        ones_b = sb.alloc(128, BF16)
        lam = sb.alloc(1, F32)
        mk_sb = sb.alloc(16 * 256, BF16)
        mv_sb = sb.alloc(2 * 2048, BF16)
        sb.persist()

        S.dma("sp", DMA(ident, ident_in), "c0", writes=["ident"])
        S.op("dve", TC(identb, ident), reads=["ident"], writes=["identb"])
        S.op("dve", MEMSET(ones_b, 1.0), writes=["ones_b"])

        def bcreg(e):
            if "bc" not in S.regs:
                S.regs["bc"] = e.to_reg(NROWS - 1)
            return S.regs["bc"]


        def phase0():
            lv = sb.alloc(4 * 64, F32)
            pr = sb.alloc(2 * 64, F32)
            sm = sb.alloc(2, F32)
            S.dma("sp", DMA(lv, lamv.rearrange("a b -> (a b)").partition_broadcast(128)), "c1", writes=["lv"])
            S.op("dve", TT(pr[:, 0:64], lv[:, 0:64], lv[:, 64:128], ALU.mult), reads=["lv"], writes=["pr0"])
            S.op("dve", TT(pr[:, 64:128], lv[:, 128:192], lv[:, 192:256], ALU.mult), reads=["lv"], writes=["pr1"])
            S.op("dve", lambda e: e.reduce_sum(sm[:, 0:1], pr[:, 0:64], axis=AX.X), reads=["pr0"], writes=["sm0"])
            S.op("dve", lambda e: e.reduce_sum(sm[:, 1:2], pr[:, 64:128], axis=AX.X), reads=["pr1"], writes=["sm1"])
            S.op("act", ACTF(sm, sm, AF.Exp), reads=["sm0", "sm1"], writes=["sme"])
            S.op("dve", TT(lam, sm[:, 0:1], sm[:, 1:2], ALU.subtract), reads=["sme"], writes=["lam0"])
            S.op("dve", TS(lam, lam, LAMBDA_INIT, None, ALU.add), reads=["lam0"], writes=["lam"])
            mT = sb.alloc(16 * 256, BF16)
            mT3 = v3(mT, 16)
            S.dma("pool", DMA(mT3, memT.rearrange("(kc p) m -> p kc m", p=128)), "mT", writes=["mT"])
            wg = [sb.alloc(16 * 512, BF16) for _ in range(2)]
            mk3 = v3(mk_sb, 16)
            mv3 = v3(mv_sb, 2)
            for g in range(8):
                w = wg[g % 2]
                w3 = v3(w, 16)
                wk = "wg%d" % (g % 2)
                S.dma("pool", DMA(w3, mem_wkv[:, g * 512:(g + 1) * 512].rearrange("(kc p) c -> p kc c", p=128)),
                      wk, writes=[wk])
                if g < 4:
                    for s_ in range(4):
                        b = (g * 4 + s_) % 4
                        mm_group(S, bank(b, 256), "ps%d" % b,
                                 [(w3[:, kc, s_ * 128:(s_ + 1) * 128], mT3[:, kc, :]) for kc in range(16)], [wk, "mT"])
                        last = S.op("act", lambda e, b=b, c=g * 4 + s_: e.copy(mk3[:, c, :], bank(b, 256)),
                                    reads=["ps%d" % b], writes=["mk%d" % (g * 4 + s_)])
                else:
                    for m in range(2):
                        b = 4 + (g * 2 + m) % 4
                        mm_group(S, bank(b), "ps%d" % b,
                                 [(mT3[:, kc, m * 128:(m + 1) * 128], w3[:, kc, :]) for kc in range(16)], [wk, "mT"])
                        S.op("dve", TC(mv3[:, m, (g - 4) * 512:(g - 3) * 512], bank(b)),
                             reads=["ps%d" % b], writes=["mv%d_%d" % (m, g)])

        phase0()
        S.barrier()
        sb.reset()

        def phase1a():
            wk_sb = sb.alloc(16 * 1024, BF16)
            wv_sb = sb.alloc(16 * 1024, BF16)
            wk3 = v3(wk_sb, 16)
            wv3 = v3(wv_sb, 16)
            for h in range(2):
                S.dma("pool", DMA(wk3[:, :, h * 512:(h + 1) * 512],
                                  w_in[:, 1024 + h * 512:1024 + (h + 1) * 512].rearrange("(kc p) c -> p kc c", p=128)),
                      "wk", writes=["wk%d" % h])
                S.dma("pool", DMA(wv3[:, :, h * 512:(h + 1) * 512],
                                  w_in[:, 2048 + h * 512:2048 + (h + 1) * 512].rearrange("(kc p) c -> p kc c", p=128)),
                      "wv", writes=["wv%d" % h])
            xs = [sb.alloc(16 * 512, BF16) for _ in range(2)]
            kst = [sb.alloc(512, BF16) for _ in range(4)]
            vst = [sb.alloc(1024, BF16) for _ in range(2)]
            nt = SEQ // 512
            ki = 0
            vi = 0
            for t in range(nt):
                x = xs[t % 2]
                x3 = v3(x, 16)
                xk = "x%d" % (t % 2)
                S.dma("pool", DMA(x3, xT_kv[:, t * 512:(t + 1) * 512].rearrange("(kc p) c -> p kc c", p=128)),
                      xk, writes=[xk])
                for c in range(8):
                    b = c % 4
                    mm_group(S, bank(b), "ps%d" % b,
                             [(wk3[:, kc, c * 128:(c + 1) * 128], x3[:, kc, :]) for kc in range(16)], [xk, "wk0", "wk1"])
                    ks = kst[ki % 4]
                    kk = "kst%d" % (ki % 4)
                    ki += 1
                    S.op("act", lambda e, ks=ks, b=b: e.copy(ks, bank(b)), reads=["ps%d" % b], writes=[kk])
                    for hh in range(2):
                        S.dma("sp", DMA(KT[2 * c + hh, :, t * 512:(t + 1) * 512], ks[hh * 64:(hh + 1) * 64, :]),
                              kk, reads=[kk])
                for s_ in range(4):
                    vs = vst[vi % 2]
                    vk = "vst%d" % (vi % 2)
                    vi += 1
                    for g in range(2):
                        b = 4 + (s_ * 2 + g) % 4
                        mm_group(S, bank(b), "ps%d" % b,
                                 [(x3[:, kc, s_ * 128:(s_ + 1) * 128], wv3[:, kc, g * 512:(g + 1) * 512]) for kc in range(16)],
                                 [xk, "wv0", "wv1"])
                        S.op("dve", TC(vs[:, g * 512:(g + 1) * 512], bank(b)), reads=["ps%d" % b], writes=[vk + "_%d" % g])
                    S.dma("sp", DMA(VV[t * 512 + s_ * 128:t * 512 + (s_ + 1) * 128, :], vs),
                          vk, reads=[vk + "_0", vk + "_1"])

        phase1a()
        S.barrier()
        sb.reset()
        if stop_after <= 1:
            fin = S.dma("sp", DMA(out[0:128, 0:128], ident), "fin", reads=["ident"])
            S.emit(final_waits=[("sp", fin)])
            return nc

        def phase1b():
            wq_sb = sb.alloc(16 * 4096, BF16)
            w3 = v3(wq_sb, 16)
            for g in range(8):
                src = g * 512 if g < 2 else 3072 + (g - 2) * 512
                S.dma("pool", DMA(w3[:, :, g * 512:(g + 1) * 512],
                                  w_in[:, src:src + 512].rearrange("(kc p) c -> p kc c", p=128)),
                      "w1b", writes=["w1b_%d" % g])
            wkeys = ["w1b_%d" % g for g in range(8)]
            cw = sb.alloc(24, F32)
            S.dma("sp", DMA(cw, convw), "c1", writes=["cw"])
            xs = [sb.alloc(16 * 512, BF16) for _ in range(2)]
            qst = [sb.alloc(512, BF16) for _ in range(2)]
            csb = [sb.alloc(512, F32) for _ in range(2)]
            cu = [sb.alloc(512, F32) for _ in range(2)]
            acc = [sb.alloc(512, F32) for _ in range(2)]
            ocv = [sb.alloc(512, BF16) for _ in range(2)]
            qi_ = 0
            ci_ = 0
            for j in range(9):
                h0 = 510 * j
                w = min(512, TOK + 2 - h0)
                x = xs[j % 2]
                x3 = v3(x, 16)
                xk = "x%d" % (j % 2)
                S.dma("pool", DMA(x3[:, :, 0:w], xT_own[:, h0:h0 + w].rearrange("(kc p) c -> p kc c", p=128)),
                      xk, writes=[xk])
                for c in range(8):
                    b = c % 2
                    mm_group(S, bank(b, w), "ps%d" % b,
                             [(w3[:, kc, c * 128:(c + 1) * 128], x3[:, kc, 0:w]) for kc in range(16)], [xk] + wkeys)
                    qs = qst[qi_ % 2]
                    qk = "qst%d" % (qi_ % 2)
                    qi_ += 1
                    S.op("act", ACTF(qs[:, 0:w], bank(b, w), AF.Copy, scale=0.125), reads=["ps%d" % b], writes=[qk])
                    for hh in range(2):
                        S.dma("sp", DMA(QT[2 * c + hh, :, h0:h0 + w - 2], qs[hh * 64:(hh + 1) * 64, 1:w - 1]),
                              qk, reads=[qk])
                for cc in range(8):
                    par = ci_ % 2
                    ci_ += 1
                    bB, bC, bU = 2 + 3 * par, 3 + 3 * par, 4 + 3 * par
                    for (bb, off) in ((bB, 1024), (bC, 2048), (bU, 3072)):
                        mm_group(S, bank(bb, w), "ps%d" % bb,
                                 [(w3[:, kc, off + cc * 128:off + (cc + 1) * 128], x3[:, kc, 0:w]) for kc in range(16)],
                                 [xk] + wkeys)
                    cs, cu_, ac, oc = csb[par], cu[par], acc[par], ocv[par]
                    S.op("act", lambda e, cs=cs, bC=bC, w=w: e.copy(cs[:, 0:w], bank(bC, w)),
                         reads=["ps%d" % bC], writes=["cs%d" % par])
                    S.op("dve", TT(cu_[:, 0:w], cs[:, 0:w], bank(bU, w), ALU.mult),
                         reads=["cs%d" % par, "ps%d" % bU], writes=["cu%d" % par])
                    S.op("dve", TS(ac[:, 0:w - 2], cu_[:, 1:w - 1], cw[:, cc * 3 + 1:cc * 3 + 2], None, ALU.mult),
                         reads=["cu%d" % par, "cw"], writes=["ac%d" % par])
                    S.op("dve", STT(ac[:, 0:w - 2], cu_[:, 0:w - 2], cw[:, cc * 3:cc * 3 + 1], ac[:, 0:w - 2], ALU.mult, ALU.add),
                         reads=["cu%d" % par, "cw", "ac%d" % par], writes=["ac%d" % par])
                    S.op("dve", STT(ac[:, 0:w - 2], cu_[:, 2:w], cw[:, cc * 3 + 2:cc * 3 + 3], ac[:, 0:w - 2], ALU.mult, ALU.add),
                         reads=["cu%d" % par, "cw", "ac%d" % par], writes=["ac%d" % par])
                    S.op("dve", TT(oc[:, 0:w - 2], ac[:, 0:w - 2], bank(bB, w)[:, 1:w - 1], ALU.mult),
                         reads=["ac%d" % par, "ps%d" % bB], writes=["oc%d" % par])
                    S.dma("sp", DMA(OACT[1024 + cc * 128:1024 + (cc + 1) * 128, h0:h0 + w - 2], oc[:, 0:w - 2]),
                          "oc%d" % par, reads=["oc%d" % par])

        phase1b()
        S.barrier()
        sb.reset()
        if stop_after <= 2:
            fin = S.dma("sp", DMA(out[0:128, 0:128], ident), "fin", reads=["ident"])
            S.emit(final_waits=[("sp", fin)])
            return nc

        def phase2():
            T0 = sb.alloc(2048, F32)
            S.dma("sp", DMA(T0, tdiag), "c1", writes=["T0"])
            sl_t = sb.alloc(1, F32)
            S.dma("sp", DMA(sl_t, subln), "c0", writes=["sl_raw"])
            subs = sb.alloc(1, F32)
            S.op("dve", TS(subs, sl_t, 1.0 - LAMBDA_INIT, None, ALU.mult), reads=["sl_raw"], writes=["subs"])
            neglam = sb.alloc(1, F32)
            S.op("dve", TS(neglam, lam, -1.0, None, ALU.mult), writes=["neglam"])
            eps_t = sb.alloc(1, F32)
            S.op("dve", MEMSET(eps_t, EPS), writes=["eps_t"])
            sets = []
            for _ in range(2):
                kx = [sb.alloc(SEQ, BF16) for _ in range(2)]
                vh = sb.alloc(64 * 128, BF16)
                qx = [[sb.alloc(TOK, BF16) for _ in range(2)] for _ in range(2)]
                sets.append((kx, vh, qx))
            Eb = [sb.alloc(512, BF16) for _ in range(4)]
            tmpb = [sb.alloc(512, F32) for _ in range(2)]
            rzt = sb.alloc(512, F32)
            ob = [sb.alloc(512, F32) for _ in range(2)]
            df = sb.alloc(512, F32)
            sq = sb.alloc(512, BF16)
            sd = sb.alloc(512, F32)
            o16 = [sb.alloc(512, BF16) for _ in range(2)]
            dcnt = [0]
            ocnt = [0]
            cnt = [0]

            def loads(h):
                st_ = h % 2
                kx, vh, qx = sets[st_]
                vh3 = v3(vh, 64)
                semn = "set%d" % st_
                for b in range(2):
                    S.dma("sp", DMA(kx[b][0:64, :], KT[2 * h + b]), semn, writes=["Kr%d%d" % (st_, b)])
                    S.dma("pool", DMA(kx[b][64:68, :], kaug[h]), semn, writes=["Ka%d%d" % (st_, b)])
                    for sg in range(2):
                        S.dma("sp", DMA(qx[b][sg][0:64, :], QT[2 * h + b]), semn, writes=["Qr%d%d%d" % (st_, b, sg)])
                        S.dma("pool", DMA(qx[b][sg][64:68, :], qaug[h, sg]), semn, writes=["Qa%d%d%d" % (st_, b, sg)])
                for part in range(4):
                    S.dma("sp", DMA(vh3[:, part * 16:(part + 1) * 16, :],
                                    VV[part * 2048:(part + 1) * 2048, h * 128:(h + 1) * 128].rearrange("(t p) d -> p t d", p=128)),
                          semn, writes=["V%d_%d" % (st_, part)])

            items = [(h, qi, b, kj) for h in range(NH) for qi in range(8) for b in range(2) for kj in range(64)]
            slot = {}
            LA = 2
            deferred = []

            def stage1(n):
                h, qi, b, kj = items[n]
                slope = 2.0 ** (-(h + 1))
                st_ = h % 2
                kx, vh, qx = sets[st_]
                c_ = cnt[0]
                cnt[0] += 1
                slot[n] = c_ % 4
                sbk = 4 + c_ % 4
                E = Eb[c_ % 4]
                ek = "E%d" % (c_ % 4)
                kkeys = ["Kr%d%d" % (st_, b), "Ka%d%d" % (st_, b)]
                diag = (kj < 32 and 4 * qi <= kj < 4 * qi + 4)
                if diag:
                    sg, rows = 0, 64
                else:
                    sg = 1 if (kj < 32 and kj < 4 * qi) else 0
                    rows = 68
                qkeys = ["Qr%d%d%d" % (st_, b, sg), "Qa%d%d%d" % (st_, b, sg)]
                S.op("pe", MM(bank(sbk), kx[b][0:rows, kj * 128:(kj + 1) * 128],
                              qx[b][sg][0:rows, qi * 512:(qi + 1) * 512], True, True),
                     reads=kkeys + qkeys, writes=["ps%d" % sbk])
                if diag:
                    v = kj - 4 * qi
                    tm = tmpb[dcnt[0] % 2]
                    tk = "tmp%d" % (dcnt[0] % 2)
                    dcnt[0] += 1
                    S.op("dve", STT(tm, T0[:, v * 512:(v + 1) * 512], slope, bank(sbk), ALU.mult, ALU.add),
                         reads=["T0", "ps%d" % sbk], writes=[tk])
                    S.op("act", ACTF(E, tm, AF.Exp), reads=[tk], writes=[ek])
                else:
                    S.op("act", ACTF(E, bank(sbk), AF.Exp), reads=["ps%d" % sbk], writes=[ek])

            def epi_b(h, qi):
                c_ = cnt[0]
                cnt[0] += 1
                sbk = 4 + c_ % 4
                S.op("pe", MM(bank(sbk), ones_b, sq, True, True), reads=["sq", "ones_b"], writes=["ps%d" % sbk])
                S.op("act", ACTF(sd, bank(sbk), AF.Sqrt, bias=eps_t[:, 0:1], scale=1.0 / 128.0),
                     reads=["ps%d" % sbk, "eps_t"], writes=["sd"])
                S.op("dve", lambda e: e.reciprocal(sd, sd), reads=["sd"], writes=["sd"])
                S.op("dve", TT(df, df, sd, ALU.mult), reads=["df", "sd"], writes=["df"])
                o_ = o16[ocnt[0] % 2]
                ok_ = "o16_%d" % (ocnt[0] % 2)
                ocnt[0] += 1
                S.op("dve", TS(o_, df, subs[:, 0:1], None, ALU.mult), reads=["df", "subs"], writes=[ok_])
                S.dma("sp", DMA(OACT[h * 128:(h + 1) * 128, qi * 512:(qi + 1) * 512], o_), ok_, reads=[ok_])

            def stage2(m, n):
                h, qi, b, kj = items[m]
                st_ = h % 2
                kx, vh, qx = sets[st_]
                vh3 = v3(vh, 64)
                vkeys = ["V%d_%d" % (st_, part) for part in range(4)]
                E = Eb[slot[m]]
                ek = "E%d" % slot[m]
                Ob, Zb = 2 * b, 2 * b + 1
                edge = (kj == 0 or kj == 63)
                S.op("pe", MM(bank(Ob), vh3[:, kj, :], E, kj == 0, kj == 63),
                     reads=[ek] + vkeys, writes=["ps%d" % Ob] if edge else ())
                S.op("pe", MM(bank(Zb), ones_b, E, kj == 0, kj == 63),
                     reads=[ek, "ones_b"], writes=["ps%d" % Zb] if edge else ())
                if kj == 63:
                    S.op("dve", lambda e, Zb=Zb: e.reciprocal(rzt, bank(Zb)), reads=["ps%d" % Zb], writes=["rzt"])
                    S.op("dve", TT(ob[b], bank(Ob), rzt, ALU.mult), reads=["ps%d" % Ob, "rzt"], writes=["ob%d" % b])
                    if b == 1:
                        S.op("dve", STT(df, ob[1], neglam[:, 0:1], ob[0], ALU.mult, ALU.add),
                             reads=["ob0", "ob1", "neglam"], writes=["df"])
                        S.op("act", ACTF(sq, df, AF.Square), reads=["df"], writes=["sq"])
                        deferred.append((n + 6, h, qi))

            loads(0)
            nit = len(items)
            for n in range(nit + LA + 8):
                if n < nit:
                    h, qi, b, kj = items[n]
                    if (qi, b, kj) == (0, 0, 0) and h + 1 < NH:
                        loads(h + 1)
                    stage1(n)
                m = n - LA
                if 0 <= m < nit:
                    stage2(m, n)
                while deferred and deferred[0][0] <= n:
                    _, hh, qq = deferred.pop(0)
                    epi_b(hh, qq)
            assert not deferred

        phase2()
        S.barrier()
        sb.reset()
        if stop_after <= 3:
            fin = S.dma("sp", DMA(out[0:128, 0:128], ident), "fin", reads=["ident"])
            S.emit(final_waits=[("sp", fin)])
            return nc

        def ln_rows(y, gb, bb, st4, junk, tag):
            S.op("dve", lambda e: e.reduce_sum(st4[:, 0:1], y, axis=AX.X), reads=[tag], writes=[tag + "s0"])
            S.op("dve", TS(st4[:, 1:2], st4[:, 0:1], -1.0 / D, None, ALU.mult), reads=[tag + "s0"], writes=[tag + "s1"])
            S.op("act", ACTF(junk, y, AF.Square, bias=st4[:, 1:2], accum=st4[:, 2:3]),
                 reads=[tag, tag + "s1"], writes=[tag + "s2", "junk"])
            S.op("dve", TS(st4[:, 3:4], st4[:, 2:3], 1.0 / D, EPS, ALU.mult, ALU.add), reads=[tag + "s2"], writes=[tag + "s3"])
            S.op("act", lambda e: e.sqrt(st4[:, 3:4], st4[:, 3:4]), reads=[tag + "s3"], writes=[tag + "s3"])
            S.op("dve", lambda e: e.reciprocal(st4[:, 3:4], st4[:, 3:4]), reads=[tag + "s3"], writes=[tag + "s3"])
            S.op("dve", TS(y, y, st4[:, 1:2], st4[:, 3:4], ALU.add, ALU.mult), reads=[tag, tag + "s1", tag + "s3"], writes=[tag])
            S.op("pool", TT(y, y, gb, ALU.mult), reads=[tag, "gb"], writes=[tag])
            S.op("pool", TT(y, y, bb, ALU.add), reads=[tag, "bb"], writes=[tag])

        def proj_ln(inT, Wd, resid, gi, post, extra_alloc=None):
            W = sb.alloc(16 * D, BF16)
            W3 = v3(W, 16)
            for g in range(4):
                S.dma("pool", DMA(W3[:, :, g * 512:(g + 1) * 512],
                                  Wd[:, g * 512:(g + 1) * 512].rearrange("(kc p) c -> p kc c", p=128)),
                      "wp", writes=["W%d" % g])
            wkeys = ["W%d" % g for g in range(4)]
            gb = sb.alloc(D, F32)
            bb = sb.alloc(D, F32)
            S.dma("sp", DMA(gb, lnp[gi].partition_broadcast(128)), "c0", writes=["gb"])
            S.dma("sp", DMA(bb, lnp[gi + 1].partition_broadcast(128)), "c1", writes=["bb"])
            ins = [sb.alloc(16 * 512, BF16) for _ in range(2)]
            xt = [sb.alloc(D, F32) for _ in range(2)]
            ys = [sb.alloc(D, F32) for _ in range(2)]
            st4 = [sb.alloc(4, F32) for _ in range(2)]
            junk = sb.alloc(D, BF16)
            ctx = extra_alloc() if extra_alloc else None
            for grp in range(8):
                i3 = v3(ins[grp % 2], 16)
                ik = "in%d" % (grp % 2)
                S.dma("sp", DMA(i3, inT[:, grp * 512:(grp + 1) * 512].rearrange("(kc p) t -> p kc t", p=128)), ik, writes=[ik])
                for s_ in range(4):
                    t = grp * 4 + s_
                    par = t % 2
                    x_, y_ = xt[par], ys[par]
                    S.dma("sp", DMA(x_, resid[t * 128:(t + 1) * 128, :]), "xt%d" % par, writes=["xt%d" % par])
                    for cg in range(4):
                        mm_group(S, bank(cg), "ps%d" % cg,
                                 [(i3[:, kc, s_ * 128:(s_ + 1) * 128], W3[:, kc, cg * 512:(cg + 1) * 512]) for kc in range(16)],
                                 [ik] + wkeys)
                    yk = "y%d" % par
                    for cg in range(4):
                        S.op("dve", STT(y_[:, cg * 512:(cg + 1) * 512], x_[:, cg * 512:(cg + 1) * 512], ALPHA, bank(cg),
                                        ALU.mult, ALU.add),
                             reads=["xt%d" % par, "ps%d" % cg], writes=[yk])
                    ln_rows(y_, gb, bb, st4[par], junk, yk)
                    post(ctx, grp, s_, t, y_, yk)

        def phase3():
            def extra():
                return {"x1b": [sb.alloc(D, BF16) for _ in range(2)],
                        "x1T": [sb.alloc(16 * 512, BF16) for _ in range(2)]}

            def post(ctx, grp, s_, t, y_, yk):
                par = t % 2
                S.dma("sp", DMA(X1[t * 128:(t + 1) * 128, :], y_), "x1o%d" % par, reads=[yk])
                xb_ = ctx["x1b"][par]
                S.op("act", lambda e: e.copy(xb_, y_), reads=[yk], writes=["x1b%d" % par])
                psb = ps[:, 2048:3072].bitcast(BF16)
                for kc in range(16):
                    S.op("pe", TR(psb[:, kc * 128:(kc + 1) * 128], xb_[:, kc * 128:(kc + 1) * 128], identb),
                         reads=["x1b%d" % par, "identb"], writes=["pst"] if kc in (0, 15) else ())
                xT3 = v3(ctx["x1T"][grp % 2], 16)
                tk = "x1T%d" % (grp % 2)
                S.op("act", lambda e: e.copy(xT3[:, :, s_ * 128:(s_ + 1) * 128], v3(psb, 16)),
                     reads=["pst"] + ([tk] if s_ > 0 else []), writes=[tk])
                if s_ == 3:
                    S.dma("sp", DMA(X1T[:, grp * 512:(grp + 1) * 512].rearrange("(kc p) t -> p kc t", p=128), xT3),
                          tk, reads=[tk])

            proj_ln(OACT, w_out, x_tok, 0, post, extra)

        phase3()
        S.barrier()
        sb.reset()
        if stop_after <= 4:
            fin = S.dma("sp", DMA(out[0:128, 0:128], ident), "fin", reads=["ident"])
            S.emit(final_waits=[("sp", fin)])
            return nc

        def phase4a():
            W = sb.alloc(16 * D, BF16)
            W3 = v3(W, 16)
            for g in range(4):
                S.dma("pool", DMA(W3[:, :, g * 512:(g + 1) * 512],
                                  mem_wq[:, g * 512:(g + 1) * 512].rearrange("(kc p) c -> p kc c", p=128)),
                      "wp", writes=["W%d" % g])
            wkeys = ["W%d" % g for g in range(4)]
            mk3 = v3(mk_sb, 16)
            mv3 = v3(mv_sb, 2)
            ins = [sb.alloc(16 * 512, BF16) for _ in range(2)]
            qT = sb.alloc(16 * 512, BF16)
            qT3 = v3(qT, 16)
            omT = [sb.alloc(16 * 512, BF16) for _ in range(2)]
            st4 = [sb.alloc(4, F32) for _ in range(2)]
            pe_ = [sb.alloc(256, F32) for _ in range(2)]
            pn = [sb.alloc(256, BF16) for _ in range(2)]
            pT = [sb.alloc(256, BF16) for _ in range(2)]
            scale = 512.0 ** -0.5
            psb = ps[:, 3072:3584].bitcast(BF16)
            it = 0
            for grp in range(8):
                i3 = v3(ins[grp % 2], 16)
                ik = "in%d" % (grp % 2)
                S.dma("sp", DMA(i3, X1T[:, grp * 512:(grp + 1) * 512].rearrange("(kc p) t -> p kc t", p=128)), ik, writes=[ik])
                for c in range(16):
                    b = c % 2
                    mm_group(S, bank(b), "ps%d" % b,
                             [(W3[:, kc, c * 128:(c + 1) * 128], i3[:, kc, :]) for kc in range(16)], [ik] + wkeys)
                    if c % 2 == 0:
                        S.op("act", lambda e, c=c, b=b: e.copy(qT3[:, c, :], bank(b)), reads=["ps%d" % b], writes=["qT%d" % c])
                    else:
                        S.op("dve", TC(qT3[:, c, :], bank(b)), reads=["ps%d" % b], writes=["qT%d" % c])
                o3 = v3(omT[grp % 2], 16)
                ok_ = "omT%d" % (grp % 2)
                for s_ in range(4):
                    for hd in range(4):
                        par = it % 2
                        it += 1
                        sbk = 2 + par
                        mm_group(S, bank(sbk, 256), "ps%d" % sbk,
                                 [(qT3[:, hd * 4 + c, s_ * 128:(s_ + 1) * 128], mk3[:, hd * 4 + c, :]) for c in range(4)],
                                 ["qT%d" % (hd * 4 + c) for c in range(4)])
                        s4 = st4[par]
                        sk = "s4_%d" % par
                        S.op("dve", lambda e, s4=s4, sbk=sbk: e.reduce_max(s4[:, 0:1], bank(sbk, 256), axis=AX.X),
                             reads=["ps%d" % sbk], writes=[sk + "a"])
                        S.op("dve", TS(s4[:, 1:2], s4[:, 0:1], -scale, None, ALU.mult), reads=[sk + "a"], writes=[sk + "b"])
                        S.op("act", ACTF(pe_[par], bank(sbk, 256), AF.Exp, bias=s4[:, 1:2], scale=scale, accum=s4[:, 2:3]),
                             reads=["ps%d" % sbk, sk + "b"], writes=["pe%d" % par, sk + "c"])
                        S.op("dve", lambda e, s4=s4: e.reciprocal(s4[:, 3:4], s4[:, 2:3]), reads=[sk + "c"], writes=[sk + "d"])
                        S.op("dve", TS(pn[par], pe_[par], s4[:, 3:4], None, ALU.mult), reads=["pe%d" % par, sk + "d"], writes=["pn%d" % par])
                        for mt in range(2):
                            S.op("pe", TR(psb[:, (par * 2 + mt) * 128:(par * 2 + mt + 1) * 128], pn[par][:, mt * 128:(mt + 1) * 128], identb),
                                 reads=["pn%d" % par, "identb"], writes=["pstb%d" % par] if mt in (0, 1) else ())
                        S.op("act", lambda e, par=par: e.copy(pT[par], psb[:, par * 256:(par + 1) * 256]),
                             reads=["pstb%d" % par], writes=["pT%d" % par])
                        obk = 4 + par
                        for dc in range(4):
                            for mt in range(2):
                                S.op("pe", MM(bank(obk, 128, dc * 128), mv3[:, mt, hd * 512 + dc * 128:hd * 512 + (dc + 1) * 128],
                                              pT[par][:, mt * 128:(mt + 1) * 128], mt == 0, mt == 1),
                                     reads=["pT%d" % par], writes=["ps%d" % obk] if (dc, mt) in ((0, 0), (3, 1)) else ())
                        S.op("dve", lambda e, o3=o3, hd=hd, s_=s_, obk=obk: e.tensor_copy(
                            o3[:, hd * 4:(hd + 1) * 4, s_ * 128:(s_ + 1) * 128], v3(bank(obk), 4)),
                             reads=["ps%d" % obk] + ([ok_] if (s_, hd) != (0, 0) else []), writes=[ok_])
                S.dma("sp", DMA(OM[:, grp * 512:(grp + 1) * 512].rearrange("(kc p) t -> p kc t", p=128), o3), ok_, reads=[ok_])

        phase4a()
        S.barrier()
        sb.reset()
        if stop_after <= 4.5:
            fin = S.dma("sp", DMA(out[0:128, 0:128], ident), "fin", reads=["ident"])
            S.emit(final_waits=[("sp", fin)])
            return nc

        def phase4b():
            def extra():
                c = {}
                c["x2b"] = [sb.alloc(D, BF16) for _ in range(2)]
                c["x2T"] = sb.alloc(16 * 128, F32)
                c["rw"] = sb.alloc(16 * NE, F32)
                c["rb"] = sb.alloc(NE, F32)
                c["ut"] = sb.alloc(128, F32)
                c["onesf"] = sb.alloc(128, F32)
                c["ec"] = sb.alloc(NE, F32)
                c["base"] = sb.alloc(NE, F32)
                for nm in ("lg", "mask", "ex", "G", "rowid", "keyv", "oh"):
                    c[nm] = sb.alloc(NE, F32)
                c["mx"] = sb.alloc(8, F32)
                c["kmx"] = sb.alloc(8, F32)
                c["sm"] = sb.alloc(4, F32)
                c["rows4"] = sb.alloc(4, F32)
                c["idx4"] = [sb.alloc(4, I32) for _ in range(2)]
                c["g4"] = [sb.alloc(4, F32) for _ in range(2)]
                S.dma("sp", DMA(v3(c["rw"], 16), router_w.rearrange("(kc p) e -> p kc e", p=128)), "c0", writes=["rw"])
                S.dma("sp", DMA(c["rb"], router_b.partition_broadcast(128)), "c1", writes=["rb"])
                S.dma("sp", DMA(c["ut"], utri), "c0", writes=["ut"])
                S.dma("sp", DMA(c["ec"], ecap), "c1", writes=["ec"])
                S.op("dve", MEMSET(c["onesf"], 1.0), writes=["onesf"])
                S.op("dve", MEMSET(c["base"], 0.0), writes=["base"])
                return c

            def post(c, grp, s_, t, y_, yk):
                par = t % 2
                S.dma("sp", DMA(X2[t * 128:(t + 1) * 128, :], y_), "x2o%d" % par, reads=[yk])
                xb_ = c["x2b"][par]
                xbk = "x2b%d" % par
                S.op("act", lambda e: e.copy(xb_, y_), reads=[yk], writes=[xbk])
                for kc in range(16):
                    S.op("pe", TR(ps[:, 2048 + kc * 128:2048 + (kc + 1) * 128], y_[:, kc * 128:(kc + 1) * 128], ident),
                         reads=[yk, "ident"], writes=["pst", "psp", "psc"] if kc in (0, 15) else ())
                x2T = c["x2T"]
                S.op("act", lambda e: e.copy(x2T[:, 0:1024], ps[:, 2048:3072]), reads=["pst"], writes=["x2Ta"])
                S.op("dve", TC(x2T[:, 1024:2048], ps[:, 3072:4096]), reads=["pst"], writes=["x2Tb"])
                rw3 = v3(c["rw"], 16)
                x2T3 = v3(x2T, 16)
                mm_group(S, ps[:, 2048:2048 + NE], "pst",
                         [(x2T3[:, kc, :], rw3[:, kc, :]) for kc in range(16)], ["x2Ta", "x2Tb", "rw"])
                lg, mask, ex, G, rowid, keyv, oh = (c[n] for n in ("lg", "mask", "ex", "G", "rowid", "keyv", "oh"))
                mx, kmx, sm, rows4 = c["mx"], c["kmx"], c["sm"], c["rows4"]
                S.op("dve", TT(lg, ps[:, 2048:2048 + NE], c["rb"], ALU.add), reads=["pst", "rb"], writes=["lg"])
                S.op("dve", lambda e: e.max(out=mx, in_=lg), reads=["lg"], writes=["mx"])
                S.op("dve", TS(mask, lg, mx[:, 3:4], None, ALU.is_ge), reads=["lg", "mx"], writes=["mask"])
                S.op("dve", TS(sm[:, 0:1], mx[:, 0:1], -1.0, None, ALU.mult), reads=["mx"], writes=["sm0"])
                S.op("act", ACTF(ex, lg, AF.Exp, bias=sm[:, 0:1]), reads=["lg", "sm0"], writes=["ex"])
                S.op("dve", TT(ex, ex, mask, ALU.mult), reads=["ex", "mask"], writes=["ex"])
                S.op("dve", lambda e: e.reduce_sum(sm[:, 1:2], ex, axis=AX.X), reads=["ex"], writes=["sm1"])
                S.op("dve", lambda e: e.reciprocal(sm[:, 1:2], sm[:, 1:2]), reads=["sm1"], writes=["sm1"])
                S.op("dve", TS(G, ex, sm[:, 1:2], None, ALU.mult), reads=["ex", "sm1"], writes=["G"])
                S.op("pe", MM(ps[:, 2560:2560 + NE], c["ut"], mask, True, True), reads=["ut", "mask"], writes=["psp"])
                S.op("pe", MM(ps[:, 2560 + NE:2560 + 2 * NE], c["onesf"], mask, True, True), reads=["onesf", "mask"], writes=["psc"])
                S.op("dve", TT(rowid, ps[:, 2560:2560 + NE], c["base"], ALU.add), reads=["psp", "base"], writes=["rowid"])
                S.op("dve", TT(c["base"], c["base"], ps[:, 2560 + NE:2560 + 2 * NE], ALU.add), reads=["psc", "base", "rowid"], writes=["base"])
                S.op("dve", TS(oh, rowid, float(CAP), None, ALU.is_lt), reads=["rowid"], writes=["oh"])
                S.op("dve", TT(oh, oh, mask, ALU.mult), reads=["oh", "mask"], writes=["oh"])
                S.op("dve", TT(rowid, rowid, c["ec"], ALU.add), reads=["rowid", "ec"], writes=["rowid"])
                S.op("dve", TS(keyv, rowid, -1.0, BIGROW, ALU.mult, ALU.add), reads=["rowid"], writes=["keyv"])
                S.op("dve", TT(keyv, keyv, oh, ALU.mult), reads=["keyv", "oh"], writes=["keyv"])
                S.op("dve", lambda e: e.max(out=kmx, in_=keyv), reads=["keyv"], writes=["kmx"])
                S.op("dve", TS(rows4, kmx[:, 0:4], -1.0, BIGROW, ALU.mult, ALU.add), reads=["kmx"], writes=["rows4"])
                i4 = c["idx4"][par]
                g4 = c["g4"][par]
                ik4 = "idx4_%d" % par
                gk4 = "g4_%d" % par
                S.op("dve", TC(i4, rows4), reads=["rows4"], writes=[ik4])
                for j in range(4):
                    S.op("dve", TS(oh, keyv, kmx[:, j:j + 1], None, ALU.is_equal), reads=["keyv", "kmx"], writes=["oh"])
                    S.op("dve", TT(oh, oh, G, ALU.mult), reads=["oh", "G"], writes=["oh"])
                    S.op("dve", lambda e, j=j: e.reduce_sum(g4[:, j:j + 1], oh, axis=AX.X), reads=["oh"], writes=[gk4 + "_%d" % j])
                for j in range(4):
                    S.dma("pool", lambda e, j=j: e.indirect_dma_start(
                        out=XG, out_offset=bass.IndirectOffsetOnAxis(ap=i4[:, j:j + 1], axis=0),
                        in_=xb_, in_offset=None, bounds_check=bcreg(e), oob_is_err=False),
                          "scat%d_%d" % (par, j), reads=[xbk, ik4])
                S.dma("sp", DMA(IDX4[t * 128:(t + 1) * 128, :], i4), "i4o%d" % par, reads=[ik4])
                S.dma("sp", DMA(G4[t * 128:(t + 1) * 128, :], g4), "g4o%d" % par, reads=[gk4 + "_%d" % j for j in range(4)])

            proj_ln(OM, mem_wo, X1, 2, post, extra)

        phase4b()
        S.barrier()
        sb.reset()
        if stop_after <= 5:
            fin = S.dma("sp", DMA(out[0:128, 0:128], ident), "fin", reads=["ident"])
            S.emit(final_waits=[("sp", fin)])
            return nc

        NST = CAP // 128
        R2 = CAP - 512

        def phase5():
            bg_sb = sb.alloc(2 * NE * 16, F32)
            S.dma("sp", DMA(bg_sb, bgu), "c0", writes=["bgu"])
            xgT = [sb.alloc(16 * CAP, BF16) for _ in range(2)]
            wt = [sb.alloc(16 * 512, BF16) for _ in range(4)]
            actT = sb.alloc(16 * CAP, BF16)
            a3 = v3(actT, 16)
            bd = [sb.alloc(D, F32) for _ in range(2)]
            xgt = [sb.alloc(D, BF16) for _ in range(2)]
            gs = [sb.alloc(CAP, F32) for _ in range(2)]
            sg = [sb.alloc(CAP, F32) for _ in range(2)]
            us = [sb.alloc(CAP, F32) for _ in range(2)]
            ysb = [sb.alloc(512, F32) for _ in range(4)]
            psb = ps[:, 3072:4096].bitcast(BF16)
            wi = 0
            xi = 0
            yi = 0
            fci = 0
            cst = {"xi": 0}

            def xg_load(e):
                ep = e % 2
                S.dma("sp", DMA(bd[ep], b_down[e].partition_broadcast(128)), "bd%d" % ep, writes=["bd%d" % ep])
                x3 = v3(xgT[ep], 16)
                xk = "xgT%d" % ep
                xi = cst["xi"]
                for st_ in range(NST):
                    xt_ = xgt[xi % 2]
                    xtk = "xgt%d" % (xi % 2)
                    xi += 1
                    r0 = e * CAP + st_ * 128
                    S.dma("sp", DMA(xt_, XG[r0:r0 + 128, :]), xtk, writes=[xtk])
                    for kc in range(16):
                        S.op("pe", TR(psb[:, kc * 128:(kc + 1) * 128], xt_[:, kc * 128:(kc + 1) * 128], identb),
                             reads=[xtk, "identb"], writes=["ps6", "ps7"] if kc in (0, 15) else ())
                    eng = "act" if st_ % 2 == 0 else "dve"
                    S.op(eng, TC(x3[:, :, st_ * 128:(st_ + 1) * 128], v3(psb, 16)) if eng == "dve" else
                         (lambda e_, x3=x3, st_=st_: e_.copy(x3[:, :, st_ * 128:(st_ + 1) * 128], v3(psb, 16))),
                         reads=["ps6", "ps7"] + ([xk] if st_ > 0 else []), writes=[xk])
                cst["xi"] = xi

            xg_load(0)
            for e in range(NE):
                ep = e % 2
                x3 = v3(xgT[ep], 16)
                xk = "xgT%d" % ep
                for fg in range(4):
                    wg = wt[wi % 4]
                    wgk = "wt%d" % (wi % 4)
                    wi += 1
                    wu = wt[wi % 4]
                    wuk = "wt%d" % (wi % 4)
                    wi += 1
                    S.dma("pool", DMA(v3(wg, 16), w_gate[e][:, fg * 512:(fg + 1) * 512].rearrange("(kc p) c -> p kc c", p=128)),
                          wgk, writes=[wgk])
                    S.dma("pool", DMA(v3(wu, 16), w_up[e][:, fg * 512:(fg + 1) * 512].rearrange("(kc p) c -> p kc c", p=128)),
                          wuk, writes=[wuk])
                    wg3, wu3 = v3(wg, 16), v3(wu, 16)
                    for fs in range(4):
                        fc = fg * 4 + fs
                        set_ = fci % 2
                        fci += 1
                        g0, g1, u0, u1 = 4 * set_, 4 * set_ + 1, 4 * set_ + 2, 4 * set_ + 3
                        if set_ == 1:
                            g0, g1, u0, u1 = 4, 5, 6, 7
                        mm_group(S, bank(g0), "ps%d" % g0,
                                 [(wg3[:, kc, fs * 128:(fs + 1) * 128], x3[:, kc, 0:512]) for kc in range(16)], [wgk, xk])
                        mm_group(S, bank(g1, R2), "ps%d" % g1,
                                 [(wg3[:, kc, fs * 128:(fs + 1) * 128], x3[:, kc, 512:CAP]) for kc in range(16)], [wgk, xk])
                        mm_group(S, bank(u0), "ps%d" % u0,
                                 [(wu3[:, kc, fs * 128:(fs + 1) * 128], x3[:, kc, 0:512]) for kc in range(16)], [wuk, xk])
                        mm_group(S, bank(u1, R2), "ps%d" % u1,
                                 [(wu3[:, kc, fs * 128:(fs + 1) * 128], x3[:, kc, 512:CAP]) for kc in range(16)], [wuk, xk])
                        gs_, sg_, us_ = gs[set_], sg[set_], us[set_]
                        bgc = bg_sb[:, e * 16 + fc:e * 16 + fc + 1]
                        buc = bg_sb[:, NE * 16 + e * 16 + fc:NE * 16 + e * 16 + fc + 1]
                        S.op("dve", TS(gs_, ps[:, g0 * 512:g0 * 512 + CAP], bgc, 7.0, ALU.add, ALU.min),
                             reads=["ps%d" % g0, "ps%d" % g1, "bgu"], writes=["gs%d" % set_])
                        S.op("act", ACTF(sg_, gs_, AF.Sigmoid, scale=1.702), reads=["gs%d" % set_], writes=["sg%d" % set_])
                        S.op("dve", TS(us_, ps[:, u0 * 512:u0 * 512 + CAP], buc, 7.0, ALU.add, ALU.min),
                             reads=["ps%d" % u0, "ps%d" % u1, "bgu"], writes=["us%d" % set_])
                        S.op("dve", TS(us_, us_, -7.0, 1.0, ALU.max, ALU.add), reads=["us%d" % set_], writes=["us%d" % set_])
                        S.op("dve", TT(gs_, gs_, sg_, ALU.mult), reads=["gs%d" % set_, "sg%d" % set_], writes=["gs%d" % set_])
                        S.op("dve", TT(a3[:, fc, :], gs_, us_, ALU.mult), reads=["gs%d" % set_, "us%d" % set_], writes=["actT%d" % fc])
                akeys = ["actT%d" % fc for fc in range(16)]
                if e + 1 < NE:
                    xg_load(e + 1)
                for dg in range(4):
                    wd = wt[wi % 4]
                    wdk = "wt%d" % (wi % 4)
                    wi += 1
                    S.dma("pool", DMA(v3(wd, 16), w_down[e][:, dg * 512:(dg + 1) * 512].rearrange("(fc p) c -> p fc c", p=128)),
                          wdk, writes=[wdk])
                    wd3 = v3(wd, 16)
                    for st_ in range(NST):
                        b = yi % 8
                        mm_group(S, bank(b), "ps%d" % b,
                                 [(a3[:, fc, st_ * 128:(st_ + 1) * 128], wd3[:, fc, :]) for fc in range(16)], [wdk] + akeys)
                        y_ = ysb[yi % 4]
                        yk = "ysb%d" % (yi % 4)
                        yi += 1
                        S.op("dve", TT(y_, bank(b), bd[ep][:, dg * 512:(dg + 1) * 512], ALU.add),
                             reads=["ps%d" % b, "bd%d" % ep], writes=[yk])
                        r0 = e * CAP + st_ * 128
                        S.dma("sp", DMA(YG[r0:r0 + 128, dg * 512:(dg + 1) * 512], y_), yk, reads=[yk])

        phase5()
        S.barrier()
        sb.reset()

        def phase6():
            gb = sb.alloc(D, F32)
            bb = sb.alloc(D, F32)
            S.dma("sp", DMA(gb, lnp[4].partition_broadcast(128)), "c0", writes=["gb"])
            S.dma("sp", DMA(bb, lnp[5].partition_broadcast(128)), "c1", writes=["bb"])
            yj = [sb.alloc(4 * D, F32) for _ in range(2)]
            x2t = [sb.alloc(D, F32) for _ in range(2)]
            ys = [sb.alloc(D, F32) for _ in range(2)]
            i4s = [sb.alloc(4, I32) for _ in range(2)]
            g4s = [sb.alloc(4, F32) for _ in range(2)]
            st4 = [sb.alloc(4, F32) for _ in range(2)]
            junk = sb.alloc(D, BF16)
            last = {}
            for t in range(TOK // 128):
                par = t % 2
                i4, g4, x_, y_, yy = i4s[par], g4s[par], x2t[par], ys[par], yj[par]
                S.dma("sp", DMA(i4, IDX4[t * 128:(t + 1) * 128, :]), "i4l%d" % par, writes=["i4_%d" % par])
                S.dma("sp", DMA(g4, G4[t * 128:(t + 1) * 128, :]), "g4l%d" % par, writes=["g4_%d" % par])
                S.dma("sp", DMA(x_, X2[t * 128:(t + 1) * 128, :]), "x2l%d" % par, writes=["x2_%d" % par])
                S.op("pool", MEMSET(yy, 0.0), writes=["yj%d_%d" % (par, j) for j in range(4)])
                for j in range(4):
                    S.dma("pool", lambda e, j=j, yy=yy, i4=i4: e.indirect_dma_start(
                        out=yy[:, j * D:(j + 1) * D], out_offset=None, in_=YG,
                        in_offset=bass.IndirectOffsetOnAxis(ap=i4[:, j:j + 1], axis=0),
                        bounds_check=bcreg(e), oob_is_err=False),
                          "gat%d_%d" % (par, j), reads=["i4_%d" % par], writes=["yj%d_%d" % (par, j)])
                yk = "y%d" % par
                S.op("act", ACTF(y_, x_, AF.Copy, scale=ALPHA), reads=["x2_%d" % par], writes=[yk])
                for j in range(4):
                    S.op("dve", STT(y_, yy[:, j * D:(j + 1) * D], g4[:, j:j + 1], y_, ALU.mult, ALU.add),
                         reads=["yj%d_%d" % (par, j), "g4_%d" % par, yk], writes=[yk])
                ln_rows(y_, gb, bb, st4[par], junk, yk)
                last[par] = S.dma("sp", DMA(out[t * 128:(t + 1) * 128, :], y_), "outo%d" % par, reads=[yk])
            return list(last.values())

        finals = phase6()
        S.emit(final_waits=[("sp", d) for d in finals])
    return nc


def _host_inputs(inp, ncores=8):
    x = np.asarray(inp["x"], np.float32)
    mem = np.asarray(inp["mem"], np.float32)
    common = {}
    common["w_in"] = np.ascontiguousarray(inp["w_in"][0])
    cw = np.asarray(inp["conv_w"][0], np.float32)
    common["convw"] = np.ascontiguousarray(cw.reshape(3, 8, 128).transpose(2, 1, 0).reshape(128, 24))
    common["subln"] = np.ascontiguousarray(np.asarray(inp["attn_subln_w"][0], np.float32).reshape(128, 1))
    common["lamv"] = np.ascontiguousarray(np.stack([inp["lambda_q1"][0], inp["lambda_k1"][0],
                                                    inp["lambda_q2"][0], inp["lambda_k2"][0]]).astype(np.float32))
    common["w_out"] = np.ascontiguousarray(inp["w_out"][0])
    common["lnp"] = np.ascontiguousarray(np.stack([inp["ln1_g"][0], inp["ln1_b"][0], inp["ln2_g"][0],
                                                   inp["ln2_b"][0], inp["ln3_g"][0], inp["ln3_b"][0]]).astype(np.float32))
    common["mem_wq"] = np.ascontiguousarray(inp["mem_wq"][0])
    common["mem_wkv"] = np.ascontiguousarray(inp["mem_wkv"][0])
    common["mem_wo"] = np.ascontiguousarray(inp["mem_wo"][0])
    common["router_w"] = np.ascontiguousarray(inp["router_w"][0])
    common["router_b"] = np.ascontiguousarray(inp["router_b"][0])
    if "w_gate" in inp:
        common["w_gate"] = np.ascontiguousarray(inp["w_gate"][0])
        common["w_up"] = np.ascontiguousarray(inp["w_up"][0])
        common["w_down"] = np.ascontiguousarray(inp["w_down"][0])
    bg = np.asarray(inp["b_gate"][0], np.float32).reshape(NE, 16, 128).transpose(2, 0, 1).reshape(128, NE * 16)
    bu = np.asarray(inp["b_up"][0], np.float32).reshape(NE, 16, 128).transpose(2, 0, 1).reshape(128, NE * 16)
    common["bgu"] = np.ascontiguousarray(np.concatenate([bg, bu], axis=1))
    common["b_down"] = np.ascontiguousarray(inp["b_down"][0])
    kr = np.arange(128)[:, None, None]
    vv = np.arange(4)[None, :, None]
    qr = np.arange(512)[None, None, :]
    common["tdiag"] = np.ascontiguousarray((-np.abs(vv * 128 + kr - qr)).astype(np.float32).reshape(128, 2048))
    common["ident"] = np.eye(128, dtype=np.float32)
    common["ecap"] = np.ascontiguousarray(np.broadcast_to((np.arange(NE) * CAP).astype(np.float32)[None, :], (128, NE)))
    common["utri"] = np.triu(np.ones((128, 128), np.float32), k=1)
    slopes = 2.0 ** (-np.arange(1, NH + 1, dtype=np.float64))
    maps = []
    for c in range(ncores):
        b, half = c // 2, c % 2
        own = slice(half * TOK, (half + 1) * TOK)
        oth = slice((1 - half) * TOK, (2 - half) * TOK)
        xb = x[b]
        m = dict(common)
        m["xT_kv"] = np.ascontiguousarray(np.concatenate([xb[own], xb[oth]], axis=0).T)
        xo = np.zeros((TOK + 2, D), np.float32)
        xo[1:TOK + 1] = xb[own]
        if half == 1:
            xo[0] = xb[TOK - 1]
        else:
            xo[TOK + 1] = xb[TOK]
        m["xT_own"] = np.ascontiguousarray(xo.T)
        m["x_tok"] = np.ascontiguousarray(xb[own])
        m["memT"] = np.ascontiguousarray(mem[b].T)
        qpos = np.arange(half * TOK, (half + 1) * TOK)
        kpos = np.concatenate([np.arange(half * TOK, (half + 1) * TOK), np.arange((1 - half) * TOK, (2 - half) * TOK)])
        sig_other = 1.0 if half == 0 else -1.0
        ka = np.zeros((NH, 4, SEQ), np.float32)
        qa = np.zeros((NH, 2, 4, TOK), np.float32)
        for h in range(NH):
            s = slopes[h]
            ksig = np.ones(SEQ)
            ksig[TOK:] = sig_other
            ka[h, 0] = ksig
            ka[h, 1] = ksig
            ka[h, 2] = -64.0 * s * (kpos // 64) * ksig
            ka[h, 3] = -s * (kpos % 64) * ksig
            plus = np.stack([64.0 * s * (qpos // 64), s * (qpos % 64), np.ones(TOK), np.ones(TOK)])
            qa[h, 0] = plus
            qa[h, 1] = -plus
        m["kaug"] = ka
        m["qaug"] = qa
        maps.append(m)
    return maps


_CACHE = {}


def kernel(**inputs):
    stop_after = float(os.environ.get("MK_STOP", "99"))
    dbg = os.environ.get("MK_DBG", "0") == "1"
    key = (stop_after, dbg)
    if key not in _CACHE:
        _CACHE[key] = build_program(stop_after, dbg)
    nc = _CACHE[key]
    ncores = int(os.environ.get("MK_CORES", "8"))
    maps = _host_inputs(inputs, ncores)
    if os.environ.get("MK_TRACE", "0") == "1":
        res = run_bass_kernel_spmd(nc, maps, core_ids=list(range(ncores)), trace=True)
        print("EXEC_TIME_NS", res.exec_time_ns)
    else:
        res = run_bass_kernel_spmd(nc, maps, core_ids=list(range(ncores)))
    if dbg:
        return res
    outp = np.empty((NB, SEQ, D), np.float32)
    for c in range(ncores):
        b, half = c // 2, c % 2
        outp[b, half * TOK:(half + 1) * TOK] = res.results[c]["out"]
    return outp
```

```python
import math
import os
from contextlib import ExitStack

import numpy as np

import concourse.bass as bass
import concourse.mybir as mybir
from concourse.bass_utils import run_bass_kernel_spmd

F32 = mybir.dt.float32
BF16 = mybir.dt.bfloat16
I32 = mybir.dt.int32
AF = mybir.ActivationFunctionType
ALU = mybir.AluOpType
AX = mybir.AxisListType

D = 2048
SEQ = 8192
NB = 4
TOK = 4096
NH = 8
NE = 32
CAP = 768
NROWS = NE * CAP
ALPHA = 2.0 ** 0.25
LAMBDA_INIT = 0.8 - 0.6 * math.exp(0.0)
EPS = 1e-5
BIGROW = 1.0e6


class _Op:
    __slots__ = ("eng", "fn", "deps", "marked", "sem", "inc", "sigval", "is_dma", "gidx")

    def __init__(self, eng, fn):
        self.eng = eng
        self.fn = fn
        self.deps = []
        self.marked = False
        self.sem = None
        self.inc = 1
        self.sigval = None
        self.is_dma = False
        self.gidx = 0


class Sched:
    ENGS = ("pe", "act", "dve", "pool", "sp")

    def __init__(self, nc, stack):
        self.nc = nc
        self.stack = stack
        self.ops = {e: [] for e in self.ENGS}
        self.buf = {}
        self.sems = {}
        self.dma_count = {}
        self.dma_hist = {}
        self.gcount = 0
        self.last_dma = {}
        self.regs = {}
        self.pool_prev = {}

    def sem(self, name):
        if name not in self.sems:
            self.sems[name] = self.stack.enter_context(self.nc.semaphore(name))
        return self.sems[name]

    def _track(self, op, reads, writes):
        deps = []
        for k in reads:
            st = self.buf.get(k)
            if st is None:
                st = self.buf[k] = [None, []]
            if st[0] is not None:
                deps.append(st[0])
            st[1].append(op)
        for k in writes:
            st = self.buf.get(k)
            if st is None:
                st = self.buf[k] = [None, []]
            if st[0] is not None:
                deps.append(st[0])
            deps.extend(st[1])
            self.buf[k] = [op, []]
        seen = set()
        for d in deps:
            if d is op or id(d) in seen:
                continue
            seen.add(id(d))
            if d.eng == "pe" and op.eng == "pe" and not d.is_dma and not op.is_dma:
                continue
            d.marked = True
            op.deps.append(d)

    def op(self, eng, fn, reads=(), writes=()):
        o = _Op(eng, fn)
        self.gcount += 1
        o.gidx = self.gcount
        o.sem = "c_" + eng
        self._track(o, reads, writes)
        self.ops[eng].append(o)
        return o

    def dma(self, eng, fn, sem, reads=(), writes=()):
        o = _Op(eng, fn)
        self.gcount += 1
        o.gidx = self.gcount
        o.is_dma = True
        if eng == "pool":
            self.prot = (getattr(self, "prot", -1) + 1) % 16
            o.sem = "d_pq%d" % self.prot
            prev = self.last_dma.get(o.sem) or self.pool_prev.get(o.sem)
            if prev is not None:
                o.deps.append(prev)
            self.pool_prev[o.sem] = o
        else:
            o.sem = "d_" + sem
        o.inc = 16
        o.marked = True
        c = self.dma_count.get(o.sem, 0) + 16
        self.dma_count[o.sem] = c
        o.sigval = c
        self.dma_hist.setdefault(o.sem, []).append((o.gidx, c))
        self.last_dma[o.sem] = o
        self._track(o, reads, writes)
        self.ops[eng].append(o)
        return o

    def barrier(self):
        lasts = []
        for e in self.ENGS:
            for o in reversed(self.ops[e]):
                if not o.is_dma and o.fn is not None:
                    lasts.append(o)
                    break
        lasts.extend(self.last_dma.values())
        self.last_dma = {}
        for o in lasts:
            o.marked = True
        for e in self.ENGS:
            b = _Op(e, None)
            self.gcount += 1
            b.gidx = self.gcount
            b.sem = "c_" + e
            b.deps = [o for o in lasts]
            self.ops[e].append(b)
        self.buf = {}

    def emit(self, final_waits=()):
        nc = self.nc
        cnt = {e: 0 for e in self.ENGS}
        for e in self.ENGS:
            for o in self.ops[e]:
                if o.is_dma or o.fn is None:
                    continue
                if o.marked:
                    cnt[e] += 1
                    o.sigval = cnt[e]
        for name in sorted({o.sem for e in self.ENGS for o in self.ops[e] if o.marked}):
            self.sem(name)
        fw = {}
        for (ename, o) in final_waits:
            fw.setdefault(ename, []).append((o.sem, o.sigval))

        def run(ename, eng):
            waited = {}
            for o in self.ops[ename]:
                need = {}
                for d in o.deps:
                    v = d.sigval
                    if d.is_dma:
                        for (g, c) in self.dma_hist[d.sem]:
                            if g < o.gidx and c > v:
                                v = c
                    if need.get(d.sem, 0) < v:
                        need[d.sem] = v
                for sname, v in need.items():
                    if waited.get(sname, 0) >= v:
                        continue
                    if sname == "c_" + ename and ename == "pe":
                        continue
                    waited[sname] = v
                    eng.wait_ge(self.sems[sname], v)
                if o.fn is None:
                    continue
                ins = o.fn(eng)
                if o.marked:
                    ins.then_inc(self.sems[o.sem], o.inc)
            for (sname, v) in fw.get(ename, ()):
                eng.wait_ge(self.sems[sname], v)

        with nc.Block() as block:
            @block.tensor
            def _(eng):
                run("pe", eng)

            @block.scalar
            def _(eng):
                run("act", eng)

            @block.vector
            def _(eng):
                run("dve", eng)

            @block.gpsimd
            def _(eng):
                run("pool", eng)

            @block.sync
            def _(eng):
                run("sp", eng)


def MM(out, lhsT, rhs, start, stop):
    return lambda e: e.matmul(out, lhsT, rhs, start=start, stop=stop)


def TR(out, in_, ident):
    return lambda e: e.transpose(out, in_, ident)


def DMA(out, in_):
    return lambda e: e.dma_start(out=out, in_=in_)


def ACTF(out, in_, func, bias=None, scale=1.0, accum=None):
    def f(e):
        kw = {}
        if bias is not None:
            kw["bias"] = bias
        if accum is not None:
            kw["accum_out"] = accum
        return e.activation(out, in_, func, scale=scale, **kw)
    return f


def TC(out, in_):
    return lambda e: e.tensor_copy(out, in_)


def TS(out, in0, s1, s2, op0, op1=None):
    if op1 is None:
        return lambda e: e.tensor_scalar(out, in0, s1, None, op0=op0)
    return lambda e: e.tensor_scalar(out, in0, s1, s2, op0=op0, op1=op1)


def TT(out, in0, in1, op):
    return lambda e: e.tensor_tensor(out, in0, in1, op=op)


def STT(out, in0, scalar, in1, op0, op1):
    return lambda e: e.scalar_tensor_tensor(out, in0, scalar, in1, op0=op0, op1=op1)


def MEMSET(ap, v):
    return lambda e: e.memset(ap, v)

def mm_group(S, out_ap, pskey, pairs, reads):
    n = len(pairs)
    for i, (l, r) in enumerate(pairs):
        edge = (i == 0 or i == n - 1)
        S.op("pe", MM(out_ap, l, r, i == 0, i == n - 1),
             reads=reads if edge else (), writes=[pskey] if edge else ())


class SB:
    def __init__(self, big, nwords):
        self.big = big
        self.n = nwords
        self.off = 0
        self.base = 0

    def alloc(self, cols, dtype):
        size = 4 if dtype in (F32, I32) else 2
        words = (cols * size + 3) // 4
        assert self.off + words <= self.n, ("SBUF overflow", self.off, words, self.n)
        ap = self.big[:, self.off:self.off + words]
        self.off += words
        if dtype != F32:
            ap = ap.bitcast(dtype)
        return ap

    def persist(self):
        self.base = self.off

    def reset(self):
        self.off = self.base


def v3(ap, a):
    return ap.rearrange("p (a b) -> p a b", a=a)


def build_program(stop_after=99, dbg=False):
    nc = bass.Bass("TRN2", target_bir_lowering=False)

    def din(name, shape, dt=F32):
        return nc.dram_tensor(name, list(shape), dt, kind="ExternalInput").ap()

    def dscr(name, shape, dt, out=False):
        return nc.dram_tensor(name, list(shape), dt, kind="ExternalOutput" if out else "Internal").ap()

    xT_kv = din("xT_kv", [D, SEQ])
    xT_own = din("xT_own", [D, TOK + 2])
    x_tok = din("x_tok", [TOK, D])
    memT = din("memT", [D, 256])
    w_in = din("w_in", [D, 6144])
    convw = din("convw", [128, 24])
    subln = din("subln", [128, 1])
    lamv = din("lamv", [4, 64])
    w_out = din("w_out", [D, D])
    lnp = din("lnp", [6, D])
    mem_wq = din("mem_wq", [D, D])
    mem_wkv = din("mem_wkv", [D, 2 * D])
    mem_wo = din("mem_wo", [D, D])
    router_w = din("router_w", [D, NE])
    router_b = din("router_b", [NE])
    if stop_after >= 6:
        w_gate = din("w_gate", [NE, D, D])
        w_up = din("w_up", [NE, D, D])
        w_down = din("w_down", [NE, D, D])
    bgu = din("bgu", [128, 2 * NE * 16])
    b_down = din("b_down", [NE, D])
    kaug = din("kaug", [NH, 4, SEQ])
    qaug = din("qaug", [NH, 2, 4, TOK])
    tdiag = din("tdiag", [128, 4 * 512])
    ident_in = din("ident", [128, 128])
    ecap = din("ecap", [128, NE])
    utri = din("utri", [128, 128])

    out = dscr("out", [TOK, D], F32, out=True)
    KT = dscr("KT", [16, 64, SEQ], BF16, out=(dbg and stop_after == 1))
    VV = dscr("VV", [SEQ, 1024], BF16, out=(dbg and stop_after == 1))
    QT = dscr("QT", [16, 64, TOK], BF16, out=(dbg and stop_after == 2))
    OACT = dscr("OACT", [D, TOK], BF16, out=(dbg and stop_after in (2, 3)))
    X1 = dscr("X1", [TOK, D], F32, out=(dbg and stop_after == 4))
    X1T = dscr("X1T", [D, TOK], BF16)
    OM = dscr("OM", [D, TOK], BF16)
    X2 = dscr("X2", [TOK, D], F32, out=(dbg and stop_after == 5))
    XG = dscr("XG", [NROWS, D], BF16)
    YG = dscr("YG", [NROWS, D], F32)
    IDX4 = dscr("IDX4", [TOK, 4], I32, out=(dbg and stop_after == 5))
    G4 = dscr("G4", [TOK, 4], F32, out=(dbg and stop_after == 5))

    with ExitStack() as st:
        S = Sched(nc, st)
        NW = 53200
        big = st.enter_context(nc.sbuf_tensor("big", [128, NW], F32))
        sb = SB(big, NW)
        ps = st.enter_context(nc.psum_tensor("ps", [128, 4096], F32))

        def bank(i, n=512, off=0):
            return ps[:, i * 512 + off:i * 512 + off + n]

        ident = sb.alloc(128, F32)
        identb = sb.alloc(128, BF16)
        ones_b = sb.alloc(128, BF16)
        lam = sb.alloc(1, F32)
        mk_sb = sb.alloc(16 * 256, BF16)
        mv_sb = sb.alloc(2 * 2048, BF16)
        sb.persist()

        S.dma("sp", DMA(ident, ident_in), "c0", writes=["ident"])
        S.op("dve", TC(identb, ident), reads=["ident"], writes=["identb"])
        S.op("dve", MEMSET(ones_b, 1.0), writes=["ones_b"])

        def bcreg(e):
            if "bc" not in S.regs:
                S.regs["bc"] = e.to_reg(NROWS - 1)
            return S.regs["bc"]


        def phase0():
            lv = sb.alloc(4 * 64, F32)
            pr = sb.alloc(2 * 64, F32)
            sm = sb.alloc(2, F32)
            S.dma("sp", DMA(lv, lamv.rearrange("a b -> (a b)").partition_broadcast(128)), "c1", writes=["lv"])
            S.op("dve", TT(pr[:, 0:64], lv[:, 0:64], lv[:, 64:128], ALU.mult), reads=["lv"], writes=["pr0"])
            S.op("dve", TT(pr[:, 64:128], lv[:, 128:192], lv[:, 192:256], ALU.mult), reads=["lv"], writes=["pr1"])
            S.op("dve", lambda e: e.reduce_sum(sm[:, 0:1], pr[:, 0:64], axis=AX.X), reads=["pr0"], writes=["sm0"])
            S.op("dve", lambda e: e.reduce_sum(sm[:, 1:2], pr[:, 64:128], axis=AX.X), reads=["pr1"], writes=["sm1"])
            S.op("act", ACTF(sm, sm, AF.Exp), reads=["sm0", "sm1"], writes=["sme"])
            S.op("dve", TT(lam, sm[:, 0:1], sm[:, 1:2], ALU.subtract), reads=["sme"], writes=["lam0"])
            S.op("dve", TS(lam, lam, LAMBDA_INIT, None, ALU.add), reads=["lam0"], writes=["lam"])
            mT = sb.alloc(16 * 256, BF16)
            mT3 = v3(mT, 16)
            S.dma("pool", DMA(mT3, memT.rearrange("(kc p) m -> p kc m", p=128)), "mT", writes=["mT"])
            wg = [sb.alloc(16 * 512, BF16) for _ in range(2)]
            mk3 = v3(mk_sb, 16)
            mv3 = v3(mv_sb, 2)
            for g in range(8):
                w = wg[g % 2]
                w3 = v3(w, 16)
                wk = "wg%d" % (g % 2)
                S.dma("pool", DMA(w3, mem_wkv[:, g * 512:(g + 1) * 512].rearrange("(kc p) c -> p kc c", p=128)),
                      wk, writes=[wk])
                if g < 4:
                    for s_ in range(4):
                        b = (g * 4 + s_) % 4
                        mm_group(S, bank(b, 256), "ps%d" % b,
                                 [(w3[:, kc, s_ * 128:(s_ + 1) * 128], mT3[:, kc, :]) for kc in range(16)], [wk, "mT"])
                        last = S.op("act", lambda e, b=b, c=g * 4 + s_: e.copy(mk3[:, c, :], bank(b, 256)),
                                    reads=["ps%d" % b], writes=["mk%d" % (g * 4 + s_)])
                else:
                    for m in range(2):
                        b = 4 + (g * 2 + m) % 4
                        mm_group(S, bank(b), "ps%d" % b,
                                 [(mT3[:, kc, m * 128:(m + 1) * 128], w3[:, kc, :]) for kc in range(16)], [wk, "mT"])
                        S.op("dve", TC(mv3[:, m, (g - 4) * 512:(g - 3) * 512], bank(b)),
                             reads=["ps%d" % b], writes=["mv%d_%d" % (m, g)])

        phase0()
        S.barrier()
        sb.reset()

        def phase1a():
            wk_sb = sb.alloc(16 * 1024, BF16)
            wv_sb = sb.alloc(16 * 1024, BF16)
            wk3 = v3(wk_sb, 16)
            wv3 = v3(wv_sb, 16)
            for h in range(2):
                S.dma("pool", DMA(wk3[:, :, h * 512:(h + 1) * 512],
                                  w_in[:, 1024 + h * 512:1024 + (h + 1) * 512].rearrange("(kc p) c -> p kc c", p=128)),
                      "wk", writes=["wk%d" % h])
                S.dma("pool", DMA(wv3[:, :, h * 512:(h + 1) * 512],
                                  w_in[:, 2048 + h * 512:2048 + (h + 1) * 512].rearrange("(kc p) c -> p kc c", p=128)),
                      "wv", writes=["wv%d" % h])
            xs = [sb.alloc(16 * 512, BF16) for _ in range(2)]
            kst = [sb.alloc(512, BF16) for _ in range(4)]
            vst = [sb.alloc(1024, BF16) for _ in range(2)]
            nt = SEQ // 512
            ki = 0
            vi = 0
            for t in range(nt):
                x = xs[t % 2]
                x3 = v3(x, 16)
                xk = "x%d" % (t % 2)
                S.dma("pool", DMA(x3, xT_kv[:, t * 512:(t + 1) * 512].rearrange("(kc p) c -> p kc c", p=128)),
                      xk, writes=[xk])
                for c in range(8):
                    b = c % 4
                    mm_group(S, bank(b), "ps%d" % b,
                             [(wk3[:, kc, c * 128:(c + 1) * 128], x3[:, kc, :]) for kc in range(16)], [xk, "wk0", "wk1"])
                    ks = kst[ki % 4]
                    kk = "kst%d" % (ki % 4)
                    ki += 1
                    S.op("act", lambda e, ks=ks, b=b: e.copy(ks, bank(b)), reads=["ps%d" % b], writes=[kk])
                    for hh in range(2):
                        S.dma("sp", DMA(KT[2 * c + hh, :, t * 512:(t + 1) * 512], ks[hh * 64:(hh + 1) * 64, :]),
                              kk, reads=[kk])
                for s_ in range(4):
                    vs = vst[vi % 2]
                    vk = "vst%d" % (vi % 2)
                    vi += 1
                    for g in range(2):
                        b = 4 + (s_ * 2 + g) % 4
                        mm_group(S, bank(b), "ps%d" % b,
                                 [(x3[:, kc, s_ * 128:(s_ + 1) * 128], wv3[:, kc, g * 512:(g + 1) * 512]) for kc in range(16)],
                                 [xk, "wv0", "wv1"])
                        S.op("dve", TC(vs[:, g * 512:(g + 1) * 512], bank(b)), reads=["ps%d" % b], writes=[vk + "_%d" % g])
                    S.dma("sp", DMA(VV[t * 512 + s_ * 128:t * 512 + (s_ + 1) * 128, :], vs),
                          vk, reads=[vk + "_0", vk + "_1"])

        phase1a()
        S.barrier()
        sb.reset()
        if stop_after <= 1:
            fin = S.dma("sp", DMA(out[0:128, 0:128], ident), "fin", reads=["ident"])
            S.emit(final_waits=[("sp", fin)])
            return nc

        def phase1b():
            wq_sb = sb.alloc(16 * 4096, BF16)
            w3 = v3(wq_sb, 16)
            for g in range(8):
                src = g * 512 if g < 2 else 3072 + (g - 2) * 512
                S.dma("pool", DMA(w3[:, :, g * 512:(g + 1) * 512],
                                  w_in[:, src:src + 512].rearrange("(kc p) c -> p kc c", p=128)),
                      "w1b", writes=["w1b_%d" % g])
            wkeys = ["w1b_%d" % g for g in range(8)]
            cw = sb.alloc(24, F32)
            S.dma("sp", DMA(cw, convw), "c1", writes=["cw"])
            xs = [sb.alloc(16 * 512, BF16) for _ in range(2)]
            qst = [sb.alloc(512, BF16) for _ in range(2)]
            csb = [sb.alloc(512, F32) for _ in range(2)]
            cu = [sb.alloc(512, F32) for _ in range(2)]
            acc = [sb.alloc(512, F32) for _ in range(2)]
            ocv = [sb.alloc(512, BF16) for _ in range(2)]
            qi_ = 0
            ci_ = 0
            for j in range(9):
                h0 = 510 * j
                w = min(512, TOK + 2 - h0)
                x = xs[j % 2]
                x3 = v3(x, 16)
                xk = "x%d" % (j % 2)
                S.dma("pool", DMA(x3[:, :, 0:w], xT_own[:, h0:h0 + w].rearrange("(kc p) c -> p kc c", p=128)),
                      xk, writes=[xk])
                for c in range(8):
                    b = c % 2
                    mm_group(S, bank(b, w), "ps%d" % b,
                             [(w3[:, kc, c * 128:(c + 1) * 128], x3[:, kc, 0:w]) for kc in range(16)], [xk] + wkeys)
                    qs = qst[qi_ % 2]
                    qk = "qst%d" % (qi_ % 2)
                    qi_ += 1
                    S.op("act", ACTF(qs[:, 0:w], bank(b, w), AF.Copy, scale=0.125), reads=["ps%d" % b], writes=[qk])
                    for hh in range(2):
                        S.dma("sp", DMA(QT[2 * c + hh, :, h0:h0 + w - 2], qs[hh * 64:(hh + 1) * 64, 1:w - 1]),
                              qk, reads=[qk])
                for cc in range(8):
                    par = ci_ % 2
                    ci_ += 1
                    bB, bC, bU = 2 + 3 * par, 3 + 3 * par, 4 + 3 * par
                    for (bb, off) in ((bB, 1024), (bC, 2048), (bU, 3072)):
                        mm_group(S, bank(bb, w), "ps%d" % bb,
                                 [(w3[:, kc, off + cc * 128:off + (cc + 1) * 128], x3[:, kc, 0:w]) for kc in range(16)],
                                 [xk] + wkeys)
                    cs, cu_, ac, oc = csb[par], cu[par], acc[par], ocv[par]
                    S.op("act", lambda e, cs=cs, bC=bC, w=w: e.copy(cs[:, 0:w], bank(bC, w)),
                         reads=["ps%d" % bC], writes=["cs%d" % par])
                    S.op("dve", TT(cu_[:, 0:w], cs[:, 0:w], bank(bU, w), ALU.mult),
                         reads=["cs%d" % par, "ps%d" % bU], writes=["cu%d" % par])
                    S.op("dve", TS(ac[:, 0:w - 2], cu_[:, 1:w - 1], cw[:, cc * 3 + 1:cc * 3 + 2], None, ALU.mult),
                         reads=["cu%d" % par, "cw"], writes=["ac%d" % par])
                    S.op("dve", STT(ac[:, 0:w - 2], cu_[:, 0:w - 2], cw[:, cc * 3:cc * 3 + 1], ac[:, 0:w - 2], ALU.mult, ALU.add),
                         reads=["cu%d" % par, "cw", "ac%d" % par], writes=["ac%d" % par])
                    S.op("dve", STT(ac[:, 0:w - 2], cu_[:, 2:w], cw[:, cc * 3 + 2:cc * 3 + 3], ac[:, 0:w - 2], ALU.mult, ALU.add),
                         reads=["cu%d" % par, "cw", "ac%d" % par], writes=["ac%d" % par])
                    S.op("dve", TT(oc[:, 0:w - 2], ac[:, 0:w - 2], bank(bB, w)[:, 1:w - 1], ALU.mult),
                         reads=["ac%d" % par, "ps%d" % bB], writes=["oc%d" % par])
                    S.dma("sp", DMA(OACT[1024 + cc * 128:1024 + (cc + 1) * 128, h0:h0 + w - 2], oc[:, 0:w - 2]),
                          "oc%d" % par, reads=["oc%d" % par])

        phase1b()
        S.barrier()
        sb.reset()
        if stop_after <= 2:
            fin = S.dma("sp", DMA(out[0:128, 0:128], ident), "fin", reads=["ident"])
            S.emit(final_waits=[("sp", fin)])
            return nc

        def phase2():
            T0 = sb.alloc(2048, F32)
            S.dma("sp", DMA(T0, tdiag), "c1", writes=["T0"])
            sl_t = sb.alloc(1, F32)
            S.dma("sp", DMA(sl_t, subln), "c0", writes=["sl_raw"])
            subs = sb.alloc(1, F32)
            S.op("dve", TS(subs, sl_t, 1.0 - LAMBDA_INIT, None, ALU.mult), reads=["sl_raw"], writes=["subs"])
            neglam = sb.alloc(1, F32)
            S.op("dve", TS(neglam, lam, -1.0, None, ALU.mult), writes=["neglam"])
            eps_t = sb.alloc(1, F32)
            S.op("dve", MEMSET(eps_t, EPS), writes=["eps_t"])
            sets = []
            for _ in range(2):
                kx = [sb.alloc(SEQ, BF16) for _ in range(2)]
                vh = sb.alloc(64 * 128, BF16)
                qx = [[sb.alloc(TOK, BF16) for _ in range(2)] for _ in range(2)]
                sets.append((kx, vh, qx))
            Eb = [sb.alloc(512, BF16) for _ in range(4)]
            tmpb = [sb.alloc(512, F32) for _ in range(2)]
            rzt = sb.alloc(512, F32)
            ob = [sb.alloc(512, F32) for _ in range(2)]
            df = sb.alloc(512, F32)
            sq = sb.alloc(512, BF16)
            sd = sb.alloc(512, F32)
            o16 = [sb.alloc(512, BF16) for _ in range(2)]
            dcnt = [0]
            ocnt = [0]
            cnt = [0]

            def loads(h):
                st_ = h % 2
                kx, vh, qx = sets[st_]
                vh3 = v3(vh, 64)
                semn = "set%d" % st_
                for b in range(2):
                    S.dma("sp", DMA(kx[b][0:64, :], KT[2 * h + b]), semn, writes=["Kr%d%d" % (st_, b)])
                    S.dma("pool", DMA(kx[b][64:68, :], kaug[h]), semn, writes=["Ka%d%d" % (st_, b)])
                    for sg in range(2):
                        S.dma("sp", DMA(qx[b][sg][0:64, :], QT[2 * h + b]), semn, writes=["Qr%d%d%d" % (st_, b, sg)])
                        S.dma("pool", DMA(qx[b][sg][64:68, :], qaug[h, sg]), semn, writes=["Qa%d%d%d" % (st_, b, sg)])
                for part in range(4):
                    S.dma("sp", DMA(vh3[:, part * 16:(part + 1) * 16, :],
                                    VV[part * 2048:(part + 1) * 2048, h * 128:(h + 1) * 128].rearrange("(t p) d -> p t d", p=128)),
                          semn, writes=["V%d_%d" % (st_, part)])

            items = [(h, qi, b, kj) for h in range(NH) for qi in range(8) for b in range(2) for kj in range(64)]
            slot = {}
            LA = 2
            deferred = []

            def stage1(n):
                h, qi, b, kj = items[n]
                slope = 2.0 ** (-(h + 1))
                st_ = h % 2
                kx, vh, qx = sets[st_]
                c_ = cnt[0]
                cnt[0] += 1
                slot[n] = c_ % 4
                sbk = 4 + c_ % 4
                E = Eb[c_ % 4]
                ek = "E%d" % (c_ % 4)
                kkeys = ["Kr%d%d" % (st_, b), "Ka%d%d" % (st_, b)]
                diag = (kj < 32 and 4 * qi <= kj < 4 * qi + 4)
                if diag:
                    sg, rows = 0, 64
                else:
                    sg = 1 if (kj < 32 and kj < 4 * qi) else 0
                    rows = 68
                qkeys = ["Qr%d%d%d" % (st_, b, sg), "Qa%d%d%d" % (st_, b, sg)]
                S.op("pe", MM(bank(sbk), kx[b][0:rows, kj * 128:(kj + 1) * 128],
                              qx[b][sg][0:rows, qi * 512:(qi + 1) * 512], True, True),
                     reads=kkeys + qkeys, writes=["ps%d" % sbk])
                if diag:
                    v = kj - 4 * qi
                    tm = tmpb[dcnt[0] % 2]
                    tk = "tmp%d" % (dcnt[0] % 2)
                    dcnt[0] += 1
                    S.op("dve", STT(tm, T0[:, v * 512:(v + 1) * 512], slope, bank(sbk), ALU.mult, ALU.add),
                         reads=["T0", "ps%d" % sbk], writes=[tk])
                    S.op("act", ACTF(E, tm, AF.Exp), reads=[tk], writes=[ek])
                else:
                    S.op("act", ACTF(E, bank(sbk), AF.Exp), reads=["ps%d" % sbk], writes=[ek])

            def epi_b(h, qi):
                c_ = cnt[0]
                cnt[0] += 1
                sbk = 4 + c_ % 4
                S.op("pe", MM(bank(sbk), ones_b, sq, True, True), reads=["sq", "ones_b"], writes=["ps%d" % sbk])
                S.op("act", ACTF(sd, bank(sbk), AF.Sqrt, bias=eps_t[:, 0:1], scale=1.0 / 128.0),
                     reads=["ps%d" % sbk, "eps_t"], writes=["sd"])
                S.op("dve", lambda e: e.reciprocal(sd, sd), reads=["sd"], writes=["sd"])
                S.op("dve", TT(df, df, sd, ALU.mult), reads=["df", "sd"], writes=["df"])
                o_ = o16[ocnt[0] % 2]
                ok_ = "o16_%d" % (ocnt[0] % 2)
                ocnt[0] += 1
                S.op("dve", TS(o_, df, subs[:, 0:1], None, ALU.mult), reads=["df", "subs"], writes=[ok_])
                S.dma("sp", DMA(OACT[h * 128:(h + 1) * 128, qi * 512:(qi + 1) * 512], o_), ok_, reads=[ok_])

            def stage2(m, n):
                h, qi, b, kj = items[m]
                st_ = h % 2
                kx, vh, qx = sets[st_]
                vh3 = v3(vh, 64)
                vkeys = ["V%d_%d" % (st_, part) for part in range(4)]
                E = Eb[slot[m]]
                ek = "E%d" % slot[m]
                Ob, Zb = 2 * b, 2 * b + 1
                edge = (kj == 0 or kj == 63)
                S.op("pe", MM(bank(Ob), vh3[:, kj, :], E, kj == 0, kj == 63),
                     reads=[ek] + vkeys, writes=["ps%d" % Ob] if edge else ())
                S.op("pe", MM(bank(Zb), ones_b, E, kj == 0, kj == 63),
                     reads=[ek, "ones_b"], writes=["ps%d" % Zb] if edge else ())
                if kj == 63:
                    S.op("dve", lambda e, Zb=Zb: e.reciprocal(rzt, bank(Zb)), reads=["ps%d" % Zb], writes=["rzt"])
                    S.op("dve", TT(ob[b], bank(Ob), rzt, ALU.mult), reads=["ps%d" % Ob, "rzt"], writes=["ob%d" % b])
                    if b == 1:
                        S.op("dve", STT(df, ob[1], neglam[:, 0:1], ob[0], ALU.mult, ALU.add),
                             reads=["ob0", "ob1", "neglam"], writes=["df"])
                        S.op("act", ACTF(sq, df, AF.Square), reads=["df"], writes=["sq"])
                        deferred.append((n + 6, h, qi))

            loads(0)
            nit = len(items)
            for n in range(nit + LA + 8):
                if n < nit:
                    h, qi, b, kj = items[n]
                    if (qi, b, kj) == (0, 0, 0) and h + 1 < NH:
                        loads(h + 1)
                    stage1(n)
                m = n - LA
                if 0 <= m < nit:
                    stage2(m, n)
                while deferred and deferred[0][0] <= n:
                    _, hh, qq = deferred.pop(0)
                    epi_b(hh, qq)
            assert not deferred

        phase2()
        S.barrier()
        sb.reset()
        if stop_after <= 3:
            fin = S.dma("sp", DMA(out[0:128, 0:128], ident), "fin", reads=["ident"])
            S.emit(final_waits=[("sp", fin)])
            return nc

        def ln_rows(y, gb, bb, st4, junk, tag):
            S.op("dve", lambda e: e.reduce_sum(st4[:, 0:1], y, axis=AX.X), reads=[tag], writes=[tag + "s0"])
            S.op("dve", TS(st4[:, 1:2], st4[:, 0:1], -1.0 / D, None, ALU.mult), reads=[tag + "s0"], writes=[tag + "s1"])
            S.op("act", ACTF(junk, y, AF.Square, bias=st4[:, 1:2], accum=st4[:, 2:3]),
                 reads=[tag, tag + "s1"], writes=[tag + "s2", "junk"])
            S.op("dve", TS(st4[:, 3:4], st4[:, 2:3], 1.0 / D, EPS, ALU.mult, ALU.add), reads=[tag + "s2"], writes=[tag + "s3"])
            S.op("act", lambda e: e.sqrt(st4[:, 3:4], st4[:, 3:4]), reads=[tag + "s3"], writes=[tag + "s3"])
            S.op("dve", lambda e: e.reciprocal(st4[:, 3:4], st4[:, 3:4]), reads=[tag + "s3"], writes=[tag + "s3"])
            S.op("dve", TS(y, y, st4[:, 1:2], st4[:, 3:4], ALU.add, ALU.mult), reads=[tag, tag + "s1", tag + "s3"], writes=[tag])
            S.op("pool", TT(y, y, gb, ALU.mult), reads=[tag, "gb"], writes=[tag])
            S.op("pool", TT(y, y, bb, ALU.add), reads=[tag, "bb"], writes=[tag])

        def proj_ln(inT, Wd, resid, gi, post, extra_alloc=None):
            W = sb.alloc(16 * D, BF16)
            W3 = v3(W, 16)
            for g in range(4):
                S.dma("pool", DMA(W3[:, :, g * 512:(g + 1) * 512],
                                  Wd[:, g * 512:(g + 1) * 512].rearrange("(kc p) c -> p kc c", p=128)),
                      "wp", writes=["W%d" % g])
            wkeys = ["W%d" % g for g in range(4)]
            gb = sb.alloc(D, F32)
            bb = sb.alloc(D, F32)
            S.dma("sp", DMA(gb, lnp[gi].partition_broadcast(128)), "c0", writes=["gb"])
            S.dma("sp", DMA(bb, lnp[gi + 1].partition_broadcast(128)), "c1", writes=["bb"])
            ins = [sb.alloc(16 * 512, BF16) for _ in range(2)]
            xt = [sb.alloc(D, F32) for _ in range(2)]
            ys = [sb.alloc(D, F32) for _ in range(2)]
            st4 = [sb.alloc(4, F32) for _ in range(2)]
            junk = sb.alloc(D, BF16)
            ctx = extra_alloc() if extra_alloc else None
            def ld_in(g):
                S.dma("sp", DMA(v3(ins[g % 2], 16), inT[:, g * 512:(g + 1) * 512].rearrange("(kc p) t -> p kc t", p=128)),
                      "in%d" % (g % 2), writes=["in%d" % (g % 2)])

            def ld_x(tt):
                S.dma("sp", DMA(xt[tt % 2], resid[tt * 128:(tt + 1) * 128, :]), "xt%d" % (tt % 2), writes=["xt%d" % (tt % 2)])

            ld_in(0)
            ld_x(0)
            for grp in range(8):
                i3 = v3(ins[grp % 2], 16)
                ik = "in%d" % (grp % 2)
                if grp + 1 < 8:
                    ld_in(grp + 1)
                for s_ in range(4):
                    t = grp * 4 + s_
                    par = t % 2
                    x_, y_ = xt[par], ys[par]
                    if t + 1 < 32:
                        ld_x(t + 1)
                    for cg in range(4):
                        mm_group(S, bank(cg), "ps%d" % cg,
                                 [(i3[:, kc, s_ * 128:(s_ + 1) * 128], W3[:, kc, cg * 512:(cg + 1) * 512]) for kc in range(16)],
                                 [ik] + wkeys)
                    yk = "y%d" % par
                    for cg in range(4):
                        S.op("dve", STT(y_[:, cg * 512:(cg + 1) * 512], x_[:, cg * 512:(cg + 1) * 512], ALPHA, bank(cg),
                                        ALU.mult, ALU.add),
                             reads=["xt%d" % par, "ps%d" % cg], writes=[yk])
                    ln_rows(y_, gb, bb, st4[par], junk, yk)
                    post(ctx, grp, s_, t, y_, yk)

        def phase3():
            def extra():
                return {"x1b": [sb.alloc(D, BF16) for _ in range(2)],
                        "x1T": [sb.alloc(16 * 512, BF16) for _ in range(2)]}

            def post(ctx, grp, s_, t, y_, yk):
                par = t % 2
                S.dma("sp", DMA(X1[t * 128:(t + 1) * 128, :], y_), "x1o%d" % par, reads=[yk])
                xb_ = ctx["x1b"][par]
                S.op("act", lambda e: e.copy(xb_, y_), reads=[yk], writes=["x1b%d" % par])
                psb = ps[:, 2048:3072].bitcast(BF16)
                for kc in range(16):
                    S.op("pe", TR(psb[:, kc * 128:(kc + 1) * 128], xb_[:, kc * 128:(kc + 1) * 128], identb),
                         reads=["x1b%d" % par, "identb"], writes=["pst"] if kc in (0, 15) else ())
                xT3 = v3(ctx["x1T"][grp % 2], 16)
                tk = "x1T%d" % (grp % 2)
                S.op("act", lambda e: e.copy(xT3[:, :, s_ * 128:(s_ + 1) * 128], v3(psb, 16)),
                     reads=["pst"] + ([tk] if s_ > 0 else []), writes=[tk])
                if s_ == 3:
                    S.dma("sp", DMA(X1T[:, grp * 512:(grp + 1) * 512].rearrange("(kc p) t -> p kc t", p=128), xT3),
                          tk, reads=[tk])

            proj_ln(OACT, w_out, x_tok, 0, post, extra)

        phase3()
        S.barrier()
        sb.reset()
        if stop_after <= 4:
            fin = S.dma("sp", DMA(out[0:128, 0:128], ident), "fin", reads=["ident"])
            S.emit(final_waits=[("sp", fin)])
            return nc

        def phase4a():
            W = sb.alloc(16 * D, BF16)
            W3 = v3(W, 16)
            for g in range(4):
                S.dma("pool", DMA(W3[:, :, g * 512:(g + 1) * 512],
                                  mem_wq[:, g * 512:(g + 1) * 512].rearrange("(kc p) c -> p kc c", p=128)),
                      "wp", writes=["W%d" % g])
            wkeys = ["W%d" % g for g in range(4)]
            mk3 = v3(mk_sb, 16)
            mv3 = v3(mv_sb, 2)
            ins = [sb.alloc(16 * 512, BF16) for _ in range(2)]
            qT = sb.alloc(16 * 512, BF16)
            qT3 = v3(qT, 16)
            omT = [sb.alloc(16 * 512, BF16) for _ in range(2)]
            st4 = [sb.alloc(4, F32) for _ in range(2)]
            pe_ = [sb.alloc(256, F32) for _ in range(2)]
            pn = [sb.alloc(256, BF16) for _ in range(2)]
            pT = [sb.alloc(256, BF16) for _ in range(2)]
            scale = 512.0 ** -0.5
            psb = ps[:, 3072:3584].bitcast(BF16)
            it = 0
            for grp in range(8):
                i3 = v3(ins[grp % 2], 16)
                ik = "in%d" % (grp % 2)
                S.dma("sp", DMA(i3, X1T[:, grp * 512:(grp + 1) * 512].rearrange("(kc p) t -> p kc t", p=128)), ik, writes=[ik])
                for c in range(16):
                    b = c % 2
                    mm_group(S, bank(b), "ps%d" % b,
                             [(W3[:, kc, c * 128:(c + 1) * 128], i3[:, kc, :]) for kc in range(16)], [ik] + wkeys)
                    if c % 2 == 0:
                        S.op("act", lambda e, c=c, b=b: e.copy(qT3[:, c, :], bank(b)), reads=["ps%d" % b], writes=["qT%d" % c])
                    else:
                        S.op("dve", TC(qT3[:, c, :], bank(b)), reads=["ps%d" % b], writes=["qT%d" % c])
                o3 = v3(omT[grp % 2], 16)
                ok_ = "omT%d" % (grp % 2)
                for s_ in range(4):
                    for hd in range(4):
                        par = it % 2
                        it += 1
                        sbk = 2 + par
                        mm_group(S, bank(sbk, 256), "ps%d" % sbk,
                                 [(qT3[:, hd * 4 + c, s_ * 128:(s_ + 1) * 128], mk3[:, hd * 4 + c, :]) for c in range(4)],
                                 ["qT%d" % (hd * 4 + c) for c in range(4)])
                        s4 = st4[par]
                        sk = "s4_%d" % par
                        S.op("dve", lambda e, s4=s4, sbk=sbk: e.reduce_max(s4[:, 0:1], bank(sbk, 256), axis=AX.X),
                             reads=["ps%d" % sbk], writes=[sk + "a"])
                        S.op("dve", TS(s4[:, 1:2], s4[:, 0:1], -scale, None, ALU.mult), reads=[sk + "a"], writes=[sk + "b"])
                        S.op("act", ACTF(pe_[par], bank(sbk, 256), AF.Exp, bias=s4[:, 1:2], scale=scale, accum=s4[:, 2:3]),
                             reads=["ps%d" % sbk, sk + "b"], writes=["pe%d" % par, sk + "c"])
                        S.op("dve", lambda e, s4=s4: e.reciprocal(s4[:, 3:4], s4[:, 2:3]), reads=[sk + "c"], writes=[sk + "d"])
                        S.op("dve", TS(pn[par], pe_[par], s4[:, 3:4], None, ALU.mult), reads=["pe%d" % par, sk + "d"], writes=["pn%d" % par])
                        for mt in range(2):
                            S.op("pe", TR(psb[:, (par * 2 + mt) * 128:(par * 2 + mt + 1) * 128], pn[par][:, mt * 128:(mt + 1) * 128], identb),
                                 reads=["pn%d" % par, "identb"], writes=["pstb%d" % par] if mt in (0, 1) else ())
                        S.op("act", lambda e, par=par: e.copy(pT[par], psb[:, par * 256:(par + 1) * 256]),
                             reads=["pstb%d" % par], writes=["pT%d" % par])
                        obk = 4 + par
                        for dc in range(4):
                            for mt in range(2):
                                S.op("pe", MM(bank(obk, 128, dc * 128), mv3[:, mt, hd * 512 + dc * 128:hd * 512 + (dc + 1) * 128],
                                              pT[par][:, mt * 128:(mt + 1) * 128], mt == 0, mt == 1),
                                     reads=["pT%d" % par], writes=["ps%d" % obk] if (dc, mt) in ((0, 0), (3, 1)) else ())
                        S.op("dve", lambda e, o3=o3, hd=hd, s_=s_, obk=obk: e.tensor_copy(
                            o3[:, hd * 4:(hd + 1) * 4, s_ * 128:(s_ + 1) * 128], v3(bank(obk), 4)),
                             reads=["ps%d" % obk] + ([ok_] if (s_, hd) != (0, 0) else []), writes=[ok_])
                S.dma("sp", DMA(OM[:, grp * 512:(grp + 1) * 512].rearrange("(kc p) t -> p kc t", p=128), o3), ok_, reads=[ok_])

        phase4a()
        S.barrier()
        sb.reset()
        if stop_after <= 4.5:
            fin = S.dma("sp", DMA(out[0:128, 0:128], ident), "fin", reads=["ident"])
            S.emit(final_waits=[("sp", fin)])
            return nc

        def phase4b():
            def extra():
                c = {}
                c["x2b"] = [sb.alloc(D, BF16) for _ in range(2)]
                c["x2T"] = sb.alloc(16 * 128, F32)
                c["rw"] = sb.alloc(16 * NE, F32)
                c["rb"] = sb.alloc(NE, F32)
                c["ut"] = sb.alloc(128, F32)
                c["onesf"] = sb.alloc(128, F32)
                c["ec"] = sb.alloc(NE, F32)
                c["base"] = sb.alloc(NE, F32)
                for nm in ("lg", "mask", "ex", "G", "rowid", "keyv", "oh"):
                    c[nm] = sb.alloc(NE, F32)
                c["mx"] = sb.alloc(8, F32)
                c["kmx"] = sb.alloc(8, F32)
                c["sm"] = sb.alloc(4, F32)
                c["rows4"] = sb.alloc(4, F32)
                c["idx4"] = [sb.alloc(4, I32) for _ in range(2)]
                c["g4"] = [sb.alloc(4, F32) for _ in range(2)]
                S.dma("sp", DMA(v3(c["rw"], 16), router_w.rearrange("(kc p) e -> p kc e", p=128)), "c0", writes=["rw"])
                S.dma("sp", DMA(c["rb"], router_b.partition_broadcast(128)), "c1", writes=["rb"])
                S.dma("sp", DMA(c["ut"], utri), "c0", writes=["ut"])
                S.dma("sp", DMA(c["ec"], ecap), "c1", writes=["ec"])
                S.op("dve", MEMSET(c["onesf"], 1.0), writes=["onesf"])
                S.op("dve", MEMSET(c["base"], 0.0), writes=["base"])
                return c

            def post(c, grp, s_, t, y_, yk):
                par = t % 2
                S.dma("sp", DMA(X2[t * 128:(t + 1) * 128, :], y_), "x2o%d" % par, reads=[yk])
                xb_ = c["x2b"][par]
                xbk = "x2b%d" % par
                S.op("act", lambda e: e.copy(xb_, y_), reads=[yk], writes=[xbk])
                for kc in range(16):
                    S.op("pe", TR(ps[:, 2048 + kc * 128:2048 + (kc + 1) * 128], y_[:, kc * 128:(kc + 1) * 128], ident),
                         reads=[yk, "ident"], writes=["pst", "psp", "psc"] if kc in (0, 15) else ())
                x2T = c["x2T"]
                S.op("act", lambda e: e.copy(x2T[:, 0:1024], ps[:, 2048:3072]), reads=["pst"], writes=["x2Ta"])
                S.op("dve", TC(x2T[:, 1024:2048], ps[:, 3072:4096]), reads=["pst"], writes=["x2Tb"])
                rw3 = v3(c["rw"], 16)
                x2T3 = v3(x2T, 16)
                mm_group(S, ps[:, 2048:2048 + NE], "pst",
                         [(x2T3[:, kc, :], rw3[:, kc, :]) for kc in range(16)], ["x2Ta", "x2Tb", "rw"])
                lg, mask, ex, G, rowid, keyv, oh = (c[n] for n in ("lg", "mask", "ex", "G", "rowid", "keyv", "oh"))
                mx, kmx, sm, rows4 = c["mx"], c["kmx"], c["sm"], c["rows4"]
                S.op("dve", TT(lg, ps[:, 2048:2048 + NE], c["rb"], ALU.add), reads=["pst", "rb"], writes=["lg"])
                S.op("dve", lambda e: e.max(out=mx, in_=lg), reads=["lg"], writes=["mx"])
                S.op("dve", TS(mask, lg, mx[:, 3:4], None, ALU.is_ge), reads=["lg", "mx"], writes=["mask"])
                S.op("dve", TS(sm[:, 0:1], mx[:, 0:1], -1.0, None, ALU.mult), reads=["mx"], writes=["sm0"])
                S.op("act", ACTF(ex, lg, AF.Exp, bias=sm[:, 0:1]), reads=["lg", "sm0"], writes=["ex"])
                S.op("dve", TT(ex, ex, mask, ALU.mult), reads=["ex", "mask"], writes=["ex"])
                S.op("dve", lambda e: e.reduce_sum(sm[:, 1:2], ex, axis=AX.X), reads=["ex"], writes=["sm1"])
                S.op("dve", lambda e: e.reciprocal(sm[:, 1:2], sm[:, 1:2]), reads=["sm1"], writes=["sm1"])
                S.op("dve", TS(G, ex, sm[:, 1:2], None, ALU.mult), reads=["ex", "sm1"], writes=["G"])
                S.op("pe", MM(ps[:, 2560:2560 + NE], c["ut"], mask, True, True), reads=["ut", "mask"], writes=["psp"])
                S.op("pe", MM(ps[:, 2560 + NE:2560 + 2 * NE], c["onesf"], mask, True, True), reads=["onesf", "mask"], writes=["psc"])
                S.op("dve", TT(rowid, ps[:, 2560:2560 + NE], c["base"], ALU.add), reads=["psp", "base"], writes=["rowid"])
                S.op("dve", TT(c["base"], c["base"], ps[:, 2560 + NE:2560 + 2 * NE], ALU.add), reads=["psc", "base", "rowid"], writes=["base"])
                S.op("dve", TS(oh, rowid, float(CAP), None, ALU.is_lt), reads=["rowid"], writes=["oh"])
                S.op("dve", TT(oh, oh, mask, ALU.mult), reads=["oh", "mask"], writes=["oh"])
                S.op("dve", TT(rowid, rowid, c["ec"], ALU.add), reads=["rowid", "ec"], writes=["rowid"])
                S.op("dve", TS(keyv, rowid, -1.0, BIGROW, ALU.mult, ALU.add), reads=["rowid"], writes=["keyv"])
                S.op("dve", TT(keyv, keyv, oh, ALU.mult), reads=["keyv", "oh"], writes=["keyv"])
                S.op("dve", lambda e: e.max(out=kmx, in_=keyv), reads=["keyv"], writes=["kmx"])
                S.op("dve", TS(rows4, kmx[:, 0:4], -1.0, BIGROW, ALU.mult, ALU.add), reads=["kmx"], writes=["rows4"])
                i4 = c["idx4"][par]
                g4 = c["g4"][par]
                ik4 = "idx4_%d" % par
                gk4 = "g4_%d" % par
                S.op("dve", TC(i4, rows4), reads=["rows4"], writes=[ik4])
                for j in range(4):
                    S.op("dve", TS(oh, keyv, kmx[:, j:j + 1], None, ALU.is_equal), reads=["keyv", "kmx"], writes=["oh"])
                    S.op("dve", TT(oh, oh, G, ALU.mult), reads=["oh", "G"], writes=["oh"])
                    S.op("dve", lambda e, j=j: e.reduce_sum(g4[:, j:j + 1], oh, axis=AX.X), reads=["oh"], writes=[gk4 + "_%d" % j])
                for j in range(4):
                    S.dma("pool", lambda e, j=j: e.indirect_dma_start(
                        out=XG, out_offset=bass.IndirectOffsetOnAxis(ap=i4[:, j:j + 1], axis=0),
                        in_=xb_, in_offset=None, bounds_check=bcreg(e), oob_is_err=False),
                          "scat%d_%d" % (par, j), reads=[xbk, ik4])
                S.dma("sp", DMA(IDX4[t * 128:(t + 1) * 128, :], i4), "i4o%d" % par, reads=[ik4])
                S.dma("sp", DMA(G4[t * 128:(t + 1) * 128, :], g4), "g4o%d" % par, reads=[gk4 + "_%d" % j for j in range(4)])

            proj_ln(OM, mem_wo, X1, 2, post, extra)

        phase4b()
        S.barrier()
        sb.reset()
        if stop_after <= 5:
            fin = S.dma("sp", DMA(out[0:128, 0:128], ident), "fin", reads=["ident"])
            S.emit(final_waits=[("sp", fin)])
            return nc

        NST = CAP // 128
        R2 = CAP - 512

        def phase5():
            bg_sb = sb.alloc(2 * NE * 16, F32)
            S.dma("sp", DMA(bg_sb, bgu), "c0", writes=["bgu"])
            xgT = [sb.alloc(16 * CAP, BF16) for _ in range(2)]
            wt = [sb.alloc(16 * 512, BF16) for _ in range(4)]
            actT = sb.alloc(16 * CAP, BF16)
            a3 = v3(actT, 16)
            bd = [sb.alloc(D, F32) for _ in range(2)]
            xgt = [sb.alloc(D, BF16) for _ in range(2)]
            gs = [sb.alloc(CAP, F32) for _ in range(2)]
            sg = [sb.alloc(CAP, F32) for _ in range(2)]
            us = [sb.alloc(CAP, F32) for _ in range(2)]
            ysb = [sb.alloc(512, F32) for _ in range(4)]
            psb = ps[:, 3072:4096].bitcast(BF16)
            wi = 0
            xi = 0
            yi = 0
            fci = 0
            cst = {"xi": 0}

            def xg_load(e):
                ep = e % 2
                S.dma("sp", DMA(bd[ep], b_down[e].partition_broadcast(128)), "bd%d" % ep, writes=["bd%d" % ep])
                x3 = v3(xgT[ep], 16)
                xk = "xgT%d" % ep
                xi = cst["xi"]
                for st_ in range(NST):
                    xt_ = xgt[xi % 2]
                    xtk = "xgt%d" % (xi % 2)
                    xi += 1
                    r0 = e * CAP + st_ * 128
                    S.dma("sp", DMA(xt_, XG[r0:r0 + 128, :]), xtk, writes=[xtk])
                    for kc in range(16):
                        S.op("pe", TR(psb[:, kc * 128:(kc + 1) * 128], xt_[:, kc * 128:(kc + 1) * 128], identb),
                             reads=[xtk, "identb"], writes=["ps6", "ps7"] if kc in (0, 15) else ())
                    eng = "act" if st_ % 2 == 0 else "dve"
                    S.op(eng, TC(x3[:, :, st_ * 128:(st_ + 1) * 128], v3(psb, 16)) if eng == "dve" else
                         (lambda e_, x3=x3, st_=st_: e_.copy(x3[:, :, st_ * 128:(st_ + 1) * 128], v3(psb, 16))),
                         reads=["ps6", "ps7"] + ([xk] if st_ > 0 else []), writes=[xk])
                cst["xi"] = xi

            xg_load(0)
            for e in range(NE):
                ep = e % 2
                x3 = v3(xgT[ep], 16)
                xk = "xgT%d" % ep
                for fg in range(4):
                    wg = wt[wi % 4]
                    wgk = "wt%d" % (wi % 4)
                    wi += 1
                    wu = wt[wi % 4]
                    wuk = "wt%d" % (wi % 4)
                    wi += 1
                    S.dma("pool", DMA(v3(wg, 16), w_gate[e][:, fg * 512:(fg + 1) * 512].rearrange("(kc p) c -> p kc c", p=128)),
                          wgk, writes=[wgk])
                    S.dma("pool", DMA(v3(wu, 16), w_up[e][:, fg * 512:(fg + 1) * 512].rearrange("(kc p) c -> p kc c", p=128)),
                          wuk, writes=[wuk])
                    wg3, wu3 = v3(wg, 16), v3(wu, 16)
                    for fs in range(4):
                        fc = fg * 4 + fs
                        set_ = fci % 2
                        fci += 1
                        g0, g1, u0, u1 = 4 * set_, 4 * set_ + 1, 4 * set_ + 2, 4 * set_ + 3
                        if set_ == 1:
                            g0, g1, u0, u1 = 4, 5, 6, 7
                        mm_group(S, bank(g0), "ps%d" % g0,
                                 [(wg3[:, kc, fs * 128:(fs + 1) * 128], x3[:, kc, 0:512]) for kc in range(16)], [wgk, xk])
                        mm_group(S, bank(g1, R2), "ps%d" % g1,
                                 [(wg3[:, kc, fs * 128:(fs + 1) * 128], x3[:, kc, 512:CAP]) for kc in range(16)], [wgk, xk])
                        mm_group(S, bank(u0), "ps%d" % u0,
                                 [(wu3[:, kc, fs * 128:(fs + 1) * 128], x3[:, kc, 0:512]) for kc in range(16)], [wuk, xk])
                        mm_group(S, bank(u1, R2), "ps%d" % u1,
                                 [(wu3[:, kc, fs * 128:(fs + 1) * 128], x3[:, kc, 512:CAP]) for kc in range(16)], [wuk, xk])
                        gs_, sg_, us_ = gs[set_], sg[set_], us[set_]
                        bgc = bg_sb[:, e * 16 + fc:e * 16 + fc + 1]
                        buc = bg_sb[:, NE * 16 + e * 16 + fc:NE * 16 + e * 16 + fc + 1]
                        S.op("dve", TS(gs_, ps[:, g0 * 512:g0 * 512 + CAP], bgc, 7.0, ALU.add, ALU.min),
                             reads=["ps%d" % g0, "ps%d" % g1, "bgu"], writes=["gs%d" % set_])
                        S.op("act", ACTF(sg_, gs_, AF.Sigmoid, scale=1.702), reads=["gs%d" % set_], writes=["sg%d" % set_])
                        S.op("dve", TS(us_, ps[:, u0 * 512:u0 * 512 + CAP], buc, 7.0, ALU.add, ALU.min),
                             reads=["ps%d" % u0, "ps%d" % u1, "bgu"], writes=["us%d" % set_])
                        S.op("dve", TS(us_, us_, -7.0, 1.0, ALU.max, ALU.add), reads=["us%d" % set_], writes=["us%d" % set_])
                        S.op("dve", TT(gs_, gs_, sg_, ALU.mult), reads=["gs%d" % set_, "sg%d" % set_], writes=["gs%d" % set_])
                        S.op("dve", TT(a3[:, fc, :], gs_, us_, ALU.mult), reads=["gs%d" % set_, "us%d" % set_], writes=["actT%d" % fc])
                akeys = ["actT%d" % fc for fc in range(16)]
                if e + 1 < NE:
                    xg_load(e + 1)
                for dg in range(4):
                    wd = wt[wi % 4]
                    wdk = "wt%d" % (wi % 4)
                    wi += 1
                    S.dma("pool", DMA(v3(wd, 16), w_down[e][:, dg * 512:(dg + 1) * 512].rearrange("(fc p) c -> p fc c", p=128)),
                          wdk, writes=[wdk])
                    wd3 = v3(wd, 16)
                    for st_ in range(NST):
                        b = yi % 8
                        mm_group(S, bank(b), "ps%d" % b,
                                 [(a3[:, fc, st_ * 128:(st_ + 1) * 128], wd3[:, fc, :]) for fc in range(16)], [wdk] + akeys)
                        y_ = ysb[yi % 4]
                        yk = "ysb%d" % (yi % 4)
                        yi += 1
                        S.op("dve", TT(y_, bank(b), bd[ep][:, dg * 512:(dg + 1) * 512], ALU.add),
                             reads=["ps%d" % b, "bd%d" % ep], writes=[yk])
                        r0 = e * CAP + st_ * 128
                        S.dma("sp", DMA(YG[r0:r0 + 128, dg * 512:(dg + 1) * 512], y_), yk, reads=[yk])

        phase5()
        S.barrier()
        sb.reset()

        def phase6():
            gb = sb.alloc(D, F32)
            bb = sb.alloc(D, F32)
            S.dma("sp", DMA(gb, lnp[4].partition_broadcast(128)), "c0", writes=["gb"])
            S.dma("sp", DMA(bb, lnp[5].partition_broadcast(128)), "c1", writes=["bb"])
            yj = [sb.alloc(4 * D, F32) for _ in range(2)]
            x2t = [sb.alloc(D, F32) for _ in range(2)]
            ys = [sb.alloc(D, F32) for _ in range(2)]
            i4s = [sb.alloc(4, I32) for _ in range(2)]
            g4s = [sb.alloc(4, F32) for _ in range(2)]
            st4 = [sb.alloc(4, F32) for _ in range(2)]
            junk = sb.alloc(D, BF16)
            last = {}

            def fetch(t):
                par = t % 2
                i4, g4, x_, yy = i4s[par], g4s[par], x2t[par], yj[par]
                S.dma("sp", DMA(i4, IDX4[t * 128:(t + 1) * 128, :]), "i4l%d" % par, writes=["i4_%d" % par])
                S.dma("sp", DMA(g4, G4[t * 128:(t + 1) * 128, :]), "g4l%d" % par, writes=["g4_%d" % par])
                S.dma("sp", DMA(x_, X2[t * 128:(t + 1) * 128, :]), "x2l%d" % par, writes=["x2_%d" % par])
                S.op("pool", MEMSET(yy, 0.0), writes=["yj%d_%d" % (par, j) for j in range(4)])
                for j in range(4):
                    S.dma("pool", lambda e, j=j, yy=yy, i4=i4: e.indirect_dma_start(
                        out=yy[:, j * D:(j + 1) * D], out_offset=None, in_=YG,
                        in_offset=bass.IndirectOffsetOnAxis(ap=i4[:, j:j + 1], axis=0),
                        bounds_check=bcreg(e), oob_is_err=False),
                          "gat%d_%d" % (par, j), reads=["i4_%d" % par], writes=["yj%d_%d" % (par, j)])

            fetch(0)
            for t in range(TOK // 128):
                par = t % 2
                i4, g4, x_, y_, yy = i4s[par], g4s[par], x2t[par], ys[par], yj[par]
                if t + 1 < TOK // 128:
                    fetch(t + 1)
                yk = "y%d" % par
                S.op("act", ACTF(y_, x_, AF.Copy, scale=ALPHA), reads=["x2_%d" % par], writes=[yk])
                for j in range(4):
                    S.op("dve", STT(y_, yy[:, j * D:(j + 1) * D], g4[:, j:j + 1], y_, ALU.mult, ALU.add),
                         reads=["yj%d_%d" % (par, j), "g4_%d" % par, yk], writes=[yk])
                ln_rows(y_, gb, bb, st4[par], junk, yk)
                last[par] = S.dma("sp", DMA(out[t * 128:(t + 1) * 128, :], y_), "outo%d" % par, reads=[yk])
            return list(last.values())

        finals = phase6()
        S.emit(final_waits=[("sp", d) for d in finals])
    return nc


def _host_inputs(inp, ncores=8):
    x = np.asarray(inp["x"], np.float32)
    mem = np.asarray(inp["mem"], np.float32)
    common = {}
    common["w_in"] = np.ascontiguousarray(inp["w_in"][0])
    cw = np.asarray(inp["conv_w"][0], np.float32)
    common["convw"] = np.ascontiguousarray(cw.reshape(3, 8, 128).transpose(2, 1, 0).reshape(128, 24))
    common["subln"] = np.ascontiguousarray(np.asarray(inp["attn_subln_w"][0], np.float32).reshape(128, 1))
    common["lamv"] = np.ascontiguousarray(np.stack([inp["lambda_q1"][0], inp["lambda_k1"][0],
                                                    inp["lambda_q2"][0], inp["lambda_k2"][0]]).astype(np.float32))
    common["w_out"] = np.ascontiguousarray(inp["w_out"][0])
    common["lnp"] = np.ascontiguousarray(np.stack([inp["ln1_g"][0], inp["ln1_b"][0], inp["ln2_g"][0],
                                                   inp["ln2_b"][0], inp["ln3_g"][0], inp["ln3_b"][0]]).astype(np.float32))
    common["mem_wq"] = np.ascontiguousarray(inp["mem_wq"][0])
    common["mem_wkv"] = np.ascontiguousarray(inp["mem_wkv"][0])
    common["mem_wo"] = np.ascontiguousarray(inp["mem_wo"][0])
    common["router_w"] = np.ascontiguousarray(inp["router_w"][0])
    common["router_b"] = np.ascontiguousarray(inp["router_b"][0])
    if "w_gate" in inp:
        common["w_gate"] = np.ascontiguousarray(inp["w_gate"][0])
        common["w_up"] = np.ascontiguousarray(inp["w_up"][0])
        common["w_down"] = np.ascontiguousarray(inp["w_down"][0])
    bg = np.asarray(inp["b_gate"][0], np.float32).reshape(NE, 16, 128).transpose(2, 0, 1).reshape(128, NE * 16)
    bu = np.asarray(inp["b_up"][0], np.float32).reshape(NE, 16, 128).transpose(2, 0, 1).reshape(128, NE * 16)
    common["bgu"] = np.ascontiguousarray(np.concatenate([bg, bu], axis=1))
    common["b_down"] = np.ascontiguousarray(inp["b_down"][0])
    kr = np.arange(128)[:, None, None]
    vv = np.arange(4)[None, :, None]
    qr = np.arange(512)[None, None, :]
    common["tdiag"] = np.ascontiguousarray((-np.abs(vv * 128 + kr - qr)).astype(np.float32).reshape(128, 2048))
    common["ident"] = np.eye(128, dtype=np.float32)
    common["ecap"] = np.ascontiguousarray(np.broadcast_to((np.arange(NE) * CAP).astype(np.float32)[None, :], (128, NE)))
    common["utri"] = np.triu(np.ones((128, 128), np.float32), k=1)
    slopes = 2.0 ** (-np.arange(1, NH + 1, dtype=np.float64))
    maps = []
    for c in range(ncores):
        b, half = c // 2, c % 2
        own = slice(half * TOK, (half + 1) * TOK)
        oth = slice((1 - half) * TOK, (2 - half) * TOK)
        xb = x[b]
        m = dict(common)
        m["xT_kv"] = np.ascontiguousarray(np.concatenate([xb[own], xb[oth]], axis=0).T)
        xo = np.zeros((TOK + 2, D), np.float32)
        xo[1:TOK + 1] = xb[own]
        if half == 1:
            xo[0] = xb[TOK - 1]
        else:
            xo[TOK + 1] = xb[TOK]
        m["xT_own"] = np.ascontiguousarray(xo.T)
        m["x_tok"] = np.ascontiguousarray(xb[own])
        m["memT"] = np.ascontiguousarray(mem[b].T)
        qpos = np.arange(half * TOK, (half + 1) * TOK)
        kpos = np.concatenate([np.arange(half * TOK, (half + 1) * TOK), np.arange((1 - half) * TOK, (2 - half) * TOK)])
        sig_other = 1.0 if half == 0 else -1.0
        ka = np.zeros((NH, 4, SEQ), np.float32)
        qa = np.zeros((NH, 2, 4, TOK), np.float32)
        for h in range(NH):
            s = slopes[h]
            ksig = np.ones(SEQ)
            ksig[TOK:] = sig_other
            ka[h, 0] = ksig
            ka[h, 1] = ksig
            ka[h, 2] = -64.0 * s * (kpos // 64) * ksig
            ka[h, 3] = -s * (kpos % 64) * ksig
            plus = np.stack([64.0 * s * (qpos // 64), s * (qpos % 64), np.ones(TOK), np.ones(TOK)])
            qa[h, 0] = plus
            qa[h, 1] = -plus
        m["kaug"] = ka
        m["qaug"] = qa
        maps.append(m)
    return maps


_CACHE = {}


def kernel(**inputs):
    stop_after = float(os.environ.get("MK_STOP", "99"))
    dbg = os.environ.get("MK_DBG", "0") == "1"
    key = (stop_after, dbg)
    if key not in _CACHE:
        _CACHE[key] = build_program(stop_after, dbg)
    nc = _CACHE[key]
    ncores = int(os.environ.get("MK_CORES", "8"))
    maps = _host_inputs(inputs, ncores)
    if os.environ.get("MK_TRACE", "0") == "1":
        res = run_bass_kernel_spmd(nc, maps, core_ids=list(range(ncores)), trace=True)
        print("EXEC_TIME_NS", res.exec_time_ns)
    else:
        res = run_bass_kernel_spmd(nc, maps, core_ids=list(range(ncores)))
    if dbg:
        return res
    outp = np.empty((NB, SEQ, D), np.float32)
    for c in range(ncores):
        b, half = c // 2, c % 2
        outp[b, half * TOK:(half + 1) * TOK] = res.results[c]["out"]
    return outp
```

```python
import math
import os
from contextlib import ExitStack

import numpy as np

import concourse.bass as bass
import concourse.mybir as mybir
from concourse.bass_utils import run_bass_kernel_spmd

F32 = mybir.dt.float32
BF16 = mybir.dt.bfloat16
I32 = mybir.dt.int32
AF = mybir.ActivationFunctionType
ALU = mybir.AluOpType
AX = mybir.AxisListType

D = 2048
SEQ = 8192
NB = 4
TOK = 4096
NH = 8
NE = 32
CAP = 768
NROWS = NE * CAP
ALPHA = 2.0 ** 0.25
LAMBDA_INIT = 0.8 - 0.6 * math.exp(0.0)
EPS = 1e-5
BIGROW = 1.0e6


class _Op:
    __slots__ = ("eng", "fn", "deps", "marked", "sem", "inc", "sigval", "is_dma", "gidx")

    def __init__(self, eng, fn):
        self.eng = eng
        self.fn = fn
        self.deps = []
        self.marked = False
        self.sem = None
        self.inc = 1
        self.sigval = None
        self.is_dma = False
        self.gidx = 0


class Sched:
    ENGS = ("pe", "act", "dve", "pool", "sp")

    def __init__(self, nc, stack):
        self.nc = nc
        self.stack = stack
        self.ops = {e: [] for e in self.ENGS}
        self.buf = {}
        self.sems = {}
        self.dma_count = {}
        self.dma_hist = {}
        self.gcount = 0
        self.last_dma = {}
        self.regs = {}
        self.pool_prev = {}

    def sem(self, name):
        if name not in self.sems:
            self.sems[name] = self.stack.enter_context(self.nc.semaphore(name))
        return self.sems[name]

    def _track(self, op, reads, writes):
        deps = []
        for k in reads:
            st = self.buf.get(k)
            if st is None:
                st = self.buf[k] = [None, []]
            if st[0] is not None:
                deps.append(st[0])
            st[1].append(op)
        for k in writes:
            st = self.buf.get(k)
            if st is None:
                st = self.buf[k] = [None, []]
            if st[0] is not None:
                deps.append(st[0])
            deps.extend(st[1])
            self.buf[k] = [op, []]
        seen = set()
        for d in deps:
            if d is op or id(d) in seen:
                continue
            seen.add(id(d))
            if d.eng == "pe" and op.eng == "pe" and not d.is_dma and not op.is_dma:
                continue
            d.marked = True
            op.deps.append(d)

    def op(self, eng, fn, reads=(), writes=()):
        o = _Op(eng, fn)
        self.gcount += 1
        o.gidx = self.gcount
        o.sem = "c_" + eng
        self._track(o, reads, writes)
        self.ops[eng].append(o)
        return o

    def dma(self, eng, fn, sem, reads=(), writes=()):
        o = _Op(eng, fn)
        self.gcount += 1
        o.gidx = self.gcount
        o.is_dma = True
        if eng == "pool":
            self.prot = (getattr(self, "prot", -1) + 1) % 16
            o.sem = "d_pq%d" % self.prot
            prev = self.last_dma.get(o.sem) or self.pool_prev.get(o.sem)
            if prev is not None:
                o.deps.append(prev)
            self.pool_prev[o.sem] = o
        else:
            o.sem = "d_" + sem
        o.inc = 16
        o.marked = True
        c = self.dma_count.get(o.sem, 0) + 16
        self.dma_count[o.sem] = c
        o.sigval = c
        self.dma_hist.setdefault(o.sem, []).append((o.gidx, c))
        self.last_dma[o.sem] = o
        self._track(o, reads, writes)
        self.ops[eng].append(o)
        return o

    def barrier(self):
        lasts = []
        for e in self.ENGS:
            for o in reversed(self.ops[e]):
                if not o.is_dma and o.fn is not None:
                    lasts.append(o)
                    break
        lasts.extend(self.last_dma.values())
        self.last_dma = {}
        for o in lasts:
            o.marked = True
        for e in self.ENGS:
            b = _Op(e, None)
            self.gcount += 1
            b.gidx = self.gcount
            b.sem = "c_" + e
            b.deps = [o for o in lasts]
            self.ops[e].append(b)
        self.buf = {}

    def emit(self, final_waits=()):
        nc = self.nc
        cnt = {e: 0 for e in self.ENGS}
        for e in self.ENGS:
            for o in self.ops[e]:
                if o.is_dma or o.fn is None:
                    continue
                if o.marked:
                    cnt[e] += 1
                    o.sigval = cnt[e]
        for name in sorted({o.sem for e in self.ENGS for o in self.ops[e] if o.marked}):
            self.sem(name)
        fw = {}
        for (ename, o) in final_waits:
            fw.setdefault(ename, []).append((o.sem, o.sigval))

        def run(ename, eng):
            waited = {}
            for o in self.ops[ename]:
                need = {}
                for d in o.deps:
                    v = d.sigval
                    if d.is_dma:
                        for (g, c) in self.dma_hist[d.sem]:
                            if g < o.gidx and c > v:
                                v = c
                    if need.get(d.sem, 0) < v:
                        need[d.sem] = v
                for sname, v in need.items():
                    if waited.get(sname, 0) >= v:
                        continue
                    if sname == "c_" + ename and ename == "pe":
                        continue
                    waited[sname] = v
                    eng.wait_ge(self.sems[sname], v)
                if o.fn is None:
                    continue
                ins = o.fn(eng)
                if o.marked:
                    ins.then_inc(self.sems[o.sem], o.inc)
            for (sname, v) in fw.get(ename, ()):
                eng.wait_ge(self.sems[sname], v)

        with nc.Block() as block:
            @block.tensor
            def _(eng):
                run("pe", eng)

            @block.scalar
            def _(eng):
                run("act", eng)

            @block.vector
            def _(eng):
                run("dve", eng)

            @block.gpsimd
            def _(eng):
                run("pool", eng)

            @block.sync
            def _(eng):
                run("sp", eng)


def MM(out, lhsT, rhs, start, stop):
    return lambda e: e.matmul(out, lhsT, rhs, start=start, stop=stop)


def TR(out, in_, ident):
    return lambda e: e.transpose(out, in_, ident)


def DMA(out, in_):
    return lambda e: e.dma_start(out=out, in_=in_)


def ACTF(out, in_, func, bias=None, scale=1.0, accum=None):
    def f(e):
        kw = {}
        if bias is not None:
            kw["bias"] = bias
        if accum is not None:
            kw["accum_out"] = accum
        return e.activation(out, in_, func, scale=scale, **kw)
    return f


def TC(out, in_):
    return lambda e: e.tensor_copy(out, in_)


def TS(out, in0, s1, s2, op0, op1=None):
    if op1 is None:
        return lambda e: e.tensor_scalar(out, in0, s1, None, op0=op0)
    return lambda e: e.tensor_scalar(out, in0, s1, s2, op0=op0, op1=op1)


def TT(out, in0, in1, op):
    return lambda e: e.tensor_tensor(out, in0, in1, op=op)


def STT(out, in0, scalar, in1, op0, op1):
    return lambda e: e.scalar_tensor_tensor(out, in0, scalar, in1, op0=op0, op1=op1)


def MEMSET(ap, v):
    return lambda e: e.memset(ap, v)

def mm_group(S, out_ap, pskey, pairs, reads):
    n = len(pairs)
    for i, (l, r) in enumerate(pairs):
        edge = (i == 0 or i == n - 1)
        S.op("pe", MM(out_ap, l, r, i == 0, i == n - 1),
             reads=reads if edge else (), writes=[pskey] if edge else ())


class SB:
    def __init__(self, big, nwords):
        self.big = big
        self.n = nwords
        self.off = 0
        self.base = 0

    def alloc(self, cols, dtype):
        size = 4 if dtype in (F32, I32) else 2
        words = (cols * size + 3) // 4
        assert self.off + words <= self.n, ("SBUF overflow", self.off, words, self.n)
        ap = self.big[:, self.off:self.off + words]
        self.off += words
        if dtype != F32:
            ap = ap.bitcast(dtype)
        return ap

    def persist(self):
        self.base = self.off

    def reset(self):
        self.off = self.base


def v3(ap, a):
    return ap.rearrange("p (a b) -> p a b", a=a)


def build_program(stop_after=99, dbg=False):
    nc = bass.Bass("TRN2", target_bir_lowering=False)

    def din(name, shape, dt=F32):
        return nc.dram_tensor(name, list(shape), dt, kind="ExternalInput").ap()

    def dscr(name, shape, dt, out=False):
        return nc.dram_tensor(name, list(shape), dt, kind="ExternalOutput" if out else "Internal").ap()

    xT_kv = din("xT_kv", [D, SEQ])
    xT_own = din("xT_own", [D, TOK + 2])
    x_tok = din("x_tok", [TOK, D])
    memT = din("memT", [D, 256])
    w_in = din("w_in", [D, 6144])
    convw = din("convw", [128, 24])
    subln = din("subln", [128, 1])
    lamv = din("lamv", [4, 64])
    w_out = din("w_out", [D, D])
    lnp = din("lnp", [6, D])
    mem_wq = din("mem_wq", [D, D])
    mem_wkv = din("mem_wkv", [D, 2 * D])
    mem_wo = din("mem_wo", [D, D])
    router_w = din("router_w", [D, NE])
    router_b = din("router_b", [NE])
    if stop_after >= 6:
        w_gate = din("w_gate", [NE, D, D])
        w_up = din("w_up", [NE, D, D])
        w_down = din("w_down", [NE, D, D])
    bgu = din("bgu", [128, 2 * NE * 16])
    b_down = din("b_down", [NE, D])
    kaug = din("kaug", [NH, 4, SEQ])
    qaug = din("qaug", [NH, 2, 4, TOK])
    tdiag = din("tdiag", [128, 4 * 512])
    ident_in = din("ident", [128, 128])
    ecap = din("ecap", [128, NE])
    utri = din("utri", [128, 128])

    out = dscr("out", [TOK, D], F32, out=True)
    KT = dscr("KT", [16, 64, SEQ], BF16, out=(dbg and stop_after == 1))
    VV = dscr("VV", [SEQ, 1024], BF16, out=(dbg and stop_after == 1))
    QT = dscr("QT", [16, 64, TOK], BF16, out=(dbg and stop_after == 2))
    OACT = dscr("OACT", [D, TOK], BF16, out=(dbg and stop_after in (2, 3)))
    X1 = dscr("X1", [TOK, D], F32, out=(dbg and stop_after == 4))
    X1T = dscr("X1T", [D, TOK], BF16)
    OM = dscr("OM", [D, TOK], BF16)
    X2 = dscr("X2", [TOK, D], F32, out=(dbg and stop_after == 5))
    XG = dscr("XG", [NROWS, D], BF16)
    YG = dscr("YG", [NROWS, D], F32)
    IDX4 = dscr("IDX4", [TOK, 4], I32, out=(dbg and stop_after == 5))
    G4 = dscr("G4", [TOK, 4], F32, out=(dbg and stop_after == 5))

    with ExitStack() as st:
        S = Sched(nc, st)
        NW = 53200
        big = st.enter_context(nc.sbuf_tensor("big", [128, NW], F32))
        sb = SB(big, NW)
        ps = st.enter_context(nc.psum_tensor("ps", [128, 4096], F32))

        def bank(i, n=512, off=0):
            return ps[:, i * 512 + off:i * 512 + off + n]

        ident = sb.alloc(128, F32)
        identb = sb.alloc(128, BF16)
        ones_b = sb.alloc(128, BF16)
        lam = sb.alloc(1, F32)
        mk_sb = sb.alloc(16 * 256, BF16)
        mv_sb = sb.alloc(2 * 2048, BF16)
        sb.persist()

        S.dma("sp", DMA(ident, ident_in), "c0", writes=["ident"])
        S.op("dve", TC(identb, ident), reads=["ident"], writes=["identb"])
        S.op("dve", MEMSET(ones_b, 1.0), writes=["ones_b"])

        def bcreg(e):
            if "bc" not in S.regs:
                S.regs["bc"] = e.to_reg(NROWS - 1)
            return S.regs["bc"]


        def phase0():
            lv = sb.alloc(4 * 64, F32)
            pr = sb.alloc(2 * 64, F32)
            sm = sb.alloc(2, F32)
            S.dma("sp", DMA(lv, lamv.rearrange("a b -> (a b)").partition_broadcast(128)), "c1", writes=["lv"])
            S.op("dve", TT(pr[:, 0:64], lv[:, 0:64], lv[:, 64:128], ALU.mult), reads=["lv"], writes=["pr0"])
            S.op("dve", TT(pr[:, 64:128], lv[:, 128:192], lv[:, 192:256], ALU.mult), reads=["lv"], writes=["pr1"])
            S.op("dve", lambda e: e.reduce_sum(sm[:, 0:1], pr[:, 0:64], axis=AX.X), reads=["pr0"], writes=["sm0"])
            S.op("dve", lambda e: e.reduce_sum(sm[:, 1:2], pr[:, 64:128], axis=AX.X), reads=["pr1"], writes=["sm1"])
            S.op("act", ACTF(sm, sm, AF.Exp), reads=["sm0", "sm1"], writes=["sme"])
            S.op("dve", TT(lam, sm[:, 0:1], sm[:, 1:2], ALU.subtract), reads=["sme"], writes=["lam0"])
            S.op("dve", TS(lam, lam, LAMBDA_INIT, None, ALU.add), reads=["lam0"], writes=["lam"])
            mT = sb.alloc(16 * 256, BF16)
            mT3 = v3(mT, 16)
            S.dma("pool", DMA(mT3, memT.rearrange("(kc p) m -> p kc m", p=128)), "mT", writes=["mT"])
            wg = [sb.alloc(16 * 512, BF16) for _ in range(2)]
            mk3 = v3(mk_sb, 16)
            mv3 = v3(mv_sb, 2)
            for g in range(8):
                w = wg[g % 2]
                w3 = v3(w, 16)
                wk = "wg%d" % (g % 2)
                S.dma("pool", DMA(w3, mem_wkv[:, g * 512:(g + 1) * 512].rearrange("(kc p) c -> p kc c", p=128)),
                      wk, writes=[wk])
                if g < 4:
                    for s_ in range(4):
                        b = (g * 4 + s_) % 4
                        mm_group(S, bank(b, 256), "ps%d" % b,
                                 [(w3[:, kc, s_ * 128:(s_ + 1) * 128], mT3[:, kc, :]) for kc in range(16)], [wk, "mT"])
                        last = S.op("act", lambda e, b=b, c=g * 4 + s_: e.copy(mk3[:, c, :], bank(b, 256)),
                                    reads=["ps%d" % b], writes=["mk%d" % (g * 4 + s_)])
                else:
                    for m in range(2):
                        b = 4 + (g * 2 + m) % 4
                        mm_group(S, bank(b), "ps%d" % b,
                                 [(mT3[:, kc, m * 128:(m + 1) * 128], w3[:, kc, :]) for kc in range(16)], [wk, "mT"])
                        S.op("dve", TC(mv3[:, m, (g - 4) * 512:(g - 3) * 512], bank(b)),
                             reads=["ps%d" % b], writes=["mv%d_%d" % (m, g)])

        phase0()
        S.barrier()
        sb.reset()

        def phase1a():
            wk_sb = sb.alloc(16 * 1024, BF16)
            wv_sb = sb.alloc(16 * 1024, BF16)
            wk3 = v3(wk_sb, 16)
            wv3 = v3(wv_sb, 16)
            for h in range(2):
                S.dma("pool", DMA(wk3[:, :, h * 512:(h + 1) * 512],
                                  w_in[:, 1024 + h * 512:1024 + (h + 1) * 512].rearrange("(kc p) c -> p kc c", p=128)),
                      "wk", writes=["wk%d" % h])
                S.dma("pool", DMA(wv3[:, :, h * 512:(h + 1) * 512],
                                  w_in[:, 2048 + h * 512:2048 + (h + 1) * 512].rearrange("(kc p) c -> p kc c", p=128)),
                      "wv", writes=["wv%d" % h])
            xs = [sb.alloc(16 * 512, BF16) for _ in range(2)]
            kst = [sb.alloc(512, BF16) for _ in range(4)]
            vst = [sb.alloc(1024, BF16) for _ in range(2)]
            nt = SEQ // 512
            ki = 0
            vi = 0
            for t in range(nt):
                x = xs[t % 2]
                x3 = v3(x, 16)
                xk = "x%d" % (t % 2)
                S.dma("pool", DMA(x3, xT_kv[:, t * 512:(t + 1) * 512].rearrange("(kc p) c -> p kc c", p=128)),
                      xk, writes=[xk])
                for c in range(8):
                    b = c % 4
                    mm_group(S, bank(b), "ps%d" % b,
                             [(wk3[:, kc, c * 128:(c + 1) * 128], x3[:, kc, :]) for kc in range(16)], [xk, "wk0", "wk1"])
                    ks = kst[ki % 4]
                    kk = "kst%d" % (ki % 4)
                    ki += 1
                    S.op("act", lambda e, ks=ks, b=b: e.copy(ks, bank(b)), reads=["ps%d" % b], writes=[kk])
                    for hh in range(2):
                        S.dma("sp", DMA(KT[2 * c + hh, :, t * 512:(t + 1) * 512], ks[hh * 64:(hh + 1) * 64, :]),
                              kk, reads=[kk])
                for s_ in range(4):
                    vs = vst[vi % 2]
                    vk = "vst%d" % (vi % 2)
                    vi += 1
                    for g in range(2):
                        b = 4 + (s_ * 2 + g) % 4
                        mm_group(S, bank(b), "ps%d" % b,
                                 [(x3[:, kc, s_ * 128:(s_ + 1) * 128], wv3[:, kc, g * 512:(g + 1) * 512]) for kc in range(16)],
                                 [xk, "wv0", "wv1"])
                        S.op("dve", TC(vs[:, g * 512:(g + 1) * 512], bank(b)), reads=["ps%d" % b], writes=[vk + "_%d" % g])
                    S.dma("sp", DMA(VV[t * 512 + s_ * 128:t * 512 + (s_ + 1) * 128, :], vs),
                          vk, reads=[vk + "_0", vk + "_1"])

        phase1a()
        S.barrier()
        sb.reset()
        if stop_after <= 1:
            fin = S.dma("sp", DMA(out[0:128, 0:128], ident), "fin", reads=["ident"])
            S.emit(final_waits=[("sp", fin)])
            return nc

        def phase1b():
            wq_sb = sb.alloc(16 * 4096, BF16)
            w3 = v3(wq_sb, 16)
            for g in range(8):
                src = g * 512 if g < 2 else 3072 + (g - 2) * 512
                S.dma("pool", DMA(w3[:, :, g * 512:(g + 1) * 512],
                                  w_in[:, src:src + 512].rearrange("(kc p) c -> p kc c", p=128)),
                      "w1b", writes=["w1b_%d" % g])
            wkeys = ["w1b_%d" % g for g in range(8)]
            cw = sb.alloc(24, F32)
            S.dma("sp", DMA(cw, convw), "c1", writes=["cw"])
            xs = [sb.alloc(16 * 512, BF16) for _ in range(2)]
            qst = [sb.alloc(512, BF16) for _ in range(2)]
            csb = [sb.alloc(512, F32) for _ in range(2)]
            cu = [sb.alloc(512, F32) for _ in range(2)]
            acc = [sb.alloc(512, F32) for _ in range(2)]
            ocv = [sb.alloc(512, BF16) for _ in range(2)]
            qi_ = 0
            ci_ = 0
            for j in range(9):
                h0 = 510 * j
                w = min(512, TOK + 2 - h0)
                x = xs[j % 2]
                x3 = v3(x, 16)
                xk = "x%d" % (j % 2)
                S.dma("pool", DMA(x3[:, :, 0:w], xT_own[:, h0:h0 + w].rearrange("(kc p) c -> p kc c", p=128)),
                      xk, writes=[xk])
                for c in range(8):
                    b = c % 2
                    mm_group(S, bank(b, w), "ps%d" % b,
                             [(w3[:, kc, c * 128:(c + 1) * 128], x3[:, kc, 0:w]) for kc in range(16)], [xk] + wkeys)
                    qs = qst[qi_ % 2]
                    qk = "qst%d" % (qi_ % 2)
                    qi_ += 1
                    S.op("act", ACTF(qs[:, 0:w], bank(b, w), AF.Copy, scale=0.125), reads=["ps%d" % b], writes=[qk])
                    for hh in range(2):
                        S.dma("sp", DMA(QT[2 * c + hh, :, h0:h0 + w - 2], qs[hh * 64:(hh + 1) * 64, 1:w - 1]),
                              qk, reads=[qk])
                for cc in range(8):
                    par = ci_ % 2
                    ci_ += 1
                    bB, bC, bU = 2 + 3 * par, 3 + 3 * par, 4 + 3 * par
                    for (bb, off) in ((bB, 1024), (bC, 2048), (bU, 3072)):
                        mm_group(S, bank(bb, w), "ps%d" % bb,
                                 [(w3[:, kc, off + cc * 128:off + (cc + 1) * 128], x3[:, kc, 0:w]) for kc in range(16)],
                                 [xk] + wkeys)
                    cs, cu_, ac, oc = csb[par], cu[par], acc[par], ocv[par]
                    S.op("act", lambda e, cs=cs, bC=bC, w=w: e.copy(cs[:, 0:w], bank(bC, w)),
                         reads=["ps%d" % bC], writes=["cs%d" % par])
                    S.op("dve", TT(cu_[:, 0:w], cs[:, 0:w], bank(bU, w), ALU.mult),
                         reads=["cs%d" % par, "ps%d" % bU], writes=["cu%d" % par])
                    S.op("dve", TS(ac[:, 0:w - 2], cu_[:, 1:w - 1], cw[:, cc * 3 + 1:cc * 3 + 2], None, ALU.mult),
                         reads=["cu%d" % par, "cw"], writes=["ac%d" % par])
                    S.op("dve", STT(ac[:, 0:w - 2], cu_[:, 0:w - 2], cw[:, cc * 3:cc * 3 + 1], ac[:, 0:w - 2], ALU.mult, ALU.add),
                         reads=["cu%d" % par, "cw", "ac%d" % par], writes=["ac%d" % par])
                    S.op("dve", STT(ac[:, 0:w - 2], cu_[:, 2:w], cw[:, cc * 3 + 2:cc * 3 + 3], ac[:, 0:w - 2], ALU.mult, ALU.add),
                         reads=["cu%d" % par, "cw", "ac%d" % par], writes=["ac%d" % par])
                    S.op("dve", TT(oc[:, 0:w - 2], ac[:, 0:w - 2], bank(bB, w)[:, 1:w - 1], ALU.mult),
                         reads=["ac%d" % par, "ps%d" % bB], writes=["oc%d" % par])
                    S.dma("sp", DMA(OACT[1024 + cc * 128:1024 + (cc + 1) * 128, h0:h0 + w - 2], oc[:, 0:w - 2]),
                          "oc%d" % par, reads=["oc%d" % par])

        phase1b()
        S.barrier()
        sb.reset()
        if stop_after <= 2:
            fin = S.dma("sp", DMA(out[0:128, 0:128], ident), "fin", reads=["ident"])
            S.emit(final_waits=[("sp", fin)])
            return nc

        def phase2():
            T0 = sb.alloc(2048, F32)
            S.dma("sp", DMA(T0, tdiag), "c1", writes=["T0"])
            sl_t = sb.alloc(1, F32)
            S.dma("sp", DMA(sl_t, subln), "c0", writes=["sl_raw"])
            subs = sb.alloc(1, F32)
            S.op("dve", TS(subs, sl_t, 1.0 - LAMBDA_INIT, None, ALU.mult), reads=["sl_raw"], writes=["subs"])
            neglam = sb.alloc(1, F32)
            S.op("dve", TS(neglam, lam, -1.0, None, ALU.mult), writes=["neglam"])
            eps_t = sb.alloc(1, F32)
            S.op("dve", MEMSET(eps_t, EPS), writes=["eps_t"])
            sets = []
            for _ in range(2):
                kx = [sb.alloc(SEQ, BF16) for _ in range(2)]
                vh = sb.alloc(64 * 128, BF16)
                qx = [[sb.alloc(TOK, BF16) for _ in range(2)] for _ in range(2)]
                sets.append((kx, vh, qx))
            Eb = [sb.alloc(512, BF16) for _ in range(4)]
            tmpb = [sb.alloc(512, F32) for _ in range(2)]
            rzt = sb.alloc(512, F32)
            ob = [sb.alloc(512, F32) for _ in range(2)]
            df = sb.alloc(512, F32)
            sq = sb.alloc(512, BF16)
            sd = sb.alloc(512, F32)
            o16 = [sb.alloc(512, BF16) for _ in range(2)]
            dcnt = [0]
            ocnt = [0]
            cnt = [0]

            def loads(h):
                st_ = h % 2
                kx, vh, qx = sets[st_]
                vh3 = v3(vh, 64)
                semn = "set%d" % st_
                for b in range(2):
                    S.dma("sp", DMA(kx[b][0:64, :], KT[2 * h + b]), semn, writes=["Kr%d%d" % (st_, b)])
                    S.dma("pool", DMA(kx[b][64:68, :], kaug[h]), semn, writes=["Ka%d%d" % (st_, b)])
                    for sg in range(2):
                        S.dma("sp", DMA(qx[b][sg][0:64, :], QT[2 * h + b]), semn, writes=["Qr%d%d%d" % (st_, b, sg)])
                        S.dma("pool", DMA(qx[b][sg][64:68, :], qaug[h, sg]), semn, writes=["Qa%d%d%d" % (st_, b, sg)])
                for part in range(4):
                    S.dma("sp", DMA(vh3[:, part * 16:(part + 1) * 16, :],
                                    VV[part * 2048:(part + 1) * 2048, h * 128:(h + 1) * 128].rearrange("(t p) d -> p t d", p=128)),
                          semn, writes=["V%d_%d" % (st_, part)])

            items = [(h, qi, b, kj) for h in range(NH) for qi in range(8) for b in range(2) for kj in range(64)]
            slot = {}
            LA = 3
            deferred = []

            def stage1(n):
                h, qi, b, kj = items[n]
                slope = 2.0 ** (-(h + 1))
                st_ = h % 2
                kx, vh, qx = sets[st_]
                c_ = cnt[0]
                cnt[0] += 1
                slot[n] = n % 4
                sbk = 4 + c_ % 4
                E = Eb[n % 4]
                ek = "E%d" % (n % 4)
                kkeys = ["Kr%d%d" % (st_, b), "Ka%d%d" % (st_, b)]
                diag = (kj < 32 and 4 * qi <= kj < 4 * qi + 4)
                if diag:
                    sg, rows = 0, 64
                else:
                    sg = 1 if (kj < 32 and kj < 4 * qi) else 0
                    rows = 68
                qkeys = ["Qr%d%d%d" % (st_, b, sg), "Qa%d%d%d" % (st_, b, sg)]
                S.op("pe", MM(bank(sbk), kx[b][0:rows, kj * 128:(kj + 1) * 128],
                              qx[b][sg][0:rows, qi * 512:(qi + 1) * 512], True, True),
                     reads=kkeys + qkeys, writes=["ps%d" % sbk])
                if diag:
                    v = kj - 4 * qi
                    tm = tmpb[dcnt[0] % 2]
                    tk = "tmp%d" % (dcnt[0] % 2)
                    dcnt[0] += 1
                    S.op("dve", STT(tm, T0[:, v * 512:(v + 1) * 512], slope, bank(sbk), ALU.mult, ALU.add),
                         reads=["T0", "ps%d" % sbk], writes=[tk])
                    S.op("act", ACTF(E, tm, AF.Exp), reads=[tk], writes=[ek])
                else:
                    S.op("act", ACTF(E, bank(sbk), AF.Exp), reads=["ps%d" % sbk], writes=[ek])

            def epi_b(h, qi):
                c_ = cnt[0]
                cnt[0] += 1
                sbk = 4 + c_ % 4
                S.op("pe", MM(bank(sbk), ones_b, sq, True, True), reads=["sq", "ones_b"], writes=["ps%d" % sbk])
                S.op("act", ACTF(sd, bank(sbk), AF.Sqrt, bias=eps_t[:, 0:1], scale=1.0 / 128.0),
                     reads=["ps%d" % sbk, "eps_t"], writes=["sd"])
                S.op("dve", lambda e: e.reciprocal(sd, sd), reads=["sd"], writes=["sd"])
                S.op("dve", TT(df, df, sd, ALU.mult), reads=["df", "sd"], writes=["df"])
                o_ = o16[ocnt[0] % 2]
                ok_ = "o16_%d" % (ocnt[0] % 2)
                ocnt[0] += 1
                S.op("dve", TS(o_, df, subs[:, 0:1], None, ALU.mult), reads=["df", "subs"], writes=[ok_])
                S.dma("sp", DMA(OACT[h * 128:(h + 1) * 128, qi * 512:(qi + 1) * 512], o_), ok_, reads=[ok_])

            def stage2(m, n):
                h, qi, b, kj = items[m]
                st_ = h % 2
                kx, vh, qx = sets[st_]
                vh3 = v3(vh, 64)
                vkeys = ["V%d_%d" % (st_, part) for part in range(4)]
                E = Eb[slot[m]]
                ek = "E%d" % slot[m]
                Ob, Zb = 2 * b, 2 * b + 1
                edge = (kj == 0 or kj == 63)
                S.op("pe", MM(bank(Ob), vh3[:, kj, :], E, kj == 0, kj == 63),
                     reads=[ek] + vkeys, writes=["ps%d" % Ob] if edge else ())
                S.op("pe", MM(bank(Zb), ones_b, E, kj == 0, kj == 63),
                     reads=[ek, "ones_b"], writes=["ps%d" % Zb] if edge else ())
                if kj == 63:
                    S.op("dve", lambda e, Zb=Zb: e.reciprocal(rzt, bank(Zb)), reads=["ps%d" % Zb], writes=["rzt"])
                    S.op("dve", TT(ob[b], bank(Ob), rzt, ALU.mult), reads=["ps%d" % Ob, "rzt"], writes=["ob%d" % b])
                    if b == 1:
                        S.op("dve", STT(df, ob[1], neglam[:, 0:1], ob[0], ALU.mult, ALU.add),
                             reads=["ob0", "ob1", "neglam"], writes=["df"])
                        S.op("act", ACTF(sq, df, AF.Square), reads=["df"], writes=["sq"])
                        deferred.append((n + 6, h, qi))

            loads(0)
            nit = len(items)
            for n in range(nit + LA + 8):
                if n < nit:
                    h, qi, b, kj = items[n]
                    if (qi, b, kj) == (0, 0, 0) and h + 1 < NH:
                        loads(h + 1)
                    stage1(n)
                m = n - LA
                if 0 <= m < nit:
                    stage2(m, n)
                while deferred and deferred[0][0] <= n:
                    _, hh, qq = deferred.pop(0)
                    epi_b(hh, qq)
            assert not deferred

        phase2()
        S.barrier()
        sb.reset()
        if stop_after <= 3:
            fin = S.dma("sp", DMA(out[0:128, 0:128], ident), "fin", reads=["ident"])
            S.emit(final_waits=[("sp", fin)])
            return nc

        def ln_rows(y, gb, bb, st4, junk, tag):
            S.op("dve", lambda e: e.reduce_sum(st4[:, 0:1], y, axis=AX.X), reads=[tag], writes=[tag + "s0"])
            S.op("dve", TS(st4[:, 1:2], st4[:, 0:1], -1.0 / D, None, ALU.mult), reads=[tag + "s0"], writes=[tag + "s1"])
            S.op("act", ACTF(junk, y, AF.Square, bias=st4[:, 1:2], accum=st4[:, 2:3]),
                 reads=[tag, tag + "s1"], writes=[tag + "s2", "junk"])
            S.op("dve", TS(st4[:, 3:4], st4[:, 2:3], 1.0 / D, EPS, ALU.mult, ALU.add), reads=[tag + "s2"], writes=[tag + "s3"])
            S.op("act", lambda e: e.sqrt(st4[:, 3:4], st4[:, 3:4]), reads=[tag + "s3"], writes=[tag + "s3"])
            S.op("dve", lambda e: e.reciprocal(st4[:, 3:4], st4[:, 3:4]), reads=[tag + "s3"], writes=[tag + "s3"])
            S.op("dve", TS(y, y, st4[:, 1:2], st4[:, 3:4], ALU.add, ALU.mult), reads=[tag, tag + "s1", tag + "s3"], writes=[tag])
            S.op("pool", TT(y, y, gb, ALU.mult), reads=[tag, "gb"], writes=[tag])
            S.op("pool", TT(y, y, bb, ALU.add), reads=[tag, "bb"], writes=[tag])

        def proj_ln(inT, Wd, resid, gi, post, extra_alloc=None):
            W = sb.alloc(16 * D, BF16)
            W3 = v3(W, 16)
            for g in range(4):
                S.dma("pool", DMA(W3[:, :, g * 512:(g + 1) * 512],
                                  Wd[:, g * 512:(g + 1) * 512].rearrange("(kc p) c -> p kc c", p=128)),
                      "wp", writes=["W%d" % g])
            wkeys = ["W%d" % g for g in range(4)]
            gb = sb.alloc(D, F32)
            bb = sb.alloc(D, F32)
            S.dma("sp", DMA(gb, lnp[gi].partition_broadcast(128)), "c0", writes=["gb"])
            S.dma("sp", DMA(bb, lnp[gi + 1].partition_broadcast(128)), "c1", writes=["bb"])
            ins = [sb.alloc(16 * 512, BF16) for _ in range(2)]
            xt = [sb.alloc(D, F32) for _ in range(2)]
            ys = [sb.alloc(D, F32) for _ in range(2)]
            st4 = [sb.alloc(4, F32) for _ in range(2)]
            junk = sb.alloc(D, BF16)
            ctx = extra_alloc() if extra_alloc else None
            def ld_in(g):
                S.dma("sp", DMA(v3(ins[g % 2], 16), inT[:, g * 512:(g + 1) * 512].rearrange("(kc p) t -> p kc t", p=128)),
                      "in%d" % (g % 2), writes=["in%d" % (g % 2)])

            def ld_x(tt):
                S.dma("sp", DMA(xt[tt % 2], resid[tt * 128:(tt + 1) * 128, :]), "xt%d" % (tt % 2), writes=["xt%d" % (tt % 2)])

            ld_in(0)
            ld_x(0)
            for grp in range(8):
                i3 = v3(ins[grp % 2], 16)
                ik = "in%d" % (grp % 2)
                if grp + 1 < 8:
                    ld_in(grp + 1)
                for s_ in range(4):
                    t = grp * 4 + s_
                    par = t % 2
                    x_, y_ = xt[par], ys[par]
                    if t + 1 < 32:
                        ld_x(t + 1)
                    for cg in range(4):
                        mm_group(S, bank(cg), "ps%d" % cg,
                                 [(i3[:, kc, s_ * 128:(s_ + 1) * 128], W3[:, kc, cg * 512:(cg + 1) * 512]) for kc in range(16)],
                                 [ik] + wkeys)
                    yk = "y%d" % par
                    for cg in range(4):
                        S.op("dve", STT(y_[:, cg * 512:(cg + 1) * 512], x_[:, cg * 512:(cg + 1) * 512], ALPHA, bank(cg),
                                        ALU.mult, ALU.add),
                             reads=["xt%d" % par, "ps%d" % cg], writes=[yk])
                    ln_rows(y_, gb, bb, st4[par], junk, yk)
                    post(ctx, grp, s_, t, y_, yk)

        def phase3():
            def extra():
                return {"x1b": [sb.alloc(D, BF16) for _ in range(2)],
                        "x1T": [sb.alloc(16 * 512, BF16) for _ in range(2)]}

            def post(ctx, grp, s_, t, y_, yk):
                par = t % 2
                S.dma("sp", DMA(X1[t * 128:(t + 1) * 128, :], y_), "x1o%d" % par, reads=[yk])
                xb_ = ctx["x1b"][par]
                S.op("act", lambda e: e.copy(xb_, y_), reads=[yk], writes=["x1b%d" % par])
                psb = ps[:, 2048:3072].bitcast(BF16)
                for kc in range(16):
                    S.op("pe", TR(psb[:, kc * 128:(kc + 1) * 128], xb_[:, kc * 128:(kc + 1) * 128], identb),
                         reads=["x1b%d" % par, "identb"], writes=["pst"] if kc in (0, 15) else ())
                xT3 = v3(ctx["x1T"][grp % 2], 16)
                tk = "x1T%d" % (grp % 2)
                S.op("act", lambda e: e.copy(xT3[:, :, s_ * 128:(s_ + 1) * 128], v3(psb, 16)),
                     reads=["pst"] + ([tk] if s_ > 0 else []), writes=[tk])
                if s_ == 3:
                    S.dma("sp", DMA(X1T[:, grp * 512:(grp + 1) * 512].rearrange("(kc p) t -> p kc t", p=128), xT3),
                          tk, reads=[tk])

            proj_ln(OACT, w_out, x_tok, 0, post, extra)

        phase3()
        S.barrier()
        sb.reset()
        if stop_after <= 4:
            fin = S.dma("sp", DMA(out[0:128, 0:128], ident), "fin", reads=["ident"])
            S.emit(final_waits=[("sp", fin)])
            return nc

        def phase4a():
            W = sb.alloc(16 * D, BF16)
            W3 = v3(W, 16)
            for g in range(4):
                S.dma("pool", DMA(W3[:, :, g * 512:(g + 1) * 512],
                                  mem_wq[:, g * 512:(g + 1) * 512].rearrange("(kc p) c -> p kc c", p=128)),
                      "wp", writes=["W%d" % g])
            wkeys = ["W%d" % g for g in range(4)]
            mk3 = v3(mk_sb, 16)
            mv3 = v3(mv_sb, 2)
            ins = [sb.alloc(16 * 512, BF16) for _ in range(2)]
            qT = sb.alloc(16 * 512, BF16)
            qT3 = v3(qT, 16)
            omT = [sb.alloc(16 * 512, BF16) for _ in range(2)]
            st4 = [sb.alloc(4, F32) for _ in range(2)]
            pe_ = [sb.alloc(256, F32) for _ in range(2)]
            pn = [sb.alloc(256, BF16) for _ in range(2)]
            pT = [sb.alloc(256, BF16) for _ in range(2)]
            scale = 512.0 ** -0.5
            psb = ps[:, 3072:3584].bitcast(BF16)
            it = 0
            for grp in range(8):
                i3 = v3(ins[grp % 2], 16)
                ik = "in%d" % (grp % 2)
                S.dma("sp", DMA(i3, X1T[:, grp * 512:(grp + 1) * 512].rearrange("(kc p) t -> p kc t", p=128)), ik, writes=[ik])
                for c in range(16):
                    b = c % 2
                    mm_group(S, bank(b), "ps%d" % b,
                             [(W3[:, kc, c * 128:(c + 1) * 128], i3[:, kc, :]) for kc in range(16)], [ik] + wkeys)
                    if c % 2 == 0:
                        S.op("act", lambda e, c=c, b=b: e.copy(qT3[:, c, :], bank(b)), reads=["ps%d" % b], writes=["qT%d" % c])
                    else:
                        S.op("dve", TC(qT3[:, c, :], bank(b)), reads=["ps%d" % b], writes=["qT%d" % c])
                o3 = v3(omT[grp % 2], 16)
                ok_ = "omT%d" % (grp % 2)
                for s_ in range(4):
                    for hd in range(4):
                        par = it % 2
                        it += 1
                        sbk = 2 + par
                        mm_group(S, bank(sbk, 256), "ps%d" % sbk,
                                 [(qT3[:, hd * 4 + c, s_ * 128:(s_ + 1) * 128], mk3[:, hd * 4 + c, :]) for c in range(4)],
                                 ["qT%d" % (hd * 4 + c) for c in range(4)])
                        s4 = st4[par]
                        sk = "s4_%d" % par
                        S.op("dve", lambda e, s4=s4, sbk=sbk: e.reduce_max(s4[:, 0:1], bank(sbk, 256), axis=AX.X),
                             reads=["ps%d" % sbk], writes=[sk + "a"])
                        S.op("dve", TS(s4[:, 1:2], s4[:, 0:1], -scale, None, ALU.mult), reads=[sk + "a"], writes=[sk + "b"])
                        S.op("act", ACTF(pe_[par], bank(sbk, 256), AF.Exp, bias=s4[:, 1:2], scale=scale, accum=s4[:, 2:3]),
                             reads=["ps%d" % sbk, sk + "b"], writes=["pe%d" % par, sk + "c"])
                        S.op("dve", lambda e, s4=s4: e.reciprocal(s4[:, 3:4], s4[:, 2:3]), reads=[sk + "c"], writes=[sk + "d"])
                        S.op("dve", TS(pn[par], pe_[par], s4[:, 3:4], None, ALU.mult), reads=["pe%d" % par, sk + "d"], writes=["pn%d" % par])
                        for mt in range(2):
                            S.op("pe", TR(psb[:, (par * 2 + mt) * 128:(par * 2 + mt + 1) * 128], pn[par][:, mt * 128:(mt + 1) * 128], identb),
                                 reads=["pn%d" % par, "identb"], writes=["pstb%d" % par] if mt in (0, 1) else ())
                        S.op("act", lambda e, par=par: e.copy(pT[par], psb[:, par * 256:(par + 1) * 256]),
                             reads=["pstb%d" % par], writes=["pT%d" % par])
                        obk = 4 + par
                        for dc in range(4):
                            for mt in range(2):
                                S.op("pe", MM(bank(obk, 128, dc * 128), mv3[:, mt, hd * 512 + dc * 128:hd * 512 + (dc + 1) * 128],
                                              pT[par][:, mt * 128:(mt + 1) * 128], mt == 0, mt == 1),
                                     reads=["pT%d" % par], writes=["ps%d" % obk] if (dc, mt) in ((0, 0), (3, 1)) else ())
                        S.op("dve", lambda e, o3=o3, hd=hd, s_=s_, obk=obk: e.tensor_copy(
                            o3[:, hd * 4:(hd + 1) * 4, s_ * 128:(s_ + 1) * 128], v3(bank(obk), 4)),
                             reads=["ps%d" % obk] + ([ok_] if (s_, hd) != (0, 0) else []), writes=[ok_])
                S.dma("sp", DMA(OM[:, grp * 512:(grp + 1) * 512].rearrange("(kc p) t -> p kc t", p=128), o3), ok_, reads=[ok_])

        phase4a()
        S.barrier()
        sb.reset()
        if stop_after <= 4.5:
            fin = S.dma("sp", DMA(out[0:128, 0:128], ident), "fin", reads=["ident"])
            S.emit(final_waits=[("sp", fin)])
            return nc

        def phase4b():
            def extra():
                c = {}
                c["x2b"] = [sb.alloc(D, BF16) for _ in range(2)]
                c["x2T"] = sb.alloc(16 * 128, F32)
                c["rw"] = sb.alloc(16 * NE, F32)
                c["rb"] = sb.alloc(NE, F32)
                c["ut"] = sb.alloc(128, F32)
                c["onesf"] = sb.alloc(128, F32)
                c["ec"] = sb.alloc(NE, F32)
                c["base"] = sb.alloc(NE, F32)
                for nm in ("lg", "mask", "ex", "G", "rowid", "keyv", "oh"):
                    c[nm] = sb.alloc(NE, F32)
                c["mx"] = sb.alloc(8, F32)
                c["kmx"] = sb.alloc(8, F32)
                c["sm"] = sb.alloc(4, F32)
                c["rows4"] = sb.alloc(4, F32)
                c["idx4"] = [sb.alloc(4, I32) for _ in range(2)]
                c["g4"] = [sb.alloc(4, F32) for _ in range(2)]
                S.dma("sp", DMA(v3(c["rw"], 16), router_w.rearrange("(kc p) e -> p kc e", p=128)), "c0", writes=["rw"])
                S.dma("sp", DMA(c["rb"], router_b.partition_broadcast(128)), "c1", writes=["rb"])
                S.dma("sp", DMA(c["ut"], utri), "c0", writes=["ut"])
                S.dma("sp", DMA(c["ec"], ecap), "c1", writes=["ec"])
                S.op("dve", MEMSET(c["onesf"], 1.0), writes=["onesf"])
                S.op("dve", MEMSET(c["base"], 0.0), writes=["base"])
                return c

            def post(c, grp, s_, t, y_, yk):
                par = t % 2
                S.dma("sp", DMA(X2[t * 128:(t + 1) * 128, :], y_), "x2o%d" % par, reads=[yk])
                xb_ = c["x2b"][par]
                xbk = "x2b%d" % par
                S.op("act", lambda e: e.copy(xb_, y_), reads=[yk], writes=[xbk])
                for kc in range(16):
                    S.op("pe", TR(ps[:, 2048 + kc * 128:2048 + (kc + 1) * 128], y_[:, kc * 128:(kc + 1) * 128], ident),
                         reads=[yk, "ident"], writes=["pst", "psp", "psc"] if kc in (0, 15) else ())
                x2T = c["x2T"]
                S.op("act", lambda e: e.copy(x2T[:, 0:1024], ps[:, 2048:3072]), reads=["pst"], writes=["x2Ta"])
                S.op("dve", TC(x2T[:, 1024:2048], ps[:, 3072:4096]), reads=["pst"], writes=["x2Tb"])
                rw3 = v3(c["rw"], 16)
                x2T3 = v3(x2T, 16)
                mm_group(S, ps[:, 2048:2048 + NE], "pst",
                         [(x2T3[:, kc, :], rw3[:, kc, :]) for kc in range(16)], ["x2Ta", "x2Tb", "rw"])
                lg, mask, ex, G, rowid, keyv, oh = (c[n] for n in ("lg", "mask", "ex", "G", "rowid", "keyv", "oh"))
                mx, kmx, sm, rows4 = c["mx"], c["kmx"], c["sm"], c["rows4"]
                S.op("dve", TT(lg, ps[:, 2048:2048 + NE], c["rb"], ALU.add), reads=["pst", "rb"], writes=["lg"])
                S.op("dve", lambda e: e.max(out=mx, in_=lg), reads=["lg"], writes=["mx"])
                S.op("dve", TS(mask, lg, mx[:, 3:4], None, ALU.is_ge), reads=["lg", "mx"], writes=["mask"])
                S.op("dve", TS(sm[:, 0:1], mx[:, 0:1], -1.0, None, ALU.mult), reads=["mx"], writes=["sm0"])
                S.op("act", ACTF(ex, lg, AF.Exp, bias=sm[:, 0:1]), reads=["lg", "sm0"], writes=["ex"])
                S.op("dve", TT(ex, ex, mask, ALU.mult), reads=["ex", "mask"], writes=["ex"])
                S.op("dve", lambda e: e.reduce_sum(sm[:, 1:2], ex, axis=AX.X), reads=["ex"], writes=["sm1"])
                S.op("dve", lambda e: e.reciprocal(sm[:, 1:2], sm[:, 1:2]), reads=["sm1"], writes=["sm1"])
                S.op("dve", TS(G, ex, sm[:, 1:2], None, ALU.mult), reads=["ex", "sm1"], writes=["G"])
                S.op("pe", MM(ps[:, 2560:2560 + NE], c["ut"], mask, True, True), reads=["ut", "mask"], writes=["psp"])
                S.op("pe", MM(ps[:, 2560 + NE:2560 + 2 * NE], c["onesf"], mask, True, True), reads=["onesf", "mask"], writes=["psc"])
                S.op("dve", TT(rowid, ps[:, 2560:2560 + NE], c["base"], ALU.add), reads=["psp", "base"], writes=["rowid"])
                S.op("dve", TT(c["base"], c["base"], ps[:, 2560 + NE:2560 + 2 * NE], ALU.add), reads=["psc", "base", "rowid"], writes=["base"])
                S.op("dve", TS(oh, rowid, float(CAP), None, ALU.is_lt), reads=["rowid"], writes=["oh"])
                S.op("dve", TT(oh, oh, mask, ALU.mult), reads=["oh", "mask"], writes=["oh"])
                S.op("dve", TT(rowid, rowid, c["ec"], ALU.add), reads=["rowid", "ec"], writes=["rowid"])
                S.op("dve", TS(keyv, rowid, -1.0, BIGROW, ALU.mult, ALU.add), reads=["rowid"], writes=["keyv"])
                S.op("dve", TT(keyv, keyv, oh, ALU.mult), reads=["keyv", "oh"], writes=["keyv"])
                S.op("dve", lambda e: e.max(out=kmx, in_=keyv), reads=["keyv"], writes=["kmx"])
                S.op("dve", TS(rows4, kmx[:, 0:4], -1.0, BIGROW, ALU.mult, ALU.add), reads=["kmx"], writes=["rows4"])
                i4 = c["idx4"][par]
                g4 = c["g4"][par]
                ik4 = "idx4_%d" % par
                gk4 = "g4_%d" % par
                S.op("dve", TC(i4, rows4), reads=["rows4"], writes=[ik4])
                for j in range(4):
                    S.op("dve", TS(oh, keyv, kmx[:, j:j + 1], None, ALU.is_equal), reads=["keyv", "kmx"], writes=["oh"])
                    S.op("dve", TT(oh, oh, G, ALU.mult), reads=["oh", "G"], writes=["oh"])
                    S.op("dve", lambda e, j=j: e.reduce_sum(g4[:, j:j + 1], oh, axis=AX.X), reads=["oh"], writes=[gk4 + "_%d" % j])
                for j in range(4):
                    S.dma("pool", lambda e, j=j: e.indirect_dma_start(
                        out=XG, out_offset=bass.IndirectOffsetOnAxis(ap=i4[:, j:j + 1], axis=0),
                        in_=xb_, in_offset=None, bounds_check=bcreg(e), oob_is_err=False),
                          "scat%d_%d" % (par, j), reads=[xbk, ik4])
                S.dma("sp", DMA(IDX4[t * 128:(t + 1) * 128, :], i4), "i4o%d" % par, reads=[ik4])
                S.dma("sp", DMA(G4[t * 128:(t + 1) * 128, :], g4), "g4o%d" % par, reads=[gk4 + "_%d" % j for j in range(4)])

            proj_ln(OM, mem_wo, X1, 2, post, extra)

        phase4b()
        S.barrier()
        sb.reset()
        if stop_after <= 5:
            fin = S.dma("sp", DMA(out[0:128, 0:128], ident), "fin", reads=["ident"])
            S.emit(final_waits=[("sp", fin)])
            return nc

        NST = CAP // 128
        R2 = CAP - 512

        def phase5():
            bg_sb = sb.alloc(2 * NE * 16, F32)
            S.dma("sp", DMA(bg_sb, bgu), "c0", writes=["bgu"])
            xgT = [sb.alloc(16 * CAP, BF16) for _ in range(2)]
            wt = [sb.alloc(16 * 512, BF16) for _ in range(4)]
            actT = sb.alloc(16 * CAP, BF16)
            a3 = v3(actT, 16)
            bd = [sb.alloc(D, F32) for _ in range(2)]
            xgt = [sb.alloc(D, BF16) for _ in range(2)]
            gs = [sb.alloc(CAP, F32) for _ in range(2)]
            sg = [sb.alloc(CAP, F32) for _ in range(2)]
            us = [sb.alloc(CAP, F32) for _ in range(2)]
            ysb = [sb.alloc(512, F32) for _ in range(4)]
            psb = ps[:, 3072:4096].bitcast(BF16)
            wi = 0
            xi = 0
            yi = 0
            fci = 0
            cst = {"xi": 0}

            def xg_load(e):
                ep = e % 2
                S.dma("sp", DMA(bd[ep], b_down[e].partition_broadcast(128)), "bd%d" % ep, writes=["bd%d" % ep])
                x3 = v3(xgT[ep], 16)
                xk = "xgT%d" % ep
                xi = cst["xi"]
                for st_ in range(NST):
                    xt_ = xgt[xi % 2]
                    xtk = "xgt%d" % (xi % 2)
                    xi += 1
                    r0 = e * CAP + st_ * 128
                    S.dma("sp", DMA(xt_, XG[r0:r0 + 128, :]), xtk, writes=[xtk])
                    for kc in range(16):
                        S.op("pe", TR(psb[:, kc * 128:(kc + 1) * 128], xt_[:, kc * 128:(kc + 1) * 128], identb),
                             reads=[xtk, "identb"], writes=["ps6", "ps7"] if kc in (0, 15) else ())
                    eng = "act" if st_ % 2 == 0 else "dve"
                    S.op(eng, TC(x3[:, :, st_ * 128:(st_ + 1) * 128], v3(psb, 16)) if eng == "dve" else
                         (lambda e_, x3=x3, st_=st_: e_.copy(x3[:, :, st_ * 128:(st_ + 1) * 128], v3(psb, 16))),
                         reads=["ps6", "ps7"] + ([xk] if st_ > 0 else []), writes=[xk])
                cst["xi"] = xi

            xg_load(0)
            for e in range(NE):
                ep = e % 2
                x3 = v3(xgT[ep], 16)
                xk = "xgT%d" % ep
                for fg in range(4):
                    wg = wt[wi % 4]
                    wgk = "wt%d" % (wi % 4)
                    wi += 1
                    wu = wt[wi % 4]
                    wuk = "wt%d" % (wi % 4)
                    wi += 1
                    S.dma("pool", DMA(v3(wg, 16), w_gate[e][:, fg * 512:(fg + 1) * 512].rearrange("(kc p) c -> p kc c", p=128)),
                          wgk, writes=[wgk])
                    S.dma("pool", DMA(v3(wu, 16), w_up[e][:, fg * 512:(fg + 1) * 512].rearrange("(kc p) c -> p kc c", p=128)),
                          wuk, writes=[wuk])
                    wg3, wu3 = v3(wg, 16), v3(wu, 16)
                    for fs in range(4):
                        fc = fg * 4 + fs
                        set_ = fci % 2
                        fci += 1
                        g0, g1, u0, u1 = 4 * set_, 4 * set_ + 1, 4 * set_ + 2, 4 * set_ + 3
                        if set_ == 1:
                            g0, g1, u0, u1 = 4, 5, 6, 7
                        mm_group(S, bank(g0), "ps%d" % g0,
                                 [(wg3[:, kc, fs * 128:(fs + 1) * 128], x3[:, kc, 0:512]) for kc in range(16)], [wgk, xk])
                        mm_group(S, bank(g1, R2), "ps%d" % g1,
                                 [(wg3[:, kc, fs * 128:(fs + 1) * 128], x3[:, kc, 512:CAP]) for kc in range(16)], [wgk, xk])
                        mm_group(S, bank(u0), "ps%d" % u0,
                                 [(wu3[:, kc, fs * 128:(fs + 1) * 128], x3[:, kc, 0:512]) for kc in range(16)], [wuk, xk])
                        mm_group(S, bank(u1, R2), "ps%d" % u1,
                                 [(wu3[:, kc, fs * 128:(fs + 1) * 128], x3[:, kc, 512:CAP]) for kc in range(16)], [wuk, xk])
                        gs_, sg_, us_ = gs[set_], sg[set_], us[set_]
                        bgc = bg_sb[:, e * 16 + fc:e * 16 + fc + 1]
                        buc = bg_sb[:, NE * 16 + e * 16 + fc:NE * 16 + e * 16 + fc + 1]
                        S.op("dve", TS(gs_, ps[:, g0 * 512:g0 * 512 + CAP], bgc, 7.0, ALU.add, ALU.min),
                             reads=["ps%d" % g0, "ps%d" % g1, "bgu"], writes=["gs%d" % set_])
                        S.op("act", ACTF(sg_, gs_, AF.Sigmoid, scale=1.702), reads=["gs%d" % set_], writes=["sg%d" % set_])
                        S.op("dve", TS(us_, ps[:, u0 * 512:u0 * 512 + CAP], buc, 7.0, ALU.add, ALU.min),
                             reads=["ps%d" % u0, "ps%d" % u1, "bgu"], writes=["us%d" % set_])
                        S.op("dve", TS(us_, us_, -7.0, 1.0, ALU.max, ALU.add), reads=["us%d" % set_], writes=["us%d" % set_])
                        S.op("dve", TT(gs_, gs_, sg_, ALU.mult), reads=["gs%d" % set_, "sg%d" % set_], writes=["gs%d" % set_])
                        S.op("dve", TT(a3[:, fc, :], gs_, us_, ALU.mult), reads=["gs%d" % set_, "us%d" % set_], writes=["actT%d" % fc])
                akeys = ["actT%d" % fc for fc in range(16)]
                if e + 1 < NE:
                    xg_load(e + 1)
                for dg in range(4):
                    wd = wt[wi % 4]
                    wdk = "wt%d" % (wi % 4)
                    wi += 1
                    S.dma("pool", DMA(v3(wd, 16), w_down[e][:, dg * 512:(dg + 1) * 512].rearrange("(fc p) c -> p fc c", p=128)),
                          wdk, writes=[wdk])
                    wd3 = v3(wd, 16)
                    for st_ in range(NST):
                        b = yi % 8
                        mm_group(S, bank(b), "ps%d" % b,
                                 [(a3[:, fc, st_ * 128:(st_ + 1) * 128], wd3[:, fc, :]) for fc in range(16)], [wdk] + akeys)
                        y_ = ysb[yi % 4]
                        yk = "ysb%d" % (yi % 4)
                        yi += 1
                        S.op("dve", TT(y_, bank(b), bd[ep][:, dg * 512:(dg + 1) * 512], ALU.add),
                             reads=["ps%d" % b, "bd%d" % ep], writes=[yk])
                        r0 = e * CAP + st_ * 128
                        S.dma("sp", DMA(YG[r0:r0 + 128, dg * 512:(dg + 1) * 512], y_), yk, reads=[yk])

        phase5()
        S.barrier()
        sb.reset()

        def phase6():
            gb = sb.alloc(D, F32)
            bb = sb.alloc(D, F32)
            S.dma("sp", DMA(gb, lnp[4].partition_broadcast(128)), "c0", writes=["gb"])
            S.dma("sp", DMA(bb, lnp[5].partition_broadcast(128)), "c1", writes=["bb"])
            yj = [sb.alloc(4 * D, F32) for _ in range(2)]
            x2t = [sb.alloc(D, F32) for _ in range(2)]
            ys = [sb.alloc(D, F32) for _ in range(2)]
            i4s = [sb.alloc(4, I32) for _ in range(2)]
            g4s = [sb.alloc(4, F32) for _ in range(2)]
            st4 = [sb.alloc(4, F32) for _ in range(2)]
            junk = sb.alloc(D, BF16)
            last = {}

            def fetch(t):
                par = t % 2
                i4, g4, x_, yy = i4s[par], g4s[par], x2t[par], yj[par]
                S.dma("sp", DMA(i4, IDX4[t * 128:(t + 1) * 128, :]), "i4l%d" % par, writes=["i4_%d" % par])
                S.dma("sp", DMA(g4, G4[t * 128:(t + 1) * 128, :]), "g4l%d" % par, writes=["g4_%d" % par])
                S.dma("sp", DMA(x_, X2[t * 128:(t + 1) * 128, :]), "x2l%d" % par, writes=["x2_%d" % par])
                S.op("pool", MEMSET(yy, 0.0), writes=["yj%d_%d" % (par, j) for j in range(4)])
                for j in range(4):
                    S.dma("pool", lambda e, j=j, yy=yy, i4=i4: e.indirect_dma_start(
                        out=yy[:, j * D:(j + 1) * D], out_offset=None, in_=YG,
                        in_offset=bass.IndirectOffsetOnAxis(ap=i4[:, j:j + 1], axis=0),
                        bounds_check=bcreg(e), oob_is_err=False),
                          "gat%d_%d" % (par, j), reads=["i4_%d" % par], writes=["yj%d_%d" % (par, j)])

            fetch(0)
            for t in range(TOK // 128):
                par = t % 2
                i4, g4, x_, y_, yy = i4s[par], g4s[par], x2t[par], ys[par], yj[par]
                if t + 1 < TOK // 128:
                    fetch(t + 1)
                yk = "y%d" % par
                S.op("act", ACTF(y_, x_, AF.Copy, scale=ALPHA), reads=["x2_%d" % par], writes=[yk])
                for j in range(4):
                    S.op("dve", STT(y_, yy[:, j * D:(j + 1) * D], g4[:, j:j + 1], y_, ALU.mult, ALU.add),
                         reads=["yj%d_%d" % (par, j), "g4_%d" % par, yk], writes=[yk])
                ln_rows(y_, gb, bb, st4[par], junk, yk)
                last[par] = S.dma("sp", DMA(out[t * 128:(t + 1) * 128, :], y_), "outo%d" % par, reads=[yk])
            return list(last.values())

        finals = phase6()
        S.emit(final_waits=[("sp", d) for d in finals])
    return nc


def _host_inputs(inp, ncores=8):
    x = np.asarray(inp["x"], np.float32)
    mem = np.asarray(inp["mem"], np.float32)
    common = {}
    common["w_in"] = np.ascontiguousarray(inp["w_in"][0])
    cw = np.asarray(inp["conv_w"][0], np.float32)
    common["convw"] = np.ascontiguousarray(cw.reshape(3, 8, 128).transpose(2, 1, 0).reshape(128, 24))
    common["subln"] = np.ascontiguousarray(np.asarray(inp["attn_subln_w"][0], np.float32).reshape(128, 1))
    common["lamv"] = np.ascontiguousarray(np.stack([inp["lambda_q1"][0], inp["lambda_k1"][0],
                                                    inp["lambda_q2"][0], inp["lambda_k2"][0]]).astype(np.float32))
    common["w_out"] = np.ascontiguousarray(inp["w_out"][0])
    common["lnp"] = np.ascontiguousarray(np.stack([inp["ln1_g"][0], inp["ln1_b"][0], inp["ln2_g"][0],
                                                   inp["ln2_b"][0], inp["ln3_g"][0], inp["ln3_b"][0]]).astype(np.float32))
    common["mem_wq"] = np.ascontiguousarray(inp["mem_wq"][0])
    common["mem_wkv"] = np.ascontiguousarray(inp["mem_wkv"][0])
    common["mem_wo"] = np.ascontiguousarray(inp["mem_wo"][0])
    common["router_w"] = np.ascontiguousarray(inp["router_w"][0])
    common["router_b"] = np.ascontiguousarray(inp["router_b"][0])
    if "w_gate" in inp:
        common["w_gate"] = np.ascontiguousarray(inp["w_gate"][0])
        common["w_up"] = np.ascontiguousarray(inp["w_up"][0])
        common["w_down"] = np.ascontiguousarray(inp["w_down"][0])
    bg = np.asarray(inp["b_gate"][0], np.float32).reshape(NE, 16, 128).transpose(2, 0, 1).reshape(128, NE * 16)
    bu = np.asarray(inp["b_up"][0], np.float32).reshape(NE, 16, 128).transpose(2, 0, 1).reshape(128, NE * 16)
    common["bgu"] = np.ascontiguousarray(np.concatenate([bg, bu], axis=1))
    common["b_down"] = np.ascontiguousarray(inp["b_down"][0])
    kr = np.arange(128)[:, None, None]
    vv = np.arange(4)[None, :, None]
    qr = np.arange(512)[None, None, :]
    common["tdiag"] = np.ascontiguousarray((-np.abs(vv * 128 + kr - qr)).astype(np.float32).reshape(128, 2048))
    common["ident"] = np.eye(128, dtype=np.float32)
    common["ecap"] = np.ascontiguousarray(np.broadcast_to((np.arange(NE) * CAP).astype(np.float32)[None, :], (128, NE)))
    common["utri"] = np.triu(np.ones((128, 128), np.float32), k=1)
    slopes = 2.0 ** (-np.arange(1, NH + 1, dtype=np.float64))
    maps = []
    for c in range(ncores):
        b, half = c // 2, c % 2
        own = slice(half * TOK, (half + 1) * TOK)
        oth = slice((1 - half) * TOK, (2 - half) * TOK)
        xb = x[b]
        m = dict(common)
        m["xT_kv"] = np.ascontiguousarray(np.concatenate([xb[own], xb[oth]], axis=0).T)
        xo = np.zeros((TOK + 2, D), np.float32)
        xo[1:TOK + 1] = xb[own]
        if half == 1:
            xo[0] = xb[TOK - 1]
        else:
            xo[TOK + 1] = xb[TOK]
        m["xT_own"] = np.ascontiguousarray(xo.T)
        m["x_tok"] = np.ascontiguousarray(xb[own])
        m["memT"] = np.ascontiguousarray(mem[b].T)
        qpos = np.arange(half * TOK, (half + 1) * TOK)
        kpos = np.concatenate([np.arange(half * TOK, (half + 1) * TOK), np.arange((1 - half) * TOK, (2 - half) * TOK)])
        sig_other = 1.0 if half == 0 else -1.0
        ka = np.zeros((NH, 4, SEQ), np.float32)
        qa = np.zeros((NH, 2, 4, TOK), np.float32)
        for h in range(NH):
            s = slopes[h]
            ksig = np.ones(SEQ)
            ksig[TOK:] = sig_other
            ka[h, 0] = ksig
            ka[h, 1] = ksig
            ka[h, 2] = -64.0 * s * (kpos // 64) * ksig
            ka[h, 3] = -s * (kpos % 64) * ksig
            plus = np.stack([64.0 * s * (qpos // 64), s * (qpos % 64), np.ones(TOK), np.ones(TOK)])
            qa[h, 0] = plus
            qa[h, 1] = -plus
        m["kaug"] = ka
        m["qaug"] = qa
        maps.append(m)
    return maps


_CACHE = {}


def kernel(**inputs):
    stop_after = float(os.environ.get("MK_STOP", "99"))
    dbg = os.environ.get("MK_DBG", "0") == "1"
    key = (stop_after, dbg)
    if key not in _CACHE:
        _CACHE[key] = build_program(stop_after, dbg)
    nc = _CACHE[key]
    ncores = int(os.environ.get("MK_CORES", "8"))
    maps = _host_inputs(inputs, ncores)
    if os.environ.get("MK_TRACE", "0") == "1":
        res = run_bass_kernel_spmd(nc, maps, core_ids=list(range(ncores)), trace=True)
        print("EXEC_TIME_NS", res.exec_time_ns)
    else:
        res = run_bass_kernel_spmd(nc, maps, core_ids=list(range(ncores)))
    if dbg:
        return res
    outp = np.empty((NB, SEQ, D), np.float32)
    for c in range(ncores):
        b, half = c // 2, c % 2
        outp[b, half * TOK:(half + 1) * TOK] = res.results[c]["out"]
    return outp
```
